# Optimizing a Trainium2 kernel written in Bass

```python
import jax, jax.numpy as jnp
from jax import lax
import numpy as np

D_MODEL = 2048
BATCH = 8
SEQ = 2048
DEPTH = 1

N_HEADS = 16
HEAD_DIM = 128
ATTN_WIDTH = N_HEADS * HEAD_DIM
ROPE_THETA = 10000.0
MOBA_BLOCK = 256
MOBA_TOPK = 3
Q_CHUNK = 16
CONV_CHANNELS = D_MODEL
CONV_WIDTH = 31
N_GROUPS = 4
EXPERTS_PER_GROUP = 8
N_EXPERTS = N_GROUPS * EXPERTS_PER_GROUP
TOP_K_IN_GROUP = 2
D_FF_EXPERT = 1024
EXPERT_BLOCK = 128
LN_EPS = 1e-5
DN_ALPHA = (2.0 * DEPTH) ** 0.25
DN_BETA = (8.0 * DEPTH) ** -0.25
IN_WIDTH = 3 * ATTN_WIDTH + 2 * CONV_CHANNELS + 2 * D_MODEL

kernel_name = 'hybrid_moba_conformer_hiermoe_deepnorm'


def _layer_norm(x, g, b):
    xf = x.astype(jnp.float32)
    mu = jnp.mean(xf, axis=-1, keepdims=True)
    var = jnp.mean(jnp.square(xf - mu), axis=-1, keepdims=True)
    y = (xf - mu) * lax.rsqrt(var + LN_EPS)
    return (y * g.astype(jnp.float32) + b.astype(jnp.float32)).astype(x.dtype)


def _rope(t, pos):
    half = HEAD_DIM // 2
    inv_freq = jnp.power(ROPE_THETA, -jnp.arange(half, dtype=jnp.float32) * (2.0 / HEAD_DIM))
    ang = pos.astype(jnp.float32)[:, None] * inv_freq[None, :]
    cos, sin = jnp.cos(ang), jnp.sin(ang)
    tf = t.astype(jnp.float32)
    t1, t2 = tf[..., :half], tf[..., half:]
    return jnp.concatenate([t1 * cos - t2 * sin, t2 * cos + t1 * sin], axis=-1).astype(t.dtype)


def _moba_attention(q, k, v):
    B, H, S, Dh = q.shape
    nb = -(-S // MOBA_BLOCK)
    s_pad = nb * MOBA_BLOCK
    pad = ((0, 0), (0, 0), (0, s_pad - S), (0, 0))
    k_blk = jnp.pad(k, pad).reshape(B, H, nb, MOBA_BLOCK, Dh)
    v_blk = jnp.pad(v, pad).reshape(B, H, nb, MOBA_BLOCK, Dh)
    k_mean = jnp.mean(k_blk.astype(jnp.float32), axis=3)

    pos = jnp.arange(S)
    q_blk_id = pos // MOBA_BLOCK
    gate = jnp.einsum('bhsd,bhnd->bhsn', q.astype(jnp.float32), k_mean)
    past = jnp.arange(nb)[None, :] < q_blk_id[:, None]
    gate = jnp.where(past, gate, -jnp.inf)
    n_sel = min(MOBA_TOPK, nb)
    _, sel_idx = lax.top_k(gate, n_sel)
    sel_valid = jnp.arange(n_sel)[None, :] < q_blk_id[:, None]

    scale = Dh ** -0.5
    b_ix = jnp.arange(B)[:, None, None, None]
    h_ix = jnp.arange(H)[None, :, None, None]

    def chunk(c):
        t0 = c * Q_CHUNK
        q_c = lax.dynamic_slice_in_dim(q, t0, Q_CHUNK, axis=2)
        idx_c = lax.dynamic_slice_in_dim(sel_idx, t0, Q_CHUNK, axis=2)
        valid_c = lax.dynamic_slice_in_dim(sel_valid, t0, Q_CHUNK, axis=0)
        own = t0 // MOBA_BLOCK
        k_own = lax.dynamic_index_in_dim(k_blk, own, axis=2, keepdims=False)
        v_own = lax.dynamic_index_in_dim(v_blk, own, axis=2, keepdims=False)
        q_pos = t0 + jnp.arange(Q_CHUNK)
        k_pos = own * MOBA_BLOCK + jnp.arange(MOBA_BLOCK)
        causal = k_pos[None, :] <= q_pos[:, None]
        k_sel = k_blk[b_ix, h_ix, idx_c]
        v_sel = v_blk[b_ix, h_ix, idx_c]
        s_own = jnp.einsum('bhqd,bhkd->bhqk', q_c, k_own).astype(jnp.float32) * scale
        s_own = jnp.where(causal, s_own, -jnp.inf)
        s_sel = jnp.einsum('bhqd,bhqrkd->bhqrk', q_c, k_sel).astype(jnp.float32) * scale
        s_sel = jnp.where(valid_c[:, :, None], s_sel, -jnp.inf)
        s_all = jnp.concatenate([s_own, s_sel.reshape(B, H, Q_CHUNK, n_sel * MOBA_BLOCK)], axis=-1)
        p = jax.nn.softmax(s_all, axis=-1).astype(v.dtype)
        p_own = p[..., :MOBA_BLOCK]
        p_sel = p[..., MOBA_BLOCK:].reshape(B, H, Q_CHUNK, n_sel, MOBA_BLOCK)
        return (jnp.einsum('bhqk,bhkd->bhqd', p_own, v_own)
                + jnp.einsum('bhqrk,bhqrkd->bhqd', p_sel, v_sel))

    out = lax.map(chunk, jnp.arange(S // Q_CHUNK))
    return jnp.moveaxis(out, 0, 2).reshape(B, H, S, Dh)


def _conformer_conv(u, w_dw, b_dw, ln_g, ln_b, w_pw2, b_pw2):
    a, g = jnp.split(u, 2, axis=-1)
    h = a * jax.nn.sigmoid(g)
    h = lax.conv_general_dilated(
        h, w_dw[:, None, :].astype(h.dtype), window_strides=(1,),
        padding=[(CONV_WIDTH - 1, 0)],
        dimension_numbers=('NWC', 'WIO', 'NWC'),
        feature_group_count=h.shape[-1]) + b_dw
    h = jax.nn.silu(_layer_norm(h, ln_g, ln_b))
    return h @ w_pw2 + b_pw2


def _hier_moe(x, w_rg, b_rg, w_re, b_re, w1, w3, w2):
    B, S, D = x.shape
    T = B * S
    xt = x.reshape(T, D)
    g_logits = (xt @ w_rg).astype(jnp.float32) + b_rg.astype(jnp.float32)
    g_prob = jax.nn.softmax(g_logits, axis=-1)
    grp = jnp.argmax(g_logits, axis=-1)
    grp_w = jnp.take_along_axis(g_prob, grp[:, None], axis=-1)
    e_logits = jnp.einsum('td,gde->tge', xt, w_re).astype(jnp.float32) + b_re.astype(jnp.float32)
    e_logits = jnp.take_along_axis(e_logits, grp[:, None, None], axis=1)[:, 0]
    top_v, top_i = lax.top_k(e_logits, TOP_K_IN_GROUP)
    comb = jax.nn.softmax(top_v, axis=-1) * grp_w
    expert = grp[:, None] * EXPERTS_PER_GROUP + top_i

    A = T * TOP_K_IN_GROUP
    e_flat = expert.reshape(A).astype(jnp.int32)
    tok_flat = jnp.repeat(jnp.arange(T, dtype=jnp.int32), TOP_K_IN_GROUP)
    w_flat = comb.reshape(A)
    order = jnp.argsort(e_flat)
    e_sorted = e_flat[order]
    counts = jnp.zeros((N_EXPERTS,), jnp.int32).at[e_flat].add(1)
    starts = jnp.cumsum(counts) - counts
    padded = (counts + EXPERT_BLOCK - 1) // EXPERT_BLOCK * EXPERT_BLOCK
    pstarts = jnp.cumsum(padded) - padded
    pends = pstarts + padded
    dest = pstarts[e_sorted] + (jnp.arange(A, dtype=jnp.int32) - starts[e_sorted])
    n_blocks = -(-A // EXPERT_BLOCK) + N_EXPERTS
    P = n_blocks * EXPERT_BLOCK
    slot_tok = jnp.full((P,), T, jnp.int32).at[dest].set(tok_flat[order])
    slot_w = jnp.zeros((P,), w_flat.dtype).at[dest].set(w_flat[order])
    blk_expert = jnp.minimum(
        jnp.searchsorted(pends, jnp.arange(n_blocks, dtype=jnp.int32) * EXPERT_BLOCK, side='right'),
        N_EXPERTS - 1)
    x_pad = jnp.concatenate([xt, jnp.zeros((1, D), xt.dtype)], axis=0)

    def run_block(args):
        tok, w, e = args
        h = x_pad[tok]
        y = (jax.nn.silu(h @ w1[e]) * (h @ w3[e])) @ w2[e]
        return y * w[:, None].astype(y.dtype)

    y_slots = lax.map(run_block, (slot_tok.reshape(n_blocks, EXPERT_BLOCK),
                                  slot_w.reshape(n_blocks, EXPERT_BLOCK), blk_expert))
    y = jnp.zeros((T + 1, D), y_slots.dtype).at[slot_tok].add(y_slots.reshape(P, D))
    return y[:T].reshape(B, S, D).astype(x.dtype)


def setup_inputs(seed: int = 0) -> dict:
    key = jax.random.key(seed)
    ks = jax.random.split(key, 22)
    L, D, A, C = DEPTH, D_MODEL, ATTN_WIDTH, CONV_CHANNELS
    G, E, F = N_GROUPS, EXPERTS_PER_GROUP, D_FF_EXPERT

    def nrm(k, shape, s):
        return jax.random.normal(k, shape, jnp.float32) * s

    col_scale = jnp.concatenate([jnp.ones((2 * A,), jnp.float32),
                                 jnp.full((A + 2 * C,), DN_BETA, jnp.float32),
                                 jnp.ones((2 * D,), jnp.float32)]) * (D ** -0.5)
    return {
        'x': nrm(ks[0], (BATCH, SEQ, D), 1.0),
        'w_in': nrm(ks[1], (L, D, IN_WIDTH), 1.0) * col_scale,
        'b_in': nrm(ks[2], (L, IN_WIDTH), 0.02),
        'w_o_attn': nrm(ks[3], (L, A, D), A ** -0.5 * DN_BETA),
        'w_dw': nrm(ks[4], (L, CONV_WIDTH, C), CONV_WIDTH ** -0.5),
        'b_dw': nrm(ks[5], (L, C), 0.02),
        'conv_ln_g': 1.0 + nrm(ks[6], (L, C), 0.02),
        'conv_ln_b': nrm(ks[7], (L, C), 0.02),
        'w_pw2': nrm(ks[8], (L, C, D), C ** -0.5 * DN_BETA),
        'b_pw2': nrm(ks[9], (L, D), 0.02),
        'w_out': nrm(ks[10], (L, D, D), D ** -0.5 * DN_BETA),
        'ln1_g': 1.0 + nrm(ks[11], (L, D), 0.02),
        'ln1_b': nrm(ks[12], (L, D), 0.02),
        'w_rg': nrm(ks[13], (L, D, G), D ** -0.5),
        'b_rg': nrm(ks[14], (L, G), 0.01),
        'w_re': nrm(ks[15], (L, G, D, E), D ** -0.5),
        'b_re': nrm(ks[16], (L, G, E), 0.01),
        'w1': nrm(ks[17], (L, G * E, D, F), D ** -0.5 * DN_BETA),
        'w3': nrm(ks[18], (L, G * E, D, F), D ** -0.5 * DN_BETA),
        'w2': nrm(ks[19], (L, G * E, F, D), F ** -0.5 * DN_BETA),
        'ln2_g': 1.0 + nrm(ks[20], (L, D), 0.02),
        'ln2_b': nrm(ks[21], (L, D), 0.02),
    }


def reference(x, w_in, b_in, w_o_attn, w_dw, b_dw, conv_ln_g, conv_ln_b, w_pw2, b_pw2,
              w_out, ln1_g, ln1_b, w_rg, b_rg, w_re, b_re, w1, w3, w2, ln2_g, ln2_b):
    B, S, D = x.shape
    pos = jnp.arange(S)
    A, C = ATTN_WIDTH, CONV_CHANNELS
    for l in range(DEPTH):
        u = x @ w_in[l] + b_in[l]
        q, k, v, glu_in, gates = jnp.split(u, [A, 2 * A, 3 * A, 3 * A + 2 * C], axis=-1)

        def heads(t):
            return t.reshape(B, S, N_HEADS, HEAD_DIM).transpose(0, 2, 1, 3)

        qh = _rope(heads(q), pos)
        kh = _rope(heads(k), pos)
        o = _moba_attention(qh, kh, heads(v))
        y_attn = o.transpose(0, 2, 1, 3).reshape(B, S, A) @ w_o_attn[l]
        y_conv = _conformer_conv(glu_in, w_dw[l], b_dw[l], conv_ln_g[l], conv_ln_b[l],
                                 w_pw2[l], b_pw2[l])
        g_attn, g_conv = jnp.split(gates, 2, axis=-1)
        merged = jax.nn.sigmoid(g_attn) * y_attn + jax.nn.sigmoid(g_conv) * y_conv
        h = _layer_norm(DN_ALPHA * x + merged @ w_out[l], ln1_g[l], ln1_b[l])
        moe = _hier_moe(h, w_rg[l], b_rg[l], w_re[l], b_re[l], w1[l], w3[l], w2[l])
        x = _layer_norm(DN_ALPHA * h + moe, ln2_g[l], ln2_b[l])
    return x
```

```python
import numpy as np
import ml_dtypes
import concourse.bass as bass
import concourse.mybir as mybir
from concourse.bass_utils import run_bass_kernel_spmd

F32 = mybir.dt.float32
F32R = mybir.dt.float32r
BF16 = mybir.dt.bfloat16
I32 = mybir.dt.int32
AF = mybir.ActivationFunctionType
OP = mybir.AluOpType
AX = mybir.AxisListType

S = 2048
D = 2048
NH = 16
NE = 32
FF = 1024
CAP = 256
NS = NE * CAP
LN_EPS = 1e-5
ALPHA = 2.0 ** 0.25
SCALE = 128.0 ** -0.5
NEG = -30000.0
OOBROW = 2 * S + 100


class T:
    __slots__ = ("ap", "w", "r")

    def __init__(self, ap=None):
        self.ap = ap
        self.w = {}
        self.r = {}


class Eng:
    def __init__(self, name, strict_self):
        self.name = name
        self.key = name
        self.insts = []
        self.count = 0
        self.seen = {}
        self.strict = strict_self
        self.ring_vals = None
        self.ring_pos = 0


class Sched:
    def __init__(self, nc, es, nring_sp=12, nring_pool=8):
        self.nc = nc
        self.es = es
        self.nrot = 0
        self.e = {
            "pe": Eng("pe", False),
            "act": Eng("act", True),
            "dve": Eng("dve", True),
            "pool": Eng("pool", True),
            "sp": Eng("sp", False),
        }
        self.h = {"pe": nc.tensor, "act": nc.scalar, "dve": nc.vector, "pool": nc.gpsimd, "sp": nc.sync}
        self.e["sp"].ring_vals = [0] * nring_sp
        self.e["pool"].ring_vals = [0] * nring_pool
        self.sems = {}
        for en in ("pe", "act", "dve", "pool", "sp"):
            self.sems[en] = es.enter_context(nc.semaphore(f"s_{en}"))
        for qn in ("sp", "pool"):
            for idx in range(len(self.e[qn].ring_vals)):
                self.sems[(qn, idx)] = es.enter_context(nc.semaphore(f"r_{qn}{idx}"))

    def _waits(self, eng, reads, writes, extra=()):
        deps = {}

        def add(k, v):
            if deps.get(k, 0) < v:
                deps[k] = v

        for t in reads:
            for k, v in t.w.items():
                add(k, v)
        for t in writes:
            for k, v in t.w.items():
                add(k, v)
            for k, v in t.r.items():
                add(k, v)
        for k, v in extra:
            add(k, v)
        for k, v in deps.items():
            if k == eng.key and not eng.strict:
                continue
            if eng.seen.get(k, 0) < v:
                eng.seen[k] = v
                self.h[eng.name].wait_ge(self.sems[k], v)

    def op(self, en, fn, reads=(), writes=()):
        eng = self.e[en]
        self._waits(eng, reads, writes)
        eng.count += 1
        v = eng.count
        fn(self.h[en]).then_inc(self.sems[eng.key], 1)
        for t in writes:
            t.w = {eng.key: v}
            t.r = {}
        for t in reads:
            t.r[eng.key] = v

    def dma(self, qn, fn, reads=(), writes=()):
        eng = self.e[qn]
        idx = eng.ring_pos % len(eng.ring_vals)
        eng.ring_pos += 1
        key = (qn, idx)
        prev = eng.ring_vals[idx]
        self._waits(eng, reads, writes, extra=((key, prev),) if prev else ())
        val = prev + 16
        eng.ring_vals[idx] = val
        fn(self.h[qn]).then_inc(self.sems[key], 16)
        for t in writes:
            t.w = {key: val}
            t.r = {}
        for t in reads:
            t.r[key] = val

    def fence(self):
        for en, eng in self.e.items():
            for fn_, f in self.e.items():
                if fn_ != en and f.count and eng.seen.get(f.key, 0) < f.count:
                    eng.seen[f.key] = f.count
                    self.h[en].wait_ge(self.sems[f.key], f.count)
            for qn in ("sp", "pool"):
                for idx, v in enumerate(self.e[qn].ring_vals):
                    if v and eng.seen.get((qn, idx), 0) < v:
                        eng.seen[(qn, idx)] = v
                        self.h[en].wait_ge(self.sems[(qn, idx)], v)
        for en, eng in self.e.items():
            if eng.count > 1500:
                eng.seen[eng.key] = eng.count
                self.nrot += 1
                eng.key = (en, "ep", self.nrot)
                self.sems[eng.key] = self.es.enter_context(self.nc.semaphore(f"s_{en}_{self.nrot}"))
                eng.count = 0

    def finish(self):
        for qn in ("sp", "pool"):
            eng = self.e[qn]
            for idx, v in enumerate(eng.ring_vals):
                if v and eng.seen.get((qn, idx), 0) < v:
                    self.h[qn].wait_ge(self.sems[(qn, idx)], v)


import itertools
_uid = itertools.count()


class Ring:
    def __init__(self, tiles):
        self.t = tiles
        self.i = 0

    def next(self):
        t = self.t[self.i % len(self.t)]
        self.i += 1
        return t


def r32(ap):
    return ap.bitcast(F32R)


def build(phases="ABCDE", dbg=False):
    nc = bass.Bass("TRN2", target_bir_lowering=False)
    nc.dge_precook = False
    from contextlib import ExitStack
    es = ExitStack()
    sc = Sched(nc, es)
    reg_ns = nc.gpsimd.to_reg(NS - 1)
    reg_y = nc.gpsimd.to_reg(2 * S - 1)

    def din(name, shape, dt):
        return nc.dram_tensor(name, list(shape), dt, kind="ExternalInput").ap()

    def dscr(name, shape, dt):
        kind = "ExternalOutput" if dbg else "Internal"
        return nc.dram_tensor(name, list(shape), dt, kind=kind).ap()

    xT_d = din("xT", [D, S], F32R)
    x_d = din("x", [S, D], F32)
    wA_d = din("wA", [32, 128, 16, 128], F32R)
    wV_d = din("wV", [8, 128, 16, 256], F32R)
    wG_d = din("wG", [64, 128, 16, 128], F32R)
    bq_d = din("bq", [128, 32], F32)
    bv_d = din("bv", [1, 2048], F32)
    bG_d = din("bG", [128, 64], F32)
    wo_d = din("wo", [16, 128, 16, 128], F32R)
    wp_d = din("wp", [16, 128, 16, 128], F32R)
    wout_d = din("wout", [4, 128, 16, 512], F32R)
    cvec_d = din("cvec", [128, 16, 36], F32)
    lnv_d = din("lnv", [4, 2048], F32)
    wr_d = din("wr", [128, 16, 36], F32)
    br_d = din("br", [1, 36], F32)
    ned = NE if "D" in phases else 1
    w1_d = din("w1", [ned, 4, 128, 16, 256], F32R)
    w3_d = din("w3", [ned, 4, 128, 16, 256], F32R)
    w2_d = din("w2", [ned, 4, 128, 8, 512], F32R)
    rope_d = din("rope", [128, 4096], F32)
    cf_d = din("cf", [128, 1024], F32)
    on_d = din("onesn", [128, 128], F32R)
    cb_d = din("cb", [128, 2688], BF16)
    sinit_d = din("sinit", [NS, 4], I32)
    out_d = nc.dram_tensor("out", [S, D], F32, kind="ExternalOutput").ap()

    qk_s = dscr("qk_s", [32, 128, S], BF16)
    v_s = dscr("v_s", [S, D], BF16)
    ot_s = dscr("ot_s", [16, 128, S], F32R)
    h_s = dscr("h_s", [S, D], F32)
    y_s = dscr("y_s", [2 * S, D], F32)
    stab_s = dscr("stab_s", [NS, 4], I32)
    T_qk = [T() for _ in range(32)]
    T_v = [T() for _ in range(8)]
    T_ot = [T() for _ in range(16)]
    T_h = [T() for _ in range(16)]
    T_y = T()
    T_stab = T()

    def sb(name, shape, dt):
        return es.enter_context(nc.sbuf_tensor(f"s{next(_uid)}_" + name, list(shape), dt))

    psum = [es.enter_context(nc.psum_tensor(f"ps{i}", [128, 512], F32)) for i in range(8)]
    PS = [T(p) for p in psum]

    cf = sb("cf", [128, 1024], F32)
    cbt = sb("cb", [128, 2688], BF16)
    T_cf, T_cb = T(cf), T(cbt)
    sc.dma("sp", lambda h: h.dma_start(out=cf[:], in_=cf_d), writes=[T_cf])
    sc.dma("sp", lambda h: h.dma_start(out=cbt[:], in_=cb_d), writes=[T_cb])
    onesn_t = sb("onesn", [128, 128], F32R)
    sc.dma("sp", lambda h: h.dma_start(out=onesn_t[:], in_=on_d), writes=[T_cf])
    ident32 = cf[:, 0:128]
    pastbias = cf[:, 128:256]
    notown = cf[:, 256:384]
    onesn = onesn_t[:, :]
    U32 = cf[:, 512:640]
    ones32 = cf[:, 640:768]
    slotbase = cf[:, 768:800]
    iota_p = cf[:, 800:801]
    rotT = cbt[:, 0:128]
    identb = cbt[:, 128:256]
    tri = cbt[:, 256:384]
    onesb = cbt[:, 384:512]
    blkind = cbt[:, 640:2688]

    if "A" in phases:
        with ExitStack() as ea:
            def sba(name, shape, dt):
                return ea.enter_context(nc.sbuf_tensor(f"s{next(_uid)}_" + name, list(shape), dt))
            xT = sba("xT", [128, 16, S], F32R)
            T_xT = [T() for _ in range(4)]
            for tc in range(4):
                sc.dma("sp", lambda h, tc=tc: h.dma_start(
                    out=xT[:, :, tc * 512:(tc + 1) * 512],
                    in_=xT_d.rearrange("(kc p) t -> p kc t", p=128)[:, :, tc * 512:(tc + 1) * 512]),
                    writes=[T_xT[tc]])
            with ExitStack() as e1:
                def sb1(name, shape, dt):
                    return e1.enter_context(nc.sbuf_tensor(f"s{next(_uid)}_" + name, list(shape), dt))
                rope = sb1("rope", [128, 4096], F32)
                T_rope = T()
                sc.dma("sp", lambda h: h.dma_start(out=rope[:], in_=rope_d), writes=[T_rope])
                bq = sb1("bq", [128, 32], F32)
                T_bq = T()
                sc.dma("sp", lambda h: h.dma_start(out=bq[:], in_=bq_d), writes=[T_bq])
                wring = Ring([T(sb1(f"wA{i}", [128, 16, 128], F32R)) for i in range(4)])
                qbr = Ring([T(sb1(f"qb{i}", [128, 512], BF16)) for i in range(3)])
                t1r = Ring([T(sb1(f"t1{i}", [128, 512], F32)) for i in range(2)])
                t2r = Ring([T(sb1(f"t2{i}", [128, 512], F32)) for i in range(2)])
                str_ = Ring([T(sb1(f"st{i}", [128, S], BF16)) for i in range(2)])
                psA = Ring(PS[0:3])
                psR = Ring(PS[3:5])
                def rope_tail(nb, tc, stg, qb, last):
                    ps2 = psR.next()
                    sc.op("pe", lambda h: h.matmul(ps2.ap[:, :], lhsT=rotT, rhs=qb.ap[:, :], start=True, stop=True),
                          reads=[qb, T_cb], writes=[ps2])
                    t1 = t1r.next()
                    t2 = t2r.next()
                    sc.op("pool", lambda h: h.tensor_tensor(
                        out=t1.ap[:, :], in0=qb.ap[:, :], in1=rope[:, tc * 512:(tc + 1) * 512], op=OP.mult),
                        reads=[qb, T_rope], writes=[t1])
                    sc.op("dve", lambda h: h.tensor_tensor(
                        out=t2.ap[:, :], in0=ps2.ap[:, :], in1=rope[:, 2048 + tc * 512:2048 + (tc + 1) * 512],
                        op=OP.mult), reads=[ps2, T_rope], writes=[t2])
                    sc.op("dve", lambda h: h.tensor_tensor(
                        out=stg.ap[:, tc * 512:(tc + 1) * 512], in0=t1.ap[:, :], in1=t2.ap[:, :], op=OP.add),
                        reads=[t1, t2], writes=[stg])
                    if last:
                        sc.dma("pool", lambda h: h.dma_start(out=qk_s[nb], in_=stg.ap[:, :]), reads=[stg], writes=[T_qk[nb]])

                pend = None
                for nb in range(32):
                    W = wring.next()
                    sc.dma("sp", lambda h, W=W, nb=nb: h.dma_start(out=W.ap[:], in_=wA_d[nb]), writes=[W])
                    stg = str_.next()
                    for tc in range(4):
                        ps = psA.next()
                        for kc in range(16):
                            sc.op("pe", lambda h, ps=ps, W=W, kc=kc, tc=tc: h.matmul(
                                ps.ap[:, :], lhsT=r32(W.ap[:, kc, :]), rhs=r32(xT[:, kc, tc * 512:(tc + 1) * 512]),
                                start=(kc == 0), stop=(kc == 15)),
                                reads=[W, T_xT[tc]], writes=[ps])
                        qb = qbr.next()
                        sc.op("act", lambda h, qb=qb, ps=ps, nb=nb: h.activation(
                            out=qb.ap[:, :], in_=ps.ap[:, :], func=AF.Identity, bias=bq[:, nb:nb + 1], scale=1.0),
                            reads=[ps, T_bq], writes=[qb])
                        if pend is not None:
                            rope_tail(*pend)
                        pend = (nb, tc, stg, qb, tc == 3)
                rope_tail(*pend)
            sc.fence()
            with ExitStack() as e2:
                def sb2(name, shape, dt):
                    return e2.enter_context(nc.sbuf_tensor(f"s{next(_uid)}_" + name, list(shape), dt))
                bv = sb2("bv", [128, 2048], F32)
                T_bv = T()
                sc.dma("sp", lambda h: h.dma_start(out=bv[:], in_=bv_d.partition_broadcast(128)), writes=[T_bv])
                wvr = Ring([T(sb2(f"wV{i}", [128, 16, 256], F32R)) for i in range(2)])
                vst = Ring([T(sb2(f"vst{i}", [128, 16, 256], BF16)) for i in range(2)])
                psA = Ring(PS[0:4])
                for vb in range(8):
                    W = wvr.next()
                    sc.dma("sp", lambda h, W=W, vb=vb: h.dma_start(out=W.ap[:], in_=wV_d[vb]), writes=[W])
                    stg = vst.next()
                    for tt in range(16):
                        ps = psA.next()
                        for kc in range(16):
                            sc.op("pe", lambda h, ps=ps, W=W, kc=kc, tt=tt: h.matmul(
                                ps.ap[:, 0:256], lhsT=r32(xT[:, kc, tt * 128:(tt + 1) * 128]), rhs=r32(W.ap[:, kc, :]),
                                start=(kc == 0), stop=(kc == 15)),
                                reads=[W, T_xT[tt // 4]], writes=[ps])
                        sc.op("dve", lambda h, stg=stg, ps=ps, tt=tt, vb=vb: h.tensor_tensor(
                            out=stg.ap[:, tt, :], in0=ps.ap[:, 0:256], in1=bv[:, vb * 256:(vb + 1) * 256], op=OP.add),
                            reads=[ps, T_bv], writes=[stg])
                    sc.dma("pool", lambda h, stg=stg, vb=vb: h.dma_start(
                        out=v_s.rearrange("(tt p) c -> p tt c", p=128)[:, :, vb * 256:(vb + 1) * 256],
                        in_=stg.ap[:, :, :]), reads=[stg], writes=[T_v[vb]])

            sc.fence()
        sc.fence()
    if "B" in phases:
        with ExitStack() as eb:
            def sbb(name, shape, dt):
                return eb.enter_context(nc.sbuf_tensor(f"s{next(_uid)}_" + name, list(shape), dt))
            zt = T(sbb("zt", [128, 2048], F32))
            sc.op("pool", lambda h: h.memset(zt.ap[:, :], 0.0), writes=[zt])

            def zero_fill(hd):
                for i in (2 * hd, 2 * hd + 1):
                    sc.dma("sp", lambda h, i=i: h.dma_start(out=y_s[i * 128:(i + 1) * 128, :], in_=zt.ap[:, :]),
                           reads=[zt], writes=[T()])
            qr = Ring([T(sbb(f"q{i}", [128, S], BF16)) for i in range(2)])
            kr = Ring([T(sbb(f"k{i}", [128, S], BF16)) for i in range(2)])
            vr = Ring([T(sbb(f"v{i}", [128, 16, 132], BF16)) for i in range(2)])
            for t in vr.t:
                sc.op("pool", lambda h, t=t: h.memset(t.ap[:, :, 128:132], 1.0), writes=[t])
            ks32 = T(sbb("ks32", [128, 8], F32))
            ksb = T(sbb("ksb", [128, 8], BF16))
            gm = T(sbb("gm", [128, 128], F32))
            top8 = T(sbb("top8", [128, 128], F32))
            lt = T(sbb("lt", [128, 128], F32))
            Bpr = Ring([T(sbb(f"Bpad{i}", [128, 16, 128], F32)) for i in range(2)])
            for t in Bpr.t:
                sc.op("pool", lambda h, t=t: h.memset(t.ap[:, :, :], 0.0), writes=[t])
            BTr = Ring([T(sbb(f"BT{i}", [128, 512], BF16)) for i in range(2)])
            PT = [[T() for _ in range(16)] for _ in range(2)]
            PTbuf = [sbb(f"PT{i}", [128, 16, 512], BF16) for i in range(2)]
            rinvr = Ring([T(sbb(f"rinv{i}", [128, 512], F32)) for i in range(2)])
            OTr = Ring([T(sbb(f"OTst{i}", [128, S], F32R)) for i in range(2)])
            psST = Ring(PS[0:2])
            psO = Ring(PS[2:4])
            psG = PS[4]
            psBT = PS[5]
            psOT = Ring(PS[6:8])
            def prep_load(hd):
                qT, kT, V = qr.next(), kr.next(), vr.next()
                sc.dma("sp", lambda h: h.dma_start(out=qT.ap[:, :], in_=qk_s[hd]), reads=[T_qk[hd]], writes=[qT])
                sc.dma("sp", lambda h: h.dma_start(out=kT.ap[:, :], in_=qk_s[16 + hd]), reads=[T_qk[16 + hd]], writes=[kT])
                sc.dma("sp", lambda h: h.dma_start(
                    out=V.ap[:, :, 0:128],
                    in_=v_s.rearrange("(tt p) c -> p tt c", p=128)[:, :, hd * 128:(hd + 1) * 128]),
                    reads=[T_v[hd // 2]], writes=[V])
                zero_fill(hd)
                return qT, kT, V

            def prep(hd, loaded):
                qT, kT, V = loaded
                Bpad = Bpr.next()
                sc.op("dve", lambda h: h.tensor_reduce(
                    out=ks32.ap[:, :], in_=kT.ap[:, :].rearrange("p (n k) -> p n k", k=256), axis=AX.X, op=OP.add),
                    reads=[kT], writes=[ks32])
                sc.op("dve", lambda h: h.tensor_copy(out=ksb.ap[:, :], in_=ks32.ap[:, :]), reads=[ks32], writes=[ksb])
                for i in range(16):
                    sc.op("pe", lambda h, i=i: h.matmul(
                        psG.ap[:, i * 8:(i + 1) * 8], lhsT=qT.ap[:, i * 128:(i + 1) * 128], rhs=ksb.ap[:, :],
                        start=True, stop=True), reads=[qT, ksb], writes=[psG])
                sc.op("dve", lambda h: h.tensor_tensor(out=gm.ap[:, :], in0=psG.ap[:, 0:128], in1=pastbias, op=OP.add),
                      reads=[psG, T_cf], writes=[gm])
                for i in range(16):
                    sc.op("dve", lambda h, i=i: h.max(out=top8.ap[:, i * 8:(i + 1) * 8], in_=gm.ap[:, i * 8:(i + 1) * 8]),
                          reads=[gm], writes=[top8])
                sc.op("dve", lambda h: h.tensor_tensor(
                    out=lt.ap[:, :].rearrange("p (i n) -> p i n", n=8),
                    in0=gm.ap[:, :].rearrange("p (i n) -> p i n", n=8),
                    in1=top8.ap[:, :].rearrange("p (i n) -> p i n", n=8)[:, :, 2:3].to_broadcast([128, 16, 8]),
                    op=OP.is_lt), reads=[gm, top8], writes=[lt])
                sc.op("dve", lambda h: h.tensor_tensor(
                    out=Bpad.ap[:, :, 0:8],
                    in0=lt.ap[:, :].rearrange("p (i n) -> p i n", n=8),
                    in1=notown.rearrange("p (i n) -> p i n", n=8), op=OP.mult),
                    reads=[lt, T_cf], writes=[Bpad])
                return qT, kT, V, OTr.next(), Bpad

            def stage1(ctx, c, par):
                qT, kT, V, OTst, Bpad = ctx
                for ii in range(4):
                    sc.op("pe", lambda h, ii=ii: h.transpose(
                        out=psBT.ap[:, ii * 128:(ii + 1) * 128], in_=Bpad.ap[:, 4 * c + ii, :], identity=ident32),
                        reads=[Bpad, T_cf], writes=[psBT])
                BT = BTr.next()
                sc.op("act", lambda h: h.copy(out=BT.ap[:, :], in_=psBT.ap[:, :]), reads=[psBT], writes=[BT])
                for j in range(4 * c + 4):
                    q0 = max(512 * c, 128 * j)
                    off = q0 - 512 * c
                    diag = j >= 4 * c
                    st = psST.next()
                    sc.op("pe", lambda h, st=st, j=j, q0=q0, off=off: h.matmul(
                        st.ap[:, off:512], lhsT=kT.ap[:, j * 128:(j + 1) * 128], rhs=qT.ap[:, q0:512 * c + 512],
                        start=True, stop=False), reads=[kT, qT], writes=[st])
                    sc.op("pe", lambda h, st=st, j=j, off=off, diag=diag: h.matmul(
                        st.ap[:, off:512], lhsT=blkind[:, j * 128:(j + 1) * 128], rhs=BT.ap[:, off:512],
                        start=False, stop=(not diag)), reads=[BT, T_cb], writes=[st])
                    if diag:
                        sc.op("pe", lambda h, st=st, off=off: h.matmul(
                            st.ap[:, off:off + 128], lhsT=identb, rhs=tri, start=False, stop=True,
                            skip_group_check=True), reads=[T_cb], writes=[st])
                    sc.op("act", lambda h, st=st, j=j, off=off: h.activation(
                        out=PTbuf[par][:, j, off:512], in_=st.ap[:, off:512], func=AF.Exp, scale=SCALE),
                        reads=[st], writes=[PT[par][j]])

            def stage2(ctx, c, par, hd):
                qT, kT, V, OTst, Bpad = ctx
                otp = psO.next()
                rsp = psOT.next()
                nj = 4 * c + 4
                for j in range(nj):
                    off = max(512 * c, 128 * j) - 512 * c
                    sc.op("pe", lambda h, j=j, off=off: h.matmul(
                        otp.ap[:, off:512], lhsT=V.ap[:, j, 0:128], rhs=PTbuf[par][:, j, off:512],
                        start=(j == 0), stop=(j == nj - 1), skip_group_check=True), reads=[PT[par][j], V], writes=[otp])
                    sc.op("pe", lambda h, j=j, off=off: h.matmul(
                        rsp.ap[:, off:512], lhsT=onesb, rhs=PTbuf[par][:, j, off:512],
                        start=(j == 0), stop=(j == nj - 1), skip_group_check=True), reads=[PT[par][j], T_cb], writes=[rsp])
                rinv = rinvr.next()
                sc.op("dve", lambda h: h.reciprocal(out=rinv.ap[:, :], in_=rsp.ap[:, :]), reads=[rsp], writes=[rinv])
                sc.op("dve", lambda h: h.tensor_tensor(
                    out=OTst.ap[:, c * 512:(c + 1) * 512], in0=otp.ap[:, :], in1=rinv.ap[:, :], op=OP.mult),
                    reads=[otp, rinv], writes=[OTst])
                if c == 3:
                    sc.dma("pool", lambda h: h.dma_start(out=ot_s[hd], in_=OTst.ap[:, :]), reads=[OTst], writes=[T_ot[hd]])

            items = [(hd, c) for hd in range(NH) for c in range(4)]
            ctxs = {}
            prev = None
            ctxs[0] = prep(0, prep_load(0))
            loaded = None
            for k, (hd, c) in enumerate(items):
                stage1(ctxs[hd], c, k % 2)
                if prev is not None:
                    stage2(ctxs[prev[0]], prev[1], (k - 1) % 2, prev[0])
                if c == 0 and hd + 1 < NH:
                    loaded = prep_load(hd + 1)
                if c == 1 and hd + 1 < NH:
                    ctxs[hd + 1] = prep(hd + 1, loaded)
                prev = (hd, c)
            stage2(ctxs[prev[0]], prev[1], (len(items) - 1) % 2, prev[0])
        sc.fence()
    if "C" in phases:
        with ExitStack() as ec:
            def sbc(name, shape, dt):
                return ec.enter_context(nc.sbuf_tensor(f"s{next(_uid)}_" + name, list(shape), dt))
            cvec = sbc("cvec", [128, 16, 36], F32)
            bG = sbc("bG", [128, 64], F32)
            wr = sbc("wr", [128, 16, 36], F32)
            brt = sbc("br", [128, 36], F32)
            T_cc = T()
            sc.dma("sp", lambda h: h.dma_start(out=cvec[:], in_=cvec_d), writes=[T_cc])
            sc.dma("sp", lambda h: h.dma_start(out=bG[:], in_=bG_d), writes=[T_cc])
            sc.dma("sp", lambda h: h.dma_start(out=wr[:], in_=wr_d), writes=[T_cc])
            sc.dma("sp", lambda h: h.dma_start(out=brt[:], in_=br_d.partition_broadcast(128)), writes=[T_cc])
            sc.dma("pool", lambda h: h.dma_start(out=stab_s, in_=sinit_d), writes=[T_stab])
            halo = T(sbc("halo", [128, 16, 32], F32))
            sc.op("pool", lambda h: h.memset(halo.ap[:, :, :], 0.0), writes=[halo])
            Acum = T(sbc("Acum", [128, 32], F32))
            sc.op("pool", lambda h: h.memset(Acum.ap[:, :], 0.0), writes=[Acum])
            mT = sbc("mT", [128, 16, 512], F32)
            T_mT = [T() for _ in range(16)]
            psA = Ring(PS[0:2])
            psB = Ring(PS[2:4])
            psS = [PS[4], PS[5]]
            psX = Ring(PS[6:8])

            def gemm_fm(wring, wsrc, ps, act, T_act):
                W = wring.next()
                sc.dma("sp", lambda h, W=W: h.dma_start(out=W.ap[:], in_=wsrc), writes=[W])
                for kc in range(16):
                    sc.op("pe", lambda h, W=W, kc=kc: h.matmul(
                        ps.ap[:, :], lhsT=r32(W.ap[:, kc, :]), rhs=r32(act[:, kc, :]),
                        start=(kc == 0), stop=(kc == 15)),
                        reads=[W, (T_act[kc * len(T_act) // 16] if isinstance(T_act, list) else T_act)], writes=[ps])

            for c in range(4):
                t0 = c * 512
                e12 = ExitStack()
                def sb12(name, shape, dt):
                    return e12.enter_context(nc.sbuf_tensor(f"s{next(_uid)}_" + name, list(shape), dt))
                xTc = sb12("xTc", [128, 16, 512], F32R)
                T_xTc = [T() for _ in range(4)]
                OTc = sb12("OTc", [128, 16, 512], F32R)
                T_OTc = T()
                wring = Ring([T(sb12(f"wC{i}", [128, 16, 128], F32R)) for i in range(3)])
                sgr = Ring([T(sb12(f"sg{i}", [128, 512], F32)) for i in range(2)])
                tmr = Ring([T(sb12(f"tm{i}", [128, 512], F32)) for i in range(2)])
                cT = sb12("cT", [128, 16, 512], F32)
                T_cT = [T() for _ in range(16)]
                hTr = Ring([T(sb12(f"hT{i}", [128, 544], F32)) for i in range(4)])
                dgr = Ring([T(sb12(f"dg{i}", [128, 16, 128], F32)) for i in range(2)])
                mean = T(sb12("mean", [128, 512], F32))
                rstd = T(sb12("rstd", [128, 512], F32))
                for kq in range(4):
                    sc.dma("sp", lambda h, kq=kq: h.dma_start(
                        out=xTc[:, 4 * kq:4 * kq + 4, :],
                        in_=xT_d.rearrange("(kc p) t -> p kc t", p=128)[:, 4 * kq:4 * kq + 4, t0:t0 + 512]),
                        writes=[T_xTc[kq]])
                sc.dma("sp", lambda h: h.dma_start(
                    out=OTc[:, :, :], in_=ot_s.rearrange("k p t -> p k t")[:, :, t0:t0 + 512]),
                    reads=T_ot, writes=[T_OTc])

                def conv_pe(cc, hT):
                    halves = []
                    for hh in range(2):
                        j0 = 16 * hh
                        n = 16 if hh == 0 else 15
                        dg = dgr.next()
                        sc.op("pool", lambda h, dg=dg, j0=j0, n=n: h.tensor_tensor(
                            out=r32(dg.ap[:, 0:n, :]), in0=ident32.unsqueeze(1).to_broadcast([128, n, 128]),
                            in1=cvec[:, cc, j0:j0 + n].unsqueeze(2).to_broadcast([128, n, 128]), op=OP.mult),
                            reads=[T_cf, T_cc], writes=[dg])
                        halves.append(dg)
                    ps = psX.next()
                    for j in range(31):
                        dg = halves[j // 16]
                        sc.op("pe", lambda h, j=j, dg=dg: h.matmul(
                            ps.ap[:, :], lhsT=r32(dg.ap[:, j % 16, :]), rhs=r32(hT.ap[:, 2 + j:514 + j]),
                            start=(j == 0), stop=(j == 30)), reads=[dg, hT], writes=[ps])
                    sc.op("act", lambda h: h.activation(
                        out=r32(cT[:, cc, :]), in_=ps.ap[:, :], func=AF.Identity, bias=cvec[:, cc, 31:32], scale=1.0),
                        reads=[ps, T_cc], writes=[T_cT[cc]])
                    sc.op("pool", lambda h: h.tensor_copy(out=halo.ap[:, cc, :], in_=hT.ap[:, 512:544]),
                          reads=[hT], writes=[halo])

                pend = None
                for cc in range(16):
                    hT = hTr.next()
                    sc.op("pool", lambda h, cc=cc, hT=hT: h.tensor_copy(out=r32(hT.ap[:, 0:32]), in_=halo.ap[:, cc, :]),
                          reads=[halo], writes=[hT])
                    pa, pg = psA.next(), psB.next()
                    gemm_fm(wring, wG_d[cc], pa, xTc, T_xTc)
                    gemm_fm(wring, wG_d[16 + cc], pg, xTc, T_xTc)
                    sg = sgr.next()
                    sc.op("act", lambda h, sg=sg, pg=pg, cc=cc: h.activation(
                        out=sg.ap[:, :], in_=pg.ap[:, :], func=AF.Sigmoid, bias=bG[:, 16 + cc:17 + cc], scale=1.0),
                        reads=[pg, T_cc], writes=[sg])
                    sc.op("dve", lambda h, sg=sg, pa=pa, cc=cc, hT=hT: h.scalar_tensor_tensor(
                        out=r32(hT.ap[:, 32:544]), in0=pa.ap[:, :], scalar=bG[:, cc:cc + 1], in1=sg.ap[:, :],
                        op0=OP.add, op1=OP.mult), reads=[pa, sg, T_cc, hT], writes=[hT])
                    if pend is not None:
                        conv_pe(*pend)
                    pend = (cc, hT)
                conv_pe(*pend)

                pm, pq = psS
                for cc in range(16):
                    sc.op("pe", lambda h, cc=cc: h.matmul(pm.ap[:, :], lhsT=r32(onesn), rhs=r32(cT[:, cc, :]),
                                                         start=(cc == 0), stop=(cc == 15)),
                          reads=[T_cT[cc], T_cf], writes=[pm])
                for cc in range(16):
                    sq = hTr.next()
                    sc.op("act", lambda h, sq=sq, cc=cc: h.activation(out=r32(sq.ap[:, 0:512]), in_=cT[:, cc, :], func=AF.Square),
                          reads=[T_cT[cc]], writes=[sq])
                    sc.op("pe", lambda h, sq=sq, cc=cc: h.matmul(pq.ap[:, :], lhsT=r32(onesn), rhs=r32(sq.ap[:, 0:512]),
                                                                start=(cc == 0), stop=(cc == 15)),
                          reads=[sq, T_cf], writes=[pq])
                sc.op("dve", lambda h: h.tensor_copy(out=mean.ap[:, :], in_=pm.ap[:, :]), reads=[pm], writes=[mean])
                sc.op("dve", lambda h: h.tensor_tensor(out=rstd.ap[:, :], in0=mean.ap[:, :], in1=mean.ap[:, :], op=OP.mult),
                      reads=[mean], writes=[rstd])
                sc.op("dve", lambda h: h.tensor_tensor(out=rstd.ap[:, :], in0=pq.ap[:, :], in1=rstd.ap[:, :], op=OP.subtract),
                      reads=[pq, rstd], writes=[rstd])
                sc.op("dve", lambda h: h.tensor_scalar(out=rstd.ap[:, :], in0=rstd.ap[:, :], scalar1=LN_EPS, scalar2=None,
                                                       op0=OP.add), reads=[rstd], writes=[rstd])
                sc.op("act", lambda h: h.activation(out=rstd.ap[:, :], in_=rstd.ap[:, :], func=AF.Sqrt),
                      reads=[rstd], writes=[rstd])
                sc.op("dve", lambda h: h.reciprocal(out=rstd.ap[:, :], in_=rstd.ap[:, :]), reads=[rstd], writes=[rstd])

                for j in range(16):
                    cc = j
                    en = "dve" if cc % 2 == 0 else "pool"
                    nt = hTr.next()
                    sc.op(en, lambda h, cc=cc, nt=nt: h.tensor_tensor(out=r32(nt.ap[:, 0:512]), in0=cT[:, cc, :], in1=mean.ap[:, :],
                                                                      op=OP.subtract), reads=[T_cT[cc], mean], writes=[nt])
                    sc.op(en, lambda h, cc=cc, nt=nt: h.tensor_tensor(out=r32(nt.ap[:, 0:512]), in0=nt.ap[:, 0:512], in1=rstd.ap[:, :],
                                                                      op=OP.mult), reads=[nt, rstd], writes=[nt])
                    sc.op("act", lambda h, cc=cc, nt=nt: h.activation(
                        out=r32(cT[:, cc, :]), in_=nt.ap[:, 0:512], func=AF.Silu, bias=cvec[:, cc, 33:34], scale=cvec[:, cc, 32:33]),
                        reads=[nt, T_cc], writes=[T_cT[cc]])
                    py, pg = psA.next(), psB.next()
                    gemm_fm(wring, wo_d[j], py, OTc, T_OTc)
                    gemm_fm(wring, wG_d[32 + j], pg, xTc, T_xTc)
                    sg = sgr.next()
                    sc.op("act", lambda h, sg=sg, pg=pg, j=j: h.activation(
                        out=sg.ap[:, :], in_=pg.ap[:, :], func=AF.Sigmoid, bias=bG[:, 32 + j:33 + j], scale=1.0),
                        reads=[pg, T_cc], writes=[sg])
                    sc.op("dve", lambda h, sg=sg, py=py, j=j: h.tensor_tensor(
                        out=r32(mT[:, j, :]), in0=py.ap[:, :], in1=sg.ap[:, :], op=OP.mult),
                        reads=[py, sg], writes=[T_mT[j]])
                for j in range(16):
                    py, pg = psA.next(), psB.next()
                    gemm_fm(wring, wp_d[j], py, cT, T_cT)
                    gemm_fm(wring, wG_d[48 + j], pg, xTc, T_xTc)
                    sg = sgr.next()
                    sc.op("act", lambda h, sg=sg, pg=pg, j=j: h.activation(
                        out=sg.ap[:, :], in_=pg.ap[:, :], func=AF.Sigmoid, bias=bG[:, 48 + j:49 + j], scale=1.0),
                        reads=[pg, T_cc], writes=[sg])
                    tm = tmr.next()
                    sc.op("dve", lambda h, sg=sg, py=py, j=j, tm=tm: h.scalar_tensor_tensor(
                        out=tm.ap[:, :], in0=py.ap[:, :], scalar=cvec[:, j, 34:35], in1=sg.ap[:, :],
                        op0=OP.add, op1=OP.mult), reads=[py, sg, T_cc], writes=[tm])
                    sc.op("pool", lambda h, tm=tm, j=j: h.tensor_tensor(
                        out=r32(mT[:, j, :]), in0=mT[:, j, :], in1=tm.ap[:, :], op=OP.add),
                        reads=[tm, T_mT[j]], writes=[T_mT[j]])
                e12.close()
                sc.fence()
                with ExitStack() as e3:
                    def sb3(name, shape, dt):
                        return e3.enter_context(nc.sbuf_tensor(f"s{next(_uid)}_" + name, list(shape), dt))
                    wor = Ring([T(sb3(f"wout{i}", [128, 16, 512], F32R)) for i in range(2)])
                    lng = sb3("lng", [128, 2048], F32)
                    lnb = sb3("lnb", [128, 2048], F32)
                    T_ln = T()
                    sc.dma("sp", lambda h: h.dma_start(out=lng[:], in_=lnv_d[0:1, :].partition_broadcast(128)), writes=[T_ln])
                    sc.dma("sp", lambda h: h.dma_start(out=lnb[:], in_=lnv_d[1:2, :].partition_broadcast(128)), writes=[T_ln])
                    xr = [T(sb3(f"xt{i}", [128, 2048], F32)) for i in range(4)]
                    zr = xr
                    for t in range(4):
                        sc.dma("sp", lambda h, t=t, t0=t0: h.dma_start(
                            out=xr[t].ap[:, :], in_=x_d[t0 + t * 128:t0 + (t + 1) * 128, :]), writes=[xr[t]])
                    for cb in range(4):
                        W = wor.next()
                        sc.dma("sp", lambda h, W=W, cb=cb: h.dma_start(out=W.ap[:], in_=wout_d[cb]), writes=[W])
                        for t in range(4):
                            ps = psA.next() if t % 2 == 0 else psB.next()
                            for kc in range(16):
                                sc.op("pe", lambda h, ps=ps, W=W, kc=kc, t=t: h.matmul(
                                    ps.ap[:, :], lhsT=r32(mT[:, kc, t * 128:(t + 1) * 128]), rhs=r32(W.ap[:, kc, :]),
                                    start=(kc == 0), stop=(kc == 15)), reads=[W, T_mT[kc]], writes=[ps])
                            sc.op("dve", lambda h, ps=ps, t=t, cb=cb: h.scalar_tensor_tensor(
                                out=zr[t].ap[:, cb * 512:(cb + 1) * 512], in0=xr[t].ap[:, cb * 512:(cb + 1) * 512],
                                scalar=ALPHA, in1=ps.ap[:, :], op0=OP.mult, op1=OP.add),
                                reads=[ps, xr[t]], writes=[zr[t]])
                    def mk_small(t):
                        d = {}
                        for nm, shp, dt in (("st6", [128, 4, 6], F32), ("mv", [128, 2], F32), ("rs", [128, 2], F32),
                                            ("lg", [128, 36], F32), ("sm", [128, 64], F32), ("oh", [128, 4], F32),
                                            ("esel", [128, 8], F32), ("t8", [128, 8], F32), ("mk", [128, 2, 8], F32),
                                            ("Ak", [128, 2, 32], F32), ("Asum", [128, 32], F32), ("val", [128, 32], F32),
                                            ("ov", [128, 32], F32), ("dst", [128, 2], F32), ("dsti", [128, 2], I32),
                                            ("pay", [128, 2, 4], I32), ("tokf", [128, 2], F32)):
                            d[nm] = T(sb3(f"{nm}{t}", shp, dt))
                        return d
                    small = [mk_small(t) for t in range(4)]
                    hTts = [T(sb3(f"hTt{i}", [128, 16, 128], F32)) for i in range(2)]

                    def tail1(t):
                        g = c * 4 + t
                        z = zr[t]
                        d = small[t]
                        st6, mv, rs, lg, sm, oh, esel, t8, mk, Ak, Asum = (d[k] for k in (
                            "st6", "mv", "rs", "lg", "sm", "oh", "esel", "t8", "mk", "Ak", "Asum"))
                        hTt = hTts[t % 2]
                        for q4 in range(4):
                            sc.op("dve", lambda h, q4=q4: h.bn_stats(out=st6.ap[:, q4, :], in_=z.ap[:, q4 * 512:(q4 + 1) * 512]),
                                  reads=[z], writes=[st6])
                        sc.op("dve", lambda h: h.bn_aggr(out=mv.ap[:, :], in_=st6.ap[:, :, :].rearrange("p a b -> p (a b)")),
                              reads=[st6], writes=[mv])
                        sc.op("dve", lambda h: h.tensor_scalar(out=rs.ap[:, 0:1], in0=mv.ap[:, 1:2], scalar1=LN_EPS, scalar2=None,
                                                               op0=OP.add), reads=[mv], writes=[rs])
                        yield
                        sc.op("act", lambda h: h.activation(out=rs.ap[:, 0:1], in_=rs.ap[:, 0:1], func=AF.Sqrt), reads=[rs], writes=[rs])
                        yield
                        sc.op("dve", lambda h: h.reciprocal(out=rs.ap[:, 0:1], in_=rs.ap[:, 0:1]), reads=[rs], writes=[rs])
                        sc.op("dve", lambda h: h.tensor_scalar(out=rs.ap[:, 1:2], in0=mv.ap[:, 0:1], scalar1=rs.ap[:, 0:1], scalar2=-1.0,
                                                               op0=OP.mult, op1=OP.mult), reads=[mv, rs], writes=[rs])
                        yield
                        sc.op("act", lambda h: h.activation(out=z.ap[:, :], in_=z.ap[:, :], func=AF.Identity,
                                                            scale=rs.ap[:, 0:1], bias=rs.ap[:, 1:2]), reads=[z, rs], writes=[z])
                        yield
                        sc.op("pool", lambda h: h.tensor_tensor(out=z.ap[:, :], in0=z.ap[:, :], in1=lng[:, :], op=OP.mult),
                              reads=[z, T_ln], writes=[z])
                        yield
                        sc.op("dve", lambda h: h.tensor_tensor(out=z.ap[:, :], in0=z.ap[:, :], in1=lnb[:, :], op=OP.add),
                              reads=[z, T_ln], writes=[z])
                        sc.dma("pool", lambda h: h.dma_start(out=h_s[g * 128:(g + 1) * 128, :], in_=z.ap[:, :]),
                               reads=[z], writes=[T_h[g]])
                        yield
                        for k4 in range(4):
                            tp = psX.next()
                            for ii in range(4):
                                kc = k4 * 4 + ii
                                sc.op("pe", lambda h, tp=tp, ii=ii, kc=kc: h.transpose(
                                    out=tp.ap[:, ii * 128:(ii + 1) * 128], in_=z.ap[:, kc * 128:(kc + 1) * 128], identity=ident32),
                                    reads=[z, T_cf], writes=[tp])
                            sc.op("act", lambda h, tp=tp, k4=k4: h.copy(
                                out=hTt.ap[:, k4 * 4:(k4 + 1) * 4, :].rearrange("p a b -> p (a b)"), in_=tp.ap[:, :]),
                                reads=[tp], writes=[hTt])
                        pl = psS[0]
                        for kc in range(16):
                            sc.op("pe", lambda h, kc=kc: h.matmul(pl.ap[:, t * 64:t * 64 + 36], lhsT=hTt.ap[:, kc, :], rhs=wr[:, kc, :],
                                                                  start=(kc == 0), stop=(kc == 15)),
                                  reads=[hTt, T_cc], writes=[pl])
                        yield
                        sc.op("dve", lambda h: h.tensor_tensor(out=lg.ap[:, :], in0=pl.ap[:, t * 64:t * 64 + 36], in1=brt[:, :], op=OP.add),
                              reads=[pl, T_cc], writes=[lg])
                        sc.op("dve", lambda h: h.tensor_reduce(out=sm.ap[:, 0:1], in_=lg.ap[:, 0:4], axis=AX.X, op=OP.max),
                              reads=[lg], writes=[sm])
                        sc.op("dve", lambda h: h.tensor_scalar(out=oh.ap[:, :], in0=lg.ap[:, 0:4], scalar1=sm.ap[:, 0:1], scalar2=None,
                                                               op0=OP.is_equal), reads=[lg, sm], writes=[oh])
                        sc.op("dve", lambda h: h.tensor_scalar(out=sm.ap[:, 1:2], in0=sm.ap[:, 0:1], scalar1=-1.0, scalar2=None,
                                                               op0=OP.mult), reads=[sm], writes=[sm])
                        sc.op("dve", lambda h: h.tensor_scalar(out=esel.ap[:, :], in0=lg.ap[:, 4:12], scalar1=oh.ap[:, 0:1], scalar2=None,
                                                               op0=OP.mult), reads=[lg, oh], writes=[esel])
                        for gi in range(1, 4):
                            sc.op("dve", lambda h, gi=gi: h.scalar_tensor_tensor(
                                out=esel.ap[:, :], in0=lg.ap[:, 4 + 8 * gi:12 + 8 * gi], scalar=oh.ap[:, gi:gi + 1], in1=esel.ap[:, :],
                                op0=OP.mult, op1=OP.add), reads=[lg, oh, esel], writes=[esel])
                        sc.op("dve", lambda h: h.max(out=t8.ap[:, :], in_=esel.ap[:, :]), reads=[esel], writes=[t8])
                        for k in range(2):
                            sc.op("dve", lambda h, k=k: h.tensor_scalar(out=mk.ap[:, k, :], in0=esel.ap[:, :], scalar1=t8.ap[:, k:k + 1],
                                                                        scalar2=None, op0=OP.is_equal), reads=[esel, t8], writes=[mk])
                        sc.op("dve", lambda h: h.tensor_tensor(out=sm.ap[:, 4:5], in0=t8.ap[:, 1:2], in1=t8.ap[:, 0:1], op=OP.subtract),
                              reads=[t8], writes=[sm])
                        yield
                        sc.op("act", lambda h: h.activation(out=sm.ap[:, 10:14], in_=lg.ap[:, 0:4], func=AF.Exp, bias=sm.ap[:, 1:2],
                                                            scale=1.0), reads=[lg, sm], writes=[sm])
                        sc.op("act", lambda h: h.activation(out=sm.ap[:, 5:6], in_=sm.ap[:, 4:5], func=AF.Exp), reads=[sm], writes=[sm])
                        yield
                        sc.op("dve", lambda h: h.tensor_reduce(out=sm.ap[:, 2:3], in_=sm.ap[:, 10:14], axis=AX.X, op=OP.add),
                              reads=[sm], writes=[sm])
                        sc.op("dve", lambda h: h.reciprocal(out=sm.ap[:, 3:4], in_=sm.ap[:, 2:3]), reads=[sm], writes=[sm])
                        sc.op("dve", lambda h: h.tensor_scalar(out=sm.ap[:, 6:7], in0=sm.ap[:, 5:6], scalar1=1.0, scalar2=None, op0=OP.add),
                              reads=[sm], writes=[sm])
                        sc.op("dve", lambda h: h.reciprocal(out=sm.ap[:, 6:7], in_=sm.ap[:, 6:7]), reads=[sm], writes=[sm])
                        sc.op("dve", lambda h: h.tensor_tensor(out=sm.ap[:, 7:8], in0=sm.ap[:, 6:7], in1=sm.ap[:, 3:4], op=OP.mult),
                              reads=[sm], writes=[sm])
                        sc.op("dve", lambda h: h.tensor_tensor(out=sm.ap[:, 8:9], in0=sm.ap[:, 7:8], in1=sm.ap[:, 5:6], op=OP.mult),
                              reads=[sm], writes=[sm])
                        for k in range(2):
                            for gi in range(4):
                                sc.op("dve", lambda h, k=k, gi=gi: h.tensor_scalar(
                                    out=Ak.ap[:, k, gi * 8:(gi + 1) * 8], in0=mk.ap[:, k, :], scalar1=oh.ap[:, gi:gi + 1], scalar2=None,
                                    op0=OP.mult), reads=[mk, oh], writes=[Ak])
                        sc.op("dve", lambda h: h.tensor_tensor(out=Asum.ap[:, :], in0=Ak.ap[:, 0, :], in1=Ak.ap[:, 1, :], op=OP.add),
                              reads=[Ak], writes=[Asum])
                        yield

                    def tail2(t):
                        g = c * 4 + t
                        d = small[t]
                        sm, Ak, Asum, val, ov, dst, dsti, pay, tokf = (d[k] for k in (
                            "sm", "Ak", "Asum", "val", "ov", "dst", "dsti", "pay", "tokf"))
                        pp = psS[1]
                        sc.op("pe", lambda h: h.matmul(pp.ap[:, 0:32], lhsT=U32, rhs=Asum.ap[:, :], start=True, stop=False),
                              reads=[Asum, T_cf], writes=[pp])
                        sc.op("pe", lambda h: h.matmul(pp.ap[:, 0:32], lhsT=ones32, rhs=Acum.ap[:, :], start=False, stop=True),
                              reads=[Acum, T_cf], writes=[pp])
                        sc.op("dve", lambda h: h.tensor_scalar(out=ov.ap[:, :], in0=pp.ap[:, 0:32], scalar1=float(CAP), scalar2=1.0e6,
                                                               op0=OP.is_ge, op1=OP.mult), reads=[pp], writes=[ov])
                        sc.op("dve", lambda h: h.tensor_tensor(out=val.ap[:, :], in0=pp.ap[:, 0:32], in1=slotbase, op=OP.add),
                              reads=[pp, T_cf], writes=[val])
                        sc.op("dve", lambda h: h.tensor_tensor(out=Acum.ap[:, :], in0=Acum.ap[:, :], in1=Asum.ap[:, :], op=OP.add),
                              reads=[Acum, Asum], writes=[Acum])
                        sc.op("dve", lambda h: h.tensor_tensor(out=val.ap[:, :], in0=val.ap[:, :], in1=ov.ap[:, :], op=OP.add),
                              reads=[val, ov], writes=[val])
                        for k in range(2):
                            sc.op("dve", lambda h, k=k: h.tensor_tensor(out=Ak.ap[:, k, :], in0=Ak.ap[:, k, :], in1=val.ap[:, :], op=OP.mult),
                                  reads=[Ak, val], writes=[Ak])
                            sc.op("dve", lambda h, k=k: h.tensor_reduce(out=dst.ap[:, k:k + 1], in_=Ak.ap[:, k, :], axis=AX.X, op=OP.add),
                                  reads=[Ak], writes=[dst])
                        sc.op("dve", lambda h: h.tensor_copy(out=dsti.ap[:, :], in_=dst.ap[:, :]), reads=[dst], writes=[dsti])
                        sc.op("pool", lambda h: h.tensor_scalar(out=tokf.ap[:, 0:1], in0=iota_p, scalar1=float(g * 128), scalar2=None,
                                                                op0=OP.add), reads=[T_cf], writes=[tokf])
                        sc.op("pool", lambda h: h.tensor_scalar(out=tokf.ap[:, 1:2], in0=iota_p, scalar1=float(g * 128 + S), scalar2=None,
                                                                op0=OP.add), reads=[T_cf], writes=[tokf])
                        sc.op("pool", lambda h: h.memset(pay.ap[:, :, :], 0), writes=[pay])
                        sc.op("pool", lambda h: h.tensor_copy(out=pay.ap[:, 0, 0:1], in_=tokf.ap[:, 0:1]), reads=[tokf], writes=[pay])
                        sc.op("pool", lambda h: h.tensor_copy(out=pay.ap[:, 0, 1:2], in_=tokf.ap[:, 0:1]), reads=[tokf], writes=[pay])
                        sc.op("pool", lambda h: h.tensor_copy(out=pay.ap[:, 1, 0:1], in_=tokf.ap[:, 0:1]), reads=[tokf], writes=[pay])
                        sc.op("pool", lambda h: h.tensor_copy(out=pay.ap[:, 1, 1:2], in_=tokf.ap[:, 1:2]), reads=[tokf], writes=[pay])
                        for k in range(2):
                            sc.op("dve", lambda h, k=k: h.tensor_copy(out=pay.ap[:, k, 2:3].bitcast(F32), in_=sm.ap[:, 7 + k:8 + k]),
                                  reads=[sm], writes=[pay])
                        for k in range(2):
                            sc.dma("pool", lambda h, k=k: h.indirect_dma_start(
                                out=stab_s[:, :], out_offset=bass.IndirectOffsetOnAxis(ap=dsti.ap[:, k:k + 1], axis=0),
                                in_=pay.ap[:, k, :], in_offset=None, bounds_check=reg_ns, oob_is_err=False),
                                reads=[pay, dsti], writes=[T_stab])

                    gens = [tail1(t) for t in range(4)]
                    live = list(gens)
                    while live:
                        for gnr in list(live):
                            try:
                                next(gnr)
                            except StopIteration:
                                live.remove(gnr)
                    for t in range(4):
                        tail2(t)

                sc.fence()
        sc.fence()
    if "D" in phases:
        with ExitStack() as ed:
            def sbd(name, shape, dt):
                return ed.enter_context(nc.sbuf_tensor(f"s{next(_uid)}_" + name, list(shape), dt))
            str_ = Ring([T(sbd(f"stb{i}", [128, 2, 4], I32)) for i in range(3)])
            hgr = Ring([T(sbd(f"hg{i}", [128, 2048], F32)) for i in range(2)])
            hgTr = Ring([T(sbd(f"hgT{i}", [128, 16, 256], F32R)) for i in range(2)])
            wer = Ring([T(sbd(f"web{i}", [128, 4096], F32R)) for i in range(7)])
            sgr = Ring([T(sbd(f"sgd{i}", [128, 256], F32)) for i in range(2)])
            actT = sbd("actT", [128, 8, 256], F32R)
            T_act = [T() for _ in range(8)]
            your = Ring([T(sbd(f"yout{i}", [128, 2048], F32)) for i in range(2)])
            psT = Ring(PS[0:2])
            psa = Ring(PS[2:4])
            psb = Ring(PS[4:6])
            psy = Ring(PS[6:8])

            def gather(e):
                stb = str_.next()
                sc.dma("pool", lambda h: h.dma_start(
                    out=stb.ap[:, :, :], in_=stab_s[e * CAP:(e + 1) * CAP, :].rearrange("(s p) c -> p s c", p=128)),
                    reads=[T_stab], writes=[stb])
                hgs = []
                for s in range(2):
                    hg = hgr.next()
                    sc.dma("pool", lambda h, hg=hg, s=s: h.indirect_dma_start(
                        out=hg.ap[:, :], out_offset=None, in_=h_s[:, :],
                        in_offset=bass.IndirectOffsetOnAxis(ap=stb.ap[:, s, 0:1], axis=0)),
                        reads=[stb] + T_h, writes=[hg])
                    hgs.append(hg)
                return stb, hgs

            def transposes(hgs):
                hgT = hgTr.next()
                for s in range(2):
                    hg = hgs[s]
                    for k4 in range(4):
                        tp = psT.next()
                        for ii in range(4):
                            kc = k4 * 4 + ii
                            sc.op("pe", lambda h, tp=tp, ii=ii, kc=kc, hg=hg: h.transpose(
                                out=tp.ap[:, ii * 128:(ii + 1) * 128], in_=hg.ap[:, kc * 128:(kc + 1) * 128], identity=ident32),
                                reads=[hg, T_cf], writes=[tp])
                        if k4 % 2 == 0:
                            sc.op("dve", lambda h, tp=tp, k4=k4, s=s: h.tensor_copy(
                                out=hgT.ap[:, k4 * 4:(k4 + 1) * 4, s * 128:(s + 1) * 128],
                                in_=tp.ap[:, :].rearrange("p (a b) -> p a b", b=128)), reads=[tp], writes=[hgT])
                        else:
                            sc.op("act", lambda h, tp=tp, k4=k4, s=s: h.copy(
                                out=hgT.ap[:, k4 * 4:(k4 + 1) * 4, s * 128:(s + 1) * 128],
                                in_=tp.ap[:, :].rearrange("p (a b) -> p a b", b=128)), reads=[tp], writes=[hgT])
                return hgT

            nxt = gather(0)
            hgT_next = transposes(nxt[1])
            for e in range(NE):
                stb, hgs = nxt
                hgT = hgT_next
                if e + 1 < NE:
                    nxt = gather(e + 1)
                for fb in range(4):
                    W1, W3 = wer.next(), wer.next()
                    sc.dma("sp", lambda h, W1=W1, fb=fb: h.dma_start(
                        out=W1.ap[:, :], in_=w1_d[e, fb].rearrange("p k c -> p (k c)")), writes=[W1])
                    sc.dma("sp", lambda h, W3=W3, fb=fb: h.dma_start(
                        out=W3.ap[:, :], in_=w3_d[e, fb].rearrange("p k c -> p (k c)")), writes=[W3])
                    for half in range(2):
                        fc = fb * 2 + half
                        pa, pb = psa.next(), psb.next()
                        for kc in range(16):
                            sc.op("pe", lambda h, pa=pa, W1=W1, kc=kc, half=half: h.matmul(
                                pa.ap[:, 0:256], lhsT=r32(W1.ap[:, kc * 256 + half * 128:kc * 256 + (half + 1) * 128]), rhs=r32(hgT.ap[:, kc, :]),
                                start=(kc == 0), stop=(kc == 15)), reads=[W1, hgT], writes=[pa])
                        for kc in range(16):
                            sc.op("pe", lambda h, pb=pb, W3=W3, kc=kc, half=half: h.matmul(
                                pb.ap[:, 0:256], lhsT=r32(W3.ap[:, kc * 256 + half * 128:kc * 256 + (half + 1) * 128]), rhs=r32(hgT.ap[:, kc, :]),
                                start=(kc == 0), stop=(kc == 15)), reads=[W3, hgT], writes=[pb])
                        sg = sgr.next()
                        sc.op("act", lambda h, sg=sg, pa=pa: h.activation(out=sg.ap[:, :], in_=pa.ap[:, 0:256], func=AF.Silu),
                              reads=[pa], writes=[sg])
                        sc.op("dve", lambda h, sg=sg, pb=pb, fc=fc: h.tensor_tensor(
                            out=actT[:, fc, :], in0=pb.ap[:, 0:256], in1=sg.ap[:, :], op=OP.mult),
                            reads=[pb, sg], writes=[T_act[fc]])
                if e + 1 < NE:
                    hgT_next = transposes(nxt[1])
                youts = [your.next(), your.next()]
                for cb in range(4):
                    W2 = wer.next()
                    sc.dma("sp", lambda h, W2=W2, cb=cb: h.dma_start(
                        out=W2.ap[:, :], in_=w2_d[e, cb].rearrange("p k c -> p (k c)")), writes=[W2])
                    for s in range(2):
                        py = psy.next()
                        for fc in range(8):
                            sc.op("pe", lambda h, py=py, W2=W2, fc=fc, s=s: h.matmul(
                                py.ap[:, :], lhsT=r32(actT[:, fc, s * 128:(s + 1) * 128]), rhs=r32(W2.ap[:, fc * 512:(fc + 1) * 512]),
                                start=(fc == 0), stop=(fc == 7)), reads=[W2, T_act[fc]], writes=[py])
                        yo = youts[s]
                        sc.op("act", lambda h, py=py, yo=yo, cb=cb, s=s: h.activation(
                            out=yo.ap[:, cb * 512:(cb + 1) * 512], in_=py.ap[:, :], func=AF.Copy,
                            scale=stb.ap[:, s, 2:3].bitcast(F32)), reads=[py, stb], writes=[yo])
                for s in range(2):
                    yo = youts[s]
                    sc.dma("pool", lambda h, yo=yo, s=s: h.indirect_dma_start(
                        out=y_s[:, :], out_offset=bass.IndirectOffsetOnAxis(ap=stb.ap[:, s, 1:2], axis=0),
                        in_=yo.ap[:, :], in_offset=None, bounds_check=reg_y, oob_is_err=False),
                        reads=[yo, stb], writes=[T_y])
                if e % 8 == 7 and e != NE - 1:
                    sc.fence()
        sc.fence()
    if "E" in phases:
        with ExitStack() as ee:
            def sbe(name, shape, dt):
                return ee.enter_context(nc.sbuf_tensor(f"s{next(_uid)}_" + name, list(shape), dt))
            lng = sbe("lng2", [128, 2048], F32)
            lnb = sbe("lnb2", [128, 2048], F32)
            T_ln = T()
            sc.dma("sp", lambda h: h.dma_start(out=lng[:], in_=lnv_d[2:3, :].partition_broadcast(128)), writes=[T_ln])
            sc.dma("sp", lambda h: h.dma_start(out=lnb[:], in_=lnv_d[3:4, :].partition_broadcast(128)), writes=[T_ln])
            hr = Ring([T(sbe(f"he{i}", [128, 2048], F32)) for i in range(4)])
            y0r = Ring([T(sbe(f"y0e{i}", [128, 2048], F32)) for i in range(3)])
            y1r = Ring([T(sbe(f"y1e{i}", [128, 2048], F32)) for i in range(3)])
            smr = Ring([(T(sbe(f"st6e{i}", [128, 4, 6], F32)), T(sbe(f"mve{i}", [128, 2], F32)), T(sbe(f"rse{i}", [128, 2], F32)))
                        for i in range(4)])
            T_out = T()

            def etile(g):
                ht, y0, y1 = hr.next(), y0r.next(), y1r.next()
                st6, mv, rs = smr.next()
                sc.dma("sp", lambda h: h.dma_start(out=ht.ap[:, :], in_=h_s[g * 128:(g + 1) * 128, :]), reads=[T_h[g]], writes=[ht])
                sc.dma("sp", lambda h: h.dma_start(out=y0.ap[:, :], in_=y_s[g * 128:(g + 1) * 128, :]), reads=[T_y], writes=[y0])
                sc.dma("sp", lambda h: h.dma_start(out=y1.ap[:, :], in_=y_s[S + g * 128:S + (g + 1) * 128, :]), reads=[T_y], writes=[y1])
                yield
                sc.op("pool", lambda h: h.tensor_tensor(out=y0.ap[:, :], in0=y0.ap[:, :], in1=y1.ap[:, :], op=OP.add),
                      reads=[y0, y1], writes=[y0])
                yield
                sc.op("dve", lambda h: h.scalar_tensor_tensor(
                    out=ht.ap[:, :], in0=ht.ap[:, :], scalar=ALPHA, in1=y0.ap[:, :], op0=OP.mult, op1=OP.add),
                    reads=[ht, y0], writes=[ht])
                for q4 in range(4):
                    sc.op("dve", lambda h, q4=q4: h.bn_stats(out=st6.ap[:, q4, :], in_=ht.ap[:, q4 * 512:(q4 + 1) * 512]),
                          reads=[ht], writes=[st6])
                sc.op("dve", lambda h: h.bn_aggr(out=mv.ap[:, :], in_=st6.ap[:, :, :].rearrange("p a b -> p (a b)")),
                      reads=[st6], writes=[mv])
                sc.op("dve", lambda h: h.tensor_scalar(out=rs.ap[:, 0:1], in0=mv.ap[:, 1:2], scalar1=LN_EPS, scalar2=None, op0=OP.add),
                      reads=[mv], writes=[rs])
                yield
                sc.op("act", lambda h: h.activation(out=rs.ap[:, 0:1], in_=rs.ap[:, 0:1], func=AF.Sqrt), reads=[rs], writes=[rs])
                yield
                sc.op("dve", lambda h: h.reciprocal(out=rs.ap[:, 0:1], in_=rs.ap[:, 0:1]), reads=[rs], writes=[rs])
                sc.op("dve", lambda h: h.tensor_scalar(out=rs.ap[:, 1:2], in0=mv.ap[:, 0:1], scalar1=rs.ap[:, 0:1], scalar2=-1.0,
                                                       op0=OP.mult, op1=OP.mult), reads=[mv, rs], writes=[rs])
                yield
                sc.op("act", lambda h: h.activation(out=ht.ap[:, :], in_=ht.ap[:, :], func=AF.Identity,
                                                    scale=rs.ap[:, 0:1], bias=rs.ap[:, 1:2]), reads=[ht, rs], writes=[ht])
                yield
                sc.op("pool", lambda h: h.tensor_tensor(out=ht.ap[:, :], in0=ht.ap[:, :], in1=lng[:, :], op=OP.mult),
                      reads=[ht, T_ln], writes=[ht])
                yield
                sc.op("dve", lambda h: h.tensor_tensor(out=ht.ap[:, :], in0=ht.ap[:, :], in1=lnb[:, :], op=OP.add),
                      reads=[ht, T_ln], writes=[ht])
                sc.dma("pool", lambda h: h.dma_start(out=out_d[g * 128:(g + 1) * 128, :], in_=ht.ap[:, :]),
                       reads=[ht], writes=[T()])
                yield

            pending = list(range(16))
            live = []
            while pending or live:
                if pending and len(live) < 3:
                    live.append(etile(pending.pop(0)))
                for gnr in list(live):
                    try:
                        next(gnr)
                    except StopIteration:
                        live.remove(gnr)
        sc.fence()
        sc.fence()
    sc.finish()
    es.close()
    return nc


def _st_blocks(w, cols=128):
    K, N = w.shape
    return np.ascontiguousarray(w.reshape(K // 128, 128, N // cols, cols).transpose(2, 1, 0, 3))


def _consts():
    half = 64
    inv_freq = np.power(np.float32(10000.0), -np.arange(half, dtype=np.float32) * np.float32(2.0 / 128)).astype(np.float32)
    ang = np.arange(S, dtype=np.float32)[:, None] * inv_freq[None, :]
    cos = np.cos(ang).astype(np.float32).T
    sin = np.sin(ang).astype(np.float32).T
    rope = np.concatenate([np.concatenate([cos, cos], 0), np.concatenate([sin, sin], 0)], axis=1)
    cf = np.zeros((128, 1024), np.float32)
    cf[:, 0:128] = np.eye(128, dtype=np.float32)
    p = np.arange(128)
    for i in range(16):
        blk = i // 2
        for n in range(8):
            cf[:, 128 + i * 8 + n] = 0.0 if n < blk else -1.0e30
            cf[:, 256 + i * 8 + n] = 0.0 if n == blk else NEG
    cf[:, 384:512] = 1.0 / 2048.0
    cf[:, 512:640] = (p[:, None] < p[None, :]).astype(np.float32)
    cf[:, 640:768] = 1.0
    cf[:, 768:800] = (np.arange(32) * CAP).astype(np.float32)[None, :]
    cf[:, 800] = p.astype(np.float32)
    cb = np.zeros((128, 2688), np.float32)
    for d in range(64):
        cb[d + 64, d] = -1.0
        cb[d, 64 + d] = 1.0
    cb[:, 128:256] = np.eye(128)
    cb[:, 256:384] = np.where(p[:, None] <= p[None, :], 0.0, NEG)
    cb[:, 384:512] = 1.0
    k = np.arange(S)
    for n in range(8):
        cb[n, 640:2688] = (k // 256 == n).astype(np.float32)
    sinit = np.zeros((NS, 4), np.int32)
    sinit[:, 1] = OOBROW
    return rope, cf, cb.astype(ml_dtypes.bfloat16), sinit


def _prep(inp):
    f = lambda a: np.asarray(a, dtype=np.float32)
    w_in = f(inp["w_in"])[0]
    b_in = f(inp["b_in"])[0]
    sh = {}
    sh["wA"] = _st_blocks(w_in[:, 0:4096])
    sh["wV"] = _st_blocks(w_in[:, 4096:6144], 256)
    sh["wG"] = _st_blocks(w_in[:, 6144:14336])
    sh["bq"] = np.ascontiguousarray(b_in[0:4096].reshape(32, 128).T)
    sh["bv"] = np.ascontiguousarray(b_in[4096:6144].reshape(1, 2048))
    sh["bG"] = np.ascontiguousarray(b_in[6144:14336].reshape(64, 128).T)
    sh["wo"] = _st_blocks(f(inp["w_o_attn"])[0])
    sh["wp"] = _st_blocks(f(inp["w_pw2"])[0])
    sh["wout"] = _st_blocks(f(inp["w_out"])[0], 512)
    cvec = np.zeros((128, 16, 36), np.float32)
    cvec[:, :, 0:31] = f(inp["w_dw"])[0].T.reshape(16, 128, 31).transpose(1, 0, 2)
    for i, nm in enumerate(["b_dw", "conv_ln_g", "conv_ln_b", "b_pw2"]):
        cvec[:, :, 31 + i] = f(inp[nm])[0].reshape(16, 128).T
    sh["cvec"] = cvec
    sh["lnv"] = np.ascontiguousarray(np.stack([f(inp["ln1_g"])[0], f(inp["ln1_b"])[0], f(inp["ln2_g"])[0], f(inp["ln2_b"])[0]]))
    wr = np.concatenate([f(inp["w_rg"])[0]] + [f(inp["w_re"])[0, g] for g in range(4)], axis=1)
    sh["wr"] = np.ascontiguousarray(wr.reshape(16, 128, 36).transpose(1, 0, 2))
    sh["br"] = np.concatenate([f(inp["b_rg"])[0], f(inp["b_re"])[0].reshape(-1)]).reshape(1, 36).astype(np.float32)
    w1 = f(inp["w1"])[0]
    w3 = f(inp["w3"])[0]
    w2 = f(inp["w2"])[0]
    sh["w1"] = np.ascontiguousarray(w1.reshape(NE, 16, 128, 4, 256).transpose(0, 3, 2, 1, 4))
    sh["w3"] = np.ascontiguousarray(w3.reshape(NE, 16, 128, 4, 256).transpose(0, 3, 2, 1, 4))
    sh["w2"] = np.ascontiguousarray(w2.reshape(NE, 8, 128, 4, 512).transpose(0, 3, 2, 1, 4))
    rope, cf, cb, sinit = _consts()
    sh["rope"], sh["cf"], sh["cb"], sh["sinit"] = rope, cf, cb, sinit
    sh["onesn"] = np.full((128, 128), 1.0 / 2048.0, np.float32)
    return sh


def kernel(**inputs):
    x = np.asarray(inputs["x"], dtype=np.float32)
    shared = _prep(inputs)
    nc = build("ABCDE", dbg=False)
    in_maps = []
    for b in range(8):
        m = dict(shared)
        m["x"] = np.ascontiguousarray(x[b])
        m["xT"] = np.ascontiguousarray(x[b].T)
        in_maps.append(m)
    res = run_bass_kernel_spmd(nc, in_maps, core_ids=list(range(8)))
    return np.stack([np.asarray(r["out"], dtype=np.float32) for r in res.results], axis=0)
```

```python
import numpy as np
import ml_dtypes
import concourse.bass as bass
import concourse.mybir as mybir
from concourse.bass_utils import run_bass_kernel_spmd

F32 = mybir.dt.float32
F32R = mybir.dt.float32r
BF16 = mybir.dt.bfloat16
I32 = mybir.dt.int32
AF = mybir.ActivationFunctionType
OP = mybir.AluOpType
AX = mybir.AxisListType

S = 2048
D = 2048
NH = 16
NE = 32
FF = 1024
CAP = 256
NS = NE * CAP
LN_EPS = 1e-5
ALPHA = 2.0 ** 0.25
SCALE = 128.0 ** -0.5
NEG = -30000.0
OOBROW = 2 * S + 100


class T:
    __slots__ = ("ap", "w", "r")

    def __init__(self, ap=None):
        self.ap = ap
        self.w = {}
        self.r = {}


class Eng:
    def __init__(self, name, strict_self):
        self.name = name
        self.key = name
        self.insts = []
        self.count = 0
        self.seen = {}
        self.strict = strict_self
        self.ring_vals = None
        self.ring_pos = 0


class Sched:
    def __init__(self, nc, es, nring_sp=12, nring_pool=8):
        self.nc = nc
        self.es = es
        self.nrot = 0
        self.e = {
            "pe": Eng("pe", False),
            "act": Eng("act", True),
            "dve": Eng("dve", True),
            "pool": Eng("pool", True),
            "sp": Eng("sp", False),
        }
        self.h = {"pe": nc.tensor, "act": nc.scalar, "dve": nc.vector, "pool": nc.gpsimd, "sp": nc.sync}
        self.e["sp"].ring_vals = [0] * nring_sp
        self.e["pool"].ring_vals = [0] * nring_pool
        self.sems = {}
        for en in ("pe", "act", "dve", "pool", "sp"):
            self.sems[en] = es.enter_context(nc.semaphore(f"s_{en}"))
        for qn in ("sp", "pool"):
            for idx in range(len(self.e[qn].ring_vals)):
                self.sems[(qn, idx)] = es.enter_context(nc.semaphore(f"r_{qn}{idx}"))

    def _waits(self, eng, reads, writes, extra=()):
        deps = {}

        def add(k, v):
            if deps.get(k, 0) < v:
                deps[k] = v

        for t in reads:
            for k, v in t.w.items():
                add(k, v)
        for t in writes:
            for k, v in t.w.items():
                add(k, v)
            for k, v in t.r.items():
                add(k, v)
        for k, v in extra:
            add(k, v)
        for k, v in deps.items():
            if k == eng.key and not eng.strict:
                continue
            if eng.seen.get(k, 0) < v:
                eng.seen[k] = v
                self.h[eng.name].wait_ge(self.sems[k], v)

    def op(self, en, fn, reads=(), writes=()):
        eng = self.e[en]
        self._waits(eng, reads, writes)
        eng.count += 1
        v = eng.count
        fn(self.h[en]).then_inc(self.sems[eng.key], 1)
        for t in writes:
            t.w = {eng.key: v}
            t.r = {}
        for t in reads:
            t.r[eng.key] = v

    def dma(self, qn, fn, reads=(), writes=()):
        eng = self.e[qn]
        idx = eng.ring_pos % len(eng.ring_vals)
        eng.ring_pos += 1
        key = (qn, idx)
        prev = eng.ring_vals[idx]
        self._waits(eng, reads, writes, extra=((key, prev),) if prev else ())
        val = prev + 16
        eng.ring_vals[idx] = val
        fn(self.h[qn]).then_inc(self.sems[key], 16)
        for t in writes:
            t.w = {key: val}
            t.r = {}
        for t in reads:
            t.r[key] = val

    def fence(self):
        for en, eng in self.e.items():
            for fn_, f in self.e.items():
                if fn_ != en and f.count and eng.seen.get(f.key, 0) < f.count:
                    eng.seen[f.key] = f.count
                    self.h[en].wait_ge(self.sems[f.key], f.count)
            for qn in ("sp", "pool"):
                for idx, v in enumerate(self.e[qn].ring_vals):
                    if v and eng.seen.get((qn, idx), 0) < v:
                        eng.seen[(qn, idx)] = v
                        self.h[en].wait_ge(self.sems[(qn, idx)], v)
        for en, eng in self.e.items():
            if eng.count > 1500:
                eng.seen[eng.key] = eng.count
                self.nrot += 1
                eng.key = (en, "ep", self.nrot)
                self.sems[eng.key] = self.es.enter_context(self.nc.semaphore(f"s_{en}_{self.nrot}"))
                eng.count = 0

    def finish(self):
        for qn in ("sp", "pool"):
            eng = self.e[qn]
            for idx, v in enumerate(eng.ring_vals):
                if v and eng.seen.get((qn, idx), 0) < v:
                    self.h[qn].wait_ge(self.sems[(qn, idx)], v)


import itertools
_uid = itertools.count()


class Ring:
    def __init__(self, tiles):
        self.t = tiles
        self.i = 0

    def next(self):
        t = self.t[self.i % len(self.t)]
        self.i += 1
        return t


def r32(ap):
    return ap.bitcast(F32R)


def build(phases="ABCDE", dbg=False):
    nc = bass.Bass("TRN2", target_bir_lowering=False)
    nc.dge_precook = False
    from contextlib import ExitStack
    es = ExitStack()
    sc = Sched(nc, es)
    reg_ns = nc.gpsimd.to_reg(NS - 1)
    reg_y = nc.gpsimd.to_reg(2 * S - 1)

    def din(name, shape, dt):
        return nc.dram_tensor(name, list(shape), dt, kind="ExternalInput").ap()

    def dscr(name, shape, dt):
        kind = "ExternalOutput" if dbg else "Internal"
        return nc.dram_tensor(name, list(shape), dt, kind=kind).ap()

    xT_d = din("xT", [D, S], F32R)
    x_d = din("x", [S, D], F32)
    wA_d = din("wA", [32, 128, 16, 128], F32R)
    wV_d = din("wV", [8, 128, 16, 256], F32R)
    wG_d = din("wG", [64, 128, 16, 128], F32R)
    bq_d = din("bq", [128, 32], F32)
    bv_d = din("bv", [1, 2048], F32)
    bG_d = din("bG", [128, 64], F32)
    wo_d = din("wo", [16, 128, 16, 128], F32R)
    wp_d = din("wp", [16, 128, 16, 128], F32R)
    wout_d = din("wout", [4, 128, 16, 512], F32R)
    cvec_d = din("cvec", [128, 16, 36], F32)
    lnv_d = din("lnv", [4, 2048], F32)
    wr_d = din("wr", [128, 16, 36], F32)
    br_d = din("br", [1, 36], F32)
    ned = NE if "D" in phases else 1
    w1_d = din("w1", [ned, 4, 128, 16, 256], F32R)
    w3_d = din("w3", [ned, 4, 128, 16, 256], F32R)
    w2_d = din("w2", [ned, 4, 128, 8, 512], F32R)
    rope_d = din("rope", [128, 4096], F32)
    cf_d = din("cf", [128, 1024], F32)
    on_d = din("onesn", [128, 128], F32R)
    cb_d = din("cb", [128, 2688], BF16)
    sinit_d = din("sinit", [NS, 4], I32)
    out_d = nc.dram_tensor("out", [S, D], F32, kind="ExternalOutput").ap()

    qk_s = dscr("qk_s", [32, 128, S], BF16)
    v_s = dscr("v_s", [S, D], BF16)
    ot_s = dscr("ot_s", [16, 128, S], F32R)
    h_s = dscr("h_s", [S, D], F32)
    y_s = dscr("y_s", [2 * S, D], F32)
    stab_s = dscr("stab_s", [NS, 4], I32)
    T_qk = [T() for _ in range(32)]
    T_v = [T() for _ in range(8)]
    T_ot = [T() for _ in range(16)]
    T_h = [T() for _ in range(16)]
    T_y = T()
    T_stab = T()

    def sb(name, shape, dt):
        return es.enter_context(nc.sbuf_tensor(f"s{next(_uid)}_" + name, list(shape), dt))

    psum = [es.enter_context(nc.psum_tensor(f"ps{i}", [128, 512], F32)) for i in range(8)]
    PS = [T(p) for p in psum]

    cf = sb("cf", [128, 1024], F32)
    cbt = sb("cb", [128, 2688], BF16)
    T_cf, T_cb = T(cf), T(cbt)
    sc.dma("sp", lambda h: h.dma_start(out=cf[:], in_=cf_d), writes=[T_cf])
    sc.dma("sp", lambda h: h.dma_start(out=cbt[:], in_=cb_d), writes=[T_cb])
    onesn_t = sb("onesn", [128, 128], F32R)
    sc.dma("sp", lambda h: h.dma_start(out=onesn_t[:], in_=on_d), writes=[T_cf])
    ident32 = cf[:, 0:128]
    pastbias = cf[:, 128:256]
    notown = cf[:, 256:384]
    onesn = onesn_t[:, :]
    U32 = cf[:, 512:640]
    ones32 = cf[:, 640:768]
    slotbase = cf[:, 768:800]
    iota_p = cf[:, 800:801]
    rotT = cbt[:, 0:128]
    identb = cbt[:, 128:256]
    tri = cbt[:, 256:384]
    onesb = cbt[:, 384:512]
    blkind = cbt[:, 640:2688]

    if "A" in phases:
        with ExitStack() as ea:
            def sba(name, shape, dt):
                return ea.enter_context(nc.sbuf_tensor(f"s{next(_uid)}_" + name, list(shape), dt))
            xT = sba("xT", [128, 16, S], F32R)
            T_xT = [T() for _ in range(4)]
            for tc in range(4):
                sc.dma("sp", lambda h, tc=tc: h.dma_start(
                    out=xT[:, :, tc * 512:(tc + 1) * 512],
                    in_=xT_d.rearrange("(kc p) t -> p kc t", p=128)[:, :, tc * 512:(tc + 1) * 512]),
                    writes=[T_xT[tc]])
            with ExitStack() as e1:
                def sb1(name, shape, dt):
                    return e1.enter_context(nc.sbuf_tensor(f"s{next(_uid)}_" + name, list(shape), dt))
                rope = sb1("rope", [128, 4096], F32)
                T_rope = T()
                sc.dma("sp", lambda h: h.dma_start(out=rope[:], in_=rope_d), writes=[T_rope])
                bq = sb1("bq", [128, 32], F32)
                T_bq = T()
                sc.dma("sp", lambda h: h.dma_start(out=bq[:], in_=bq_d), writes=[T_bq])
                wring = Ring([T(sb1(f"wA{i}", [128, 16, 128], F32R)) for i in range(4)])
                qbr = Ring([T(sb1(f"qb{i}", [128, 512], BF16)) for i in range(3)])
                t1r = Ring([T(sb1(f"t1{i}", [128, 512], F32)) for i in range(2)])
                t2r = Ring([T(sb1(f"t2{i}", [128, 512], F32)) for i in range(2)])
                str_ = Ring([T(sb1(f"st{i}", [128, S], BF16)) for i in range(2)])
                psA = Ring(PS[0:3])
                psR = Ring(PS[3:5])
                def rope_tail(nb, tc, stg, qb, last):
                    ps2 = psR.next()
                    sc.op("pe", lambda h: h.matmul(ps2.ap[:, :], lhsT=rotT, rhs=qb.ap[:, :], start=True, stop=True),
                          reads=[qb, T_cb], writes=[ps2])
                    t1 = t1r.next()
                    t2 = t2r.next()
                    sc.op("pool", lambda h: h.tensor_tensor(
                        out=t1.ap[:, :], in0=qb.ap[:, :], in1=rope[:, tc * 512:(tc + 1) * 512], op=OP.mult),
                        reads=[qb, T_rope], writes=[t1])
                    sc.op("dve", lambda h: h.tensor_tensor(
                        out=t2.ap[:, :], in0=ps2.ap[:, :], in1=rope[:, 2048 + tc * 512:2048 + (tc + 1) * 512],
                        op=OP.mult), reads=[ps2, T_rope], writes=[t2])
                    sc.op("dve", lambda h: h.tensor_tensor(
                        out=stg.ap[:, tc * 512:(tc + 1) * 512], in0=t1.ap[:, :], in1=t2.ap[:, :], op=OP.add),
                        reads=[t1, t2], writes=[stg])
                    if last:
                        sc.dma("pool", lambda h: h.dma_start(out=qk_s[nb], in_=stg.ap[:, :]), reads=[stg], writes=[T_qk[nb]])

                pend = None
                for nb in range(32):
                    W = wring.next()
                    sc.dma("sp", lambda h, W=W, nb=nb: h.dma_start(out=W.ap[:], in_=wA_d[nb]), writes=[W])
                    stg = str_.next()
                    for tc in range(4):
                        ps = psA.next()
                        for kc in range(16):
                            sc.op("pe", lambda h, ps=ps, W=W, kc=kc, tc=tc: h.matmul(
                                ps.ap[:, :], lhsT=r32(W.ap[:, kc, :]), rhs=r32(xT[:, kc, tc * 512:(tc + 1) * 512]),
                                start=(kc == 0), stop=(kc == 15)),
                                reads=[W, T_xT[tc]], writes=[ps])
                        qb = qbr.next()
                        sc.op("act", lambda h, qb=qb, ps=ps, nb=nb: h.activation(
                            out=qb.ap[:, :], in_=ps.ap[:, :], func=AF.Identity, bias=bq[:, nb:nb + 1], scale=1.0),
                            reads=[ps, T_bq], writes=[qb])
                        if pend is not None:
                            rope_tail(*pend)
                        pend = (nb, tc, stg, qb, tc == 3)
                rope_tail(*pend)
            sc.fence()
            with ExitStack() as e2:
                def sb2(name, shape, dt):
                    return e2.enter_context(nc.sbuf_tensor(f"s{next(_uid)}_" + name, list(shape), dt))
                bv = sb2("bv", [128, 2048], F32)
                T_bv = T()
                sc.dma("sp", lambda h: h.dma_start(out=bv[:], in_=bv_d.partition_broadcast(128)), writes=[T_bv])
                wvr = Ring([T(sb2(f"wV{i}", [128, 16, 256], F32R)) for i in range(2)])
                vst = Ring([T(sb2(f"vst{i}", [128, 16, 256], BF16)) for i in range(2)])
                psA = Ring(PS[0:4])
                for vb in range(8):
                    W = wvr.next()
                    sc.dma("sp", lambda h, W=W, vb=vb: h.dma_start(out=W.ap[:], in_=wV_d[vb]), writes=[W])
                    stg = vst.next()
                    for tt in range(16):
                        ps = psA.next()
                        for kc in range(16):
                            sc.op("pe", lambda h, ps=ps, W=W, kc=kc, tt=tt: h.matmul(
                                ps.ap[:, 0:256], lhsT=r32(xT[:, kc, tt * 128:(tt + 1) * 128]), rhs=r32(W.ap[:, kc, :]),
                                start=(kc == 0), stop=(kc == 15)),
                                reads=[W, T_xT[tt // 4]], writes=[ps])
                        sc.op("dve", lambda h, stg=stg, ps=ps, tt=tt, vb=vb: h.tensor_tensor(
                            out=stg.ap[:, tt, :], in0=ps.ap[:, 0:256], in1=bv[:, vb * 256:(vb + 1) * 256], op=OP.add),
                            reads=[ps, T_bv], writes=[stg])
                    sc.dma("pool", lambda h, stg=stg, vb=vb: h.dma_start(
                        out=v_s.rearrange("(tt p) c -> p tt c", p=128)[:, :, vb * 256:(vb + 1) * 256],
                        in_=stg.ap[:, :, :]), reads=[stg], writes=[T_v[vb]])

            sc.fence()
        sc.fence()
    if "B" in phases:
        with ExitStack() as eb:
            def sbb(name, shape, dt):
                return eb.enter_context(nc.sbuf_tensor(f"s{next(_uid)}_" + name, list(shape), dt))
            zt = T(sbb("zt", [128, 2048], F32))
            sc.op("pool", lambda h: h.memset(zt.ap[:, :], 0.0), writes=[zt])

            def zero_fill(hd):
                for i in (2 * hd, 2 * hd + 1):
                    sc.dma("sp", lambda h, i=i: h.dma_start(out=y_s[i * 128:(i + 1) * 128, :], in_=zt.ap[:, :]),
                           reads=[zt], writes=[T()])
            qr = Ring([T(sbb(f"q{i}", [128, S], BF16)) for i in range(2)])
            kr = Ring([T(sbb(f"k{i}", [128, S], BF16)) for i in range(2)])
            vr = Ring([T(sbb(f"v{i}", [128, 16, 132], BF16)) for i in range(2)])
            for t in vr.t:
                sc.op("pool", lambda h, t=t: h.memset(t.ap[:, :, 128:132], 1.0), writes=[t])
            ks32 = T(sbb("ks32", [128, 8], F32))
            ksb = T(sbb("ksb", [128, 8], BF16))
            gm = T(sbb("gm", [128, 128], F32))
            top8 = T(sbb("top8", [128, 128], F32))
            lt = T(sbb("lt", [128, 128], F32))
            Bpr = Ring([T(sbb(f"Bpad{i}", [128, 16, 128], F32)) for i in range(2)])
            for t in Bpr.t:
                sc.op("pool", lambda h, t=t: h.memset(t.ap[:, :, :], 0.0), writes=[t])
            BTr = Ring([T(sbb(f"BT{i}", [128, 512], BF16)) for i in range(2)])
            PT = [[T() for _ in range(16)] for _ in range(2)]
            PTbuf = [sbb(f"PT{i}", [128, 16, 512], BF16) for i in range(2)]
            rinvr = Ring([T(sbb(f"rinv{i}", [128, 512], F32)) for i in range(2)])
            OTr = Ring([T(sbb(f"OTst{i}", [128, S], F32R)) for i in range(2)])
            psST = Ring(PS[0:4])
            psO = Ring(PS[4:5])
            psOT = Ring(PS[5:6])
            psG = PS[6]
            psBT = PS[7]
            def prep_load(hd):
                qT, kT, V = qr.next(), kr.next(), vr.next()
                sc.dma("sp", lambda h: h.dma_start(out=qT.ap[:, :], in_=qk_s[hd]), reads=[T_qk[hd]], writes=[qT])
                sc.dma("sp", lambda h: h.dma_start(out=kT.ap[:, :], in_=qk_s[16 + hd]), reads=[T_qk[16 + hd]], writes=[kT])
                sc.dma("sp", lambda h: h.dma_start(
                    out=V.ap[:, :, 0:128],
                    in_=v_s.rearrange("(tt p) c -> p tt c", p=128)[:, :, hd * 128:(hd + 1) * 128]),
                    reads=[T_v[hd // 2]], writes=[V])
                zero_fill(hd)
                return qT, kT, V

            def prep(hd, loaded):
                qT, kT, V = loaded
                Bpad = Bpr.next()
                sc.op("dve", lambda h: h.tensor_reduce(
                    out=ks32.ap[:, :], in_=kT.ap[:, :].rearrange("p (n k) -> p n k", k=256), axis=AX.X, op=OP.add),
                    reads=[kT], writes=[ks32])
                sc.op("dve", lambda h: h.tensor_copy(out=ksb.ap[:, :], in_=ks32.ap[:, :]), reads=[ks32], writes=[ksb])
                for i in range(16):
                    sc.op("pe", lambda h, i=i: h.matmul(
                        psG.ap[:, i * 8:(i + 1) * 8], lhsT=qT.ap[:, i * 128:(i + 1) * 128], rhs=ksb.ap[:, :],
                        start=True, stop=True), reads=[qT, ksb], writes=[psG])
                sc.op("dve", lambda h: h.tensor_tensor(out=gm.ap[:, :], in0=psG.ap[:, 0:128], in1=pastbias, op=OP.add),
                      reads=[psG, T_cf], writes=[gm])
                for i in range(16):
                    sc.op("dve", lambda h, i=i: h.max(out=top8.ap[:, i * 8:(i + 1) * 8], in_=gm.ap[:, i * 8:(i + 1) * 8]),
                          reads=[gm], writes=[top8])
                sc.op("dve", lambda h: h.tensor_tensor(
                    out=lt.ap[:, :].rearrange("p (i n) -> p i n", n=8),
                    in0=gm.ap[:, :].rearrange("p (i n) -> p i n", n=8),
                    in1=top8.ap[:, :].rearrange("p (i n) -> p i n", n=8)[:, :, 2:3].to_broadcast([128, 16, 8]),
                    op=OP.is_lt), reads=[gm, top8], writes=[lt])
                sc.op("dve", lambda h: h.tensor_tensor(
                    out=Bpad.ap[:, :, 0:8],
                    in0=lt.ap[:, :].rearrange("p (i n) -> p i n", n=8),
                    in1=notown.rearrange("p (i n) -> p i n", n=8), op=OP.mult),
                    reads=[lt, T_cf], writes=[Bpad])
                return qT, kT, V, OTr.next(), Bpad

            def stage1(ctx, c, par):
                qT, kT, V, OTst, Bpad = ctx
                for ii in range(4):
                    sc.op("pe", lambda h, ii=ii: h.transpose(
                        out=psBT.ap[:, ii * 128:(ii + 1) * 128], in_=Bpad.ap[:, 4 * c + ii, :], identity=ident32),
                        reads=[Bpad, T_cf], writes=[psBT])
                BT = BTr.next()
                sc.op("act", lambda h: h.copy(out=BT.ap[:, :], in_=psBT.ap[:, :]), reads=[psBT], writes=[BT])
                for j in range(4 * c + 4):
                    q0 = max(512 * c, 128 * j)
                    off = q0 - 512 * c
                    diag = j >= 4 * c
                    st = psST.next()
                    sc.op("pe", lambda h, st=st, j=j, q0=q0, off=off: h.matmul(
                        st.ap[:, off:512], lhsT=kT.ap[:, j * 128:(j + 1) * 128], rhs=qT.ap[:, q0:512 * c + 512],
                        start=True, stop=False), reads=[kT, qT], writes=[st])
                    sc.op("pe", lambda h, st=st, j=j, off=off, diag=diag: h.matmul(
                        st.ap[:, off:512], lhsT=blkind[:, j * 128:(j + 1) * 128], rhs=BT.ap[:, off:512],
                        start=False, stop=(not diag)), reads=[BT, T_cb], writes=[st])
                    if diag:
                        sc.op("pe", lambda h, st=st, off=off: h.matmul(
                            st.ap[:, off:off + 128], lhsT=identb, rhs=tri, start=False, stop=True,
                            skip_group_check=True), reads=[T_cb], writes=[st])
                    sc.op("act", lambda h, st=st, j=j, off=off: h.activation(
                        out=PTbuf[par][:, j, off:512], in_=st.ap[:, off:512], func=AF.Exp, scale=SCALE),
                        reads=[st], writes=[PT[par][j]])

            def stage2(ctx, c, par, hd):
                qT, kT, V, OTst, Bpad = ctx
                otp = psO.next()
                rsp = psOT.next()
                nj = 4 * c + 4
                for j in range(nj):
                    off = max(512 * c, 128 * j) - 512 * c
                    sc.op("pe", lambda h, j=j, off=off: h.matmul(
                        otp.ap[:, off:512], lhsT=V.ap[:, j, 0:128], rhs=PTbuf[par][:, j, off:512],
                        start=(j == 0), stop=(j == nj - 1), skip_group_check=True), reads=[PT[par][j], V], writes=[otp])
                    sc.op("pe", lambda h, j=j, off=off: h.matmul(
                        rsp.ap[:, off:512], lhsT=onesb, rhs=PTbuf[par][:, j, off:512],
                        start=(j == 0), stop=(j == nj - 1), skip_group_check=True), reads=[PT[par][j], T_cb], writes=[rsp])
                rinv = rinvr.next()
                sc.op("dve", lambda h: h.reciprocal(out=rinv.ap[:, :], in_=rsp.ap[:, :]), reads=[rsp], writes=[rinv])
                sc.op("dve", lambda h: h.tensor_tensor(
                    out=OTst.ap[:, c * 512:(c + 1) * 512], in0=otp.ap[:, :], in1=rinv.ap[:, :], op=OP.mult),
                    reads=[otp, rinv], writes=[OTst])
                if c == 3:
                    sc.dma("pool", lambda h: h.dma_start(out=ot_s[hd], in_=OTst.ap[:, :]), reads=[OTst], writes=[T_ot[hd]])

            items = [(hd, c) for hd in range(NH) for c in range(4)]
            ctxs = {}
            prev = None
            ctxs[0] = prep(0, prep_load(0))
            loaded = None
            for k, (hd, c) in enumerate(items):
                stage1(ctxs[hd], c, k % 2)
                if prev is not None:
                    stage2(ctxs[prev[0]], prev[1], (k - 1) % 2, prev[0])
                if c == 0 and hd + 1 < NH:
                    loaded = prep_load(hd + 1)
                if c == 1 and hd + 1 < NH:
                    ctxs[hd + 1] = prep(hd + 1, loaded)
                prev = (hd, c)
            stage2(ctxs[prev[0]], prev[1], (len(items) - 1) % 2, prev[0])
        sc.fence()
    if "C" in phases:
        with ExitStack() as ec:
            def sbc(name, shape, dt):
                return ec.enter_context(nc.sbuf_tensor(f"s{next(_uid)}_" + name, list(shape), dt))
            cvec = sbc("cvec", [128, 16, 36], F32)
            bG = sbc("bG", [128, 64], F32)
            wr = sbc("wr", [128, 16, 36], F32)
            brt = sbc("br", [128, 36], F32)
            T_cc = T()
            sc.dma("sp", lambda h: h.dma_start(out=cvec[:], in_=cvec_d), writes=[T_cc])
            sc.dma("sp", lambda h: h.dma_start(out=bG[:], in_=bG_d), writes=[T_cc])
            sc.dma("sp", lambda h: h.dma_start(out=wr[:], in_=wr_d), writes=[T_cc])
            sc.dma("sp", lambda h: h.dma_start(out=brt[:], in_=br_d.partition_broadcast(128)), writes=[T_cc])
            sc.dma("pool", lambda h: h.dma_start(out=stab_s, in_=sinit_d), writes=[T_stab])
            halo = T(sbc("halo", [128, 16, 32], F32))
            sc.op("pool", lambda h: h.memset(halo.ap[:, :, :], 0.0), writes=[halo])
            Acum = T(sbc("Acum", [128, 32], F32))
            sc.op("pool", lambda h: h.memset(Acum.ap[:, :], 0.0), writes=[Acum])
            mT = sbc("mT", [128, 16, 512], F32)
            T_mT = [T() for _ in range(16)]
            psA = Ring(PS[0:2])
            psB = Ring(PS[2:4])
            psS = [PS[4], PS[5]]
            psX = Ring(PS[6:8])

            def gemm_fm(wring, wsrc, ps, act, T_act):
                W = wring.next()
                sc.dma("sp", lambda h, W=W: h.dma_start(out=W.ap[:], in_=wsrc), writes=[W])
                for kc in range(16):
                    sc.op("pe", lambda h, W=W, kc=kc: h.matmul(
                        ps.ap[:, :], lhsT=r32(W.ap[:, kc, :]), rhs=r32(act[:, kc, :]),
                        start=(kc == 0), stop=(kc == 15)),
                        reads=[W, (T_act[kc * len(T_act) // 16] if isinstance(T_act, list) else T_act)], writes=[ps])

            for c in range(4):
                t0 = c * 512
                e12 = ExitStack()
                def sb12(name, shape, dt):
                    return e12.enter_context(nc.sbuf_tensor(f"s{next(_uid)}_" + name, list(shape), dt))
                xTc = sb12("xTc", [128, 16, 512], F32R)
                T_xTc = [T() for _ in range(4)]
                OTc = sb12("OTc", [128, 16, 512], F32R)
                T_OTc = T()
                wring = Ring([T(sb12(f"wC{i}", [128, 16, 128], F32R)) for i in range(3)])
                sgr = Ring([T(sb12(f"sg{i}", [128, 512], F32)) for i in range(2)])
                tmr = Ring([T(sb12(f"tm{i}", [128, 512], F32)) for i in range(2)])
                cT = sb12("cT", [128, 16, 512], F32)
                T_cT = [T() for _ in range(16)]
                hTr = Ring([T(sb12(f"hT{i}", [128, 544], F32)) for i in range(4)])
                dgr = Ring([T(sb12(f"dg{i}", [128, 16, 128], F32)) for i in range(2)])
                mean = T(sb12("mean", [128, 512], F32))
                rstd = T(sb12("rstd", [128, 512], F32))
                for kq in range(4):
                    sc.dma("sp", lambda h, kq=kq: h.dma_start(
                        out=xTc[:, 4 * kq:4 * kq + 4, :],
                        in_=xT_d.rearrange("(kc p) t -> p kc t", p=128)[:, 4 * kq:4 * kq + 4, t0:t0 + 512]),
                        writes=[T_xTc[kq]])
                sc.dma("sp", lambda h: h.dma_start(
                    out=OTc[:, :, :], in_=ot_s.rearrange("k p t -> p k t")[:, :, t0:t0 + 512]),
                    reads=T_ot, writes=[T_OTc])

                def conv_pe(cc, hT):
                    halves = []
                    for hh in range(2):
                        j0 = 16 * hh
                        n = 16 if hh == 0 else 15
                        dg = dgr.next()
                        sc.op("pool", lambda h, dg=dg, j0=j0, n=n: h.tensor_tensor(
                            out=r32(dg.ap[:, 0:n, :]), in0=ident32.unsqueeze(1).to_broadcast([128, n, 128]),
                            in1=cvec[:, cc, j0:j0 + n].unsqueeze(2).to_broadcast([128, n, 128]), op=OP.mult),
                            reads=[T_cf, T_cc], writes=[dg])
                        halves.append(dg)
                    ps = psX.next()
                    for j in range(31):
                        dg = halves[j // 16]
                        sc.op("pe", lambda h, j=j, dg=dg: h.matmul(
                            ps.ap[:, :], lhsT=r32(dg.ap[:, j % 16, :]), rhs=r32(hT.ap[:, 2 + j:514 + j]),
                            start=(j == 0), stop=(j == 30)), reads=[dg, hT], writes=[ps])
                    sc.op("act", lambda h: h.activation(
                        out=r32(cT[:, cc, :]), in_=ps.ap[:, :], func=AF.Identity, bias=cvec[:, cc, 31:32], scale=1.0),
                        reads=[ps, T_cc], writes=[T_cT[cc]])
                    sc.op("pool", lambda h: h.tensor_copy(out=halo.ap[:, cc, :], in_=hT.ap[:, 512:544]),
                          reads=[hT], writes=[halo])

                pend = None
                for cc in range(16):
                    hT = hTr.next()
                    sc.op("pool", lambda h, cc=cc, hT=hT: h.tensor_copy(out=r32(hT.ap[:, 0:32]), in_=halo.ap[:, cc, :]),
                          reads=[halo], writes=[hT])
                    pa, pg = psA.next(), psB.next()
                    gemm_fm(wring, wG_d[cc], pa, xTc, T_xTc)
                    gemm_fm(wring, wG_d[16 + cc], pg, xTc, T_xTc)
                    sg = sgr.next()
                    sc.op("act", lambda h, sg=sg, pg=pg, cc=cc: h.activation(
                        out=sg.ap[:, :], in_=pg.ap[:, :], func=AF.Sigmoid, bias=bG[:, 16 + cc:17 + cc], scale=1.0),
                        reads=[pg, T_cc], writes=[sg])
                    sc.op("dve", lambda h, sg=sg, pa=pa, cc=cc, hT=hT: h.scalar_tensor_tensor(
                        out=r32(hT.ap[:, 32:544]), in0=pa.ap[:, :], scalar=bG[:, cc:cc + 1], in1=sg.ap[:, :],
                        op0=OP.add, op1=OP.mult), reads=[pa, sg, T_cc, hT], writes=[hT])
                    if pend is not None:
                        conv_pe(*pend)
                    pend = (cc, hT)
                conv_pe(*pend)

                pm, pq = psS
                for cc in range(16):
                    sc.op("pe", lambda h, cc=cc: h.matmul(pm.ap[:, :], lhsT=r32(onesn), rhs=r32(cT[:, cc, :]),
                                                         start=(cc == 0), stop=(cc == 15)),
                          reads=[T_cT[cc], T_cf], writes=[pm])
                for cc in range(16):
                    sq = hTr.next()
                    sc.op("act", lambda h, sq=sq, cc=cc: h.activation(out=r32(sq.ap[:, 0:512]), in_=cT[:, cc, :], func=AF.Square),
                          reads=[T_cT[cc]], writes=[sq])
                    sc.op("pe", lambda h, sq=sq, cc=cc: h.matmul(pq.ap[:, :], lhsT=r32(onesn), rhs=r32(sq.ap[:, 0:512]),
                                                                start=(cc == 0), stop=(cc == 15)),
                          reads=[sq, T_cf], writes=[pq])
                sc.op("dve", lambda h: h.tensor_copy(out=mean.ap[:, :], in_=pm.ap[:, :]), reads=[pm], writes=[mean])
                sc.op("dve", lambda h: h.tensor_tensor(out=rstd.ap[:, :], in0=mean.ap[:, :], in1=mean.ap[:, :], op=OP.mult),
                      reads=[mean], writes=[rstd])
                sc.op("dve", lambda h: h.tensor_tensor(out=rstd.ap[:, :], in0=pq.ap[:, :], in1=rstd.ap[:, :], op=OP.subtract),
                      reads=[pq, rstd], writes=[rstd])
                sc.op("dve", lambda h: h.tensor_scalar(out=rstd.ap[:, :], in0=rstd.ap[:, :], scalar1=LN_EPS, scalar2=None,
                                                       op0=OP.add), reads=[rstd], writes=[rstd])
                sc.op("act", lambda h: h.activation(out=rstd.ap[:, :], in_=rstd.ap[:, :], func=AF.Sqrt),
                      reads=[rstd], writes=[rstd])
                sc.op("dve", lambda h: h.reciprocal(out=rstd.ap[:, :], in_=rstd.ap[:, :]), reads=[rstd], writes=[rstd])

                for j in range(16):
                    cc = j
                    en = "dve" if cc % 2 == 0 else "pool"
                    nt = hTr.next()
                    sc.op(en, lambda h, cc=cc, nt=nt: h.tensor_tensor(out=r32(nt.ap[:, 0:512]), in0=cT[:, cc, :], in1=mean.ap[:, :],
                                                                      op=OP.subtract), reads=[T_cT[cc], mean], writes=[nt])
                    sc.op(en, lambda h, cc=cc, nt=nt: h.tensor_tensor(out=r32(nt.ap[:, 0:512]), in0=nt.ap[:, 0:512], in1=rstd.ap[:, :],
                                                                      op=OP.mult), reads=[nt, rstd], writes=[nt])
                    sc.op("act", lambda h, cc=cc, nt=nt: h.activation(
                        out=r32(cT[:, cc, :]), in_=nt.ap[:, 0:512], func=AF.Silu, bias=cvec[:, cc, 33:34], scale=cvec[:, cc, 32:33]),
                        reads=[nt, T_cc], writes=[T_cT[cc]])
                    py, pg = psA.next(), psB.next()
                    gemm_fm(wring, wo_d[j], py, OTc, T_OTc)
                    gemm_fm(wring, wG_d[32 + j], pg, xTc, T_xTc)
                    sg = sgr.next()
                    sc.op("act", lambda h, sg=sg, pg=pg, j=j: h.activation(
                        out=sg.ap[:, :], in_=pg.ap[:, :], func=AF.Sigmoid, bias=bG[:, 32 + j:33 + j], scale=1.0),
                        reads=[pg, T_cc], writes=[sg])
                    sc.op("dve", lambda h, sg=sg, py=py, j=j: h.tensor_tensor(
                        out=r32(mT[:, j, :]), in0=py.ap[:, :], in1=sg.ap[:, :], op=OP.mult),
                        reads=[py, sg], writes=[T_mT[j]])
                for j in range(16):
                    py, pg = psA.next(), psB.next()
                    gemm_fm(wring, wp_d[j], py, cT, T_cT)
                    gemm_fm(wring, wG_d[48 + j], pg, xTc, T_xTc)
                    sg = sgr.next()
                    sc.op("act", lambda h, sg=sg, pg=pg, j=j: h.activation(
                        out=sg.ap[:, :], in_=pg.ap[:, :], func=AF.Sigmoid, bias=bG[:, 48 + j:49 + j], scale=1.0),
                        reads=[pg, T_cc], writes=[sg])
                    tm = tmr.next()
                    sc.op("dve", lambda h, sg=sg, py=py, j=j, tm=tm: h.scalar_tensor_tensor(
                        out=tm.ap[:, :], in0=py.ap[:, :], scalar=cvec[:, j, 34:35], in1=sg.ap[:, :],
                        op0=OP.add, op1=OP.mult), reads=[py, sg, T_cc], writes=[tm])
                    sc.op("pool", lambda h, tm=tm, j=j: h.tensor_tensor(
                        out=r32(mT[:, j, :]), in0=mT[:, j, :], in1=tm.ap[:, :], op=OP.add),
                        reads=[tm, T_mT[j]], writes=[T_mT[j]])
                e12.close()
                sc.fence()
                with ExitStack() as e3:
                    def sb3(name, shape, dt):
                        return e3.enter_context(nc.sbuf_tensor(f"s{next(_uid)}_" + name, list(shape), dt))
                    wor = Ring([T(sb3(f"wout{i}", [128, 16, 512], F32R)) for i in range(2)])
                    lng = sb3("lng", [128, 2048], F32)
                    lnb = sb3("lnb", [128, 2048], F32)
                    T_ln = T()
                    sc.dma("sp", lambda h: h.dma_start(out=lng[:], in_=lnv_d[0:1, :].partition_broadcast(128)), writes=[T_ln])
                    sc.dma("sp", lambda h: h.dma_start(out=lnb[:], in_=lnv_d[1:2, :].partition_broadcast(128)), writes=[T_ln])
                    xr = [T(sb3(f"xt{i}", [128, 2048], F32)) for i in range(4)]
                    zr = xr
                    for t in range(4):
                        sc.dma("sp", lambda h, t=t, t0=t0: h.dma_start(
                            out=xr[t].ap[:, :], in_=x_d[t0 + t * 128:t0 + (t + 1) * 128, :]), writes=[xr[t]])
                    for cb in range(4):
                        W = wor.next()
                        sc.dma("sp", lambda h, W=W, cb=cb: h.dma_start(out=W.ap[:], in_=wout_d[cb]), writes=[W])
                        for t in range(4):
                            ps = psA.next() if t % 2 == 0 else psB.next()
                            for kc in range(16):
                                sc.op("pe", lambda h, ps=ps, W=W, kc=kc, t=t: h.matmul(
                                    ps.ap[:, :], lhsT=r32(mT[:, kc, t * 128:(t + 1) * 128]), rhs=r32(W.ap[:, kc, :]),
                                    start=(kc == 0), stop=(kc == 15)), reads=[W, T_mT[kc]], writes=[ps])
                            sc.op("dve", lambda h, ps=ps, t=t, cb=cb: h.scalar_tensor_tensor(
                                out=zr[t].ap[:, cb * 512:(cb + 1) * 512], in0=xr[t].ap[:, cb * 512:(cb + 1) * 512],
                                scalar=ALPHA, in1=ps.ap[:, :], op0=OP.mult, op1=OP.add),
                                reads=[ps, xr[t]], writes=[zr[t]])
                    def mk_small(t):
                        d = {}
                        for nm, shp, dt in (("st6", [128, 4, 6], F32), ("mv", [128, 2], F32), ("rs", [128, 2], F32),
                                            ("lg", [128, 36], F32), ("sm", [128, 64], F32), ("oh", [128, 4], F32),
                                            ("esel", [128, 8], F32), ("t8", [128, 8], F32), ("mk", [128, 2, 8], F32),
                                            ("Ak", [128, 2, 32], F32), ("Asum", [128, 32], F32), ("val", [128, 32], F32),
                                            ("ov", [128, 32], F32), ("dst", [128, 2], F32), ("dsti", [128, 2], I32),
                                            ("pay", [128, 2, 4], I32), ("tokf", [128, 2], F32)):
                            d[nm] = T(sb3(f"{nm}{t}", shp, dt))
                        return d
                    small = [mk_small(t) for t in range(4)]
                    hTts = [T(sb3(f"hTt{i}", [128, 16, 128], F32)) for i in range(2)]

                    def tail1(t):
                        g = c * 4 + t
                        z = zr[t]
                        d = small[t]
                        st6, mv, rs, lg, sm, oh, esel, t8, mk, Ak, Asum = (d[k] for k in (
                            "st6", "mv", "rs", "lg", "sm", "oh", "esel", "t8", "mk", "Ak", "Asum"))
                        hTt = hTts[t % 2]
                        for q4 in range(4):
                            sc.op("dve", lambda h, q4=q4: h.bn_stats(out=st6.ap[:, q4, :], in_=z.ap[:, q4 * 512:(q4 + 1) * 512]),
                                  reads=[z], writes=[st6])
                        sc.op("dve", lambda h: h.bn_aggr(out=mv.ap[:, :], in_=st6.ap[:, :, :].rearrange("p a b -> p (a b)")),
                              reads=[st6], writes=[mv])
                        sc.op("dve", lambda h: h.tensor_scalar(out=rs.ap[:, 0:1], in0=mv.ap[:, 1:2], scalar1=LN_EPS, scalar2=None,
                                                               op0=OP.add), reads=[mv], writes=[rs])
                        yield
                        sc.op("act", lambda h: h.activation(out=rs.ap[:, 0:1], in_=rs.ap[:, 0:1], func=AF.Sqrt), reads=[rs], writes=[rs])
                        yield
                        sc.op("dve", lambda h: h.reciprocal(out=rs.ap[:, 0:1], in_=rs.ap[:, 0:1]), reads=[rs], writes=[rs])
                        sc.op("dve", lambda h: h.tensor_scalar(out=rs.ap[:, 1:2], in0=mv.ap[:, 0:1], scalar1=rs.ap[:, 0:1], scalar2=-1.0,
                                                               op0=OP.mult, op1=OP.mult), reads=[mv, rs], writes=[rs])
                        yield
                        sc.op("act", lambda h: h.activation(out=z.ap[:, :], in_=z.ap[:, :], func=AF.Identity,
                                                            scale=rs.ap[:, 0:1], bias=rs.ap[:, 1:2]), reads=[z, rs], writes=[z])
                        yield
                        sc.op("pool", lambda h: h.tensor_tensor(out=z.ap[:, :], in0=z.ap[:, :], in1=lng[:, :], op=OP.mult),
                              reads=[z, T_ln], writes=[z])
                        yield
                        sc.op("dve", lambda h: h.tensor_tensor(out=z.ap[:, :], in0=z.ap[:, :], in1=lnb[:, :], op=OP.add),
                              reads=[z, T_ln], writes=[z])
                        sc.dma("pool", lambda h: h.dma_start(out=h_s[g * 128:(g + 1) * 128, :], in_=z.ap[:, :]),
                               reads=[z], writes=[T_h[g]])
                        yield
                        for k4 in range(4):
                            tp = psX.next()
                            for ii in range(4):
                                kc = k4 * 4 + ii
                                sc.op("pe", lambda h, tp=tp, ii=ii, kc=kc: h.transpose(
                                    out=tp.ap[:, ii * 128:(ii + 1) * 128], in_=z.ap[:, kc * 128:(kc + 1) * 128], identity=ident32),
                                    reads=[z, T_cf], writes=[tp])
                            sc.op("act", lambda h, tp=tp, k4=k4: h.copy(
                                out=hTt.ap[:, k4 * 4:(k4 + 1) * 4, :].rearrange("p a b -> p (a b)"), in_=tp.ap[:, :]),
                                reads=[tp], writes=[hTt])
                        pl = psS[0]
                        for kc in range(16):
                            sc.op("pe", lambda h, kc=kc: h.matmul(pl.ap[:, t * 64:t * 64 + 36], lhsT=hTt.ap[:, kc, :], rhs=wr[:, kc, :],
                                                                  start=(kc == 0), stop=(kc == 15)),
                                  reads=[hTt, T_cc], writes=[pl])
                        yield
                        sc.op("dve", lambda h: h.tensor_tensor(out=lg.ap[:, :], in0=pl.ap[:, t * 64:t * 64 + 36], in1=brt[:, :], op=OP.add),
                              reads=[pl, T_cc], writes=[lg])
                        sc.op("dve", lambda h: h.tensor_reduce(out=sm.ap[:, 0:1], in_=lg.ap[:, 0:4], axis=AX.X, op=OP.max),
                              reads=[lg], writes=[sm])
                        sc.op("dve", lambda h: h.tensor_scalar(out=oh.ap[:, :], in0=lg.ap[:, 0:4], scalar1=sm.ap[:, 0:1], scalar2=None,
                                                               op0=OP.is_equal), reads=[lg, sm], writes=[oh])
                        sc.op("dve", lambda h: h.tensor_scalar(out=sm.ap[:, 1:2], in0=sm.ap[:, 0:1], scalar1=-1.0, scalar2=None,
                                                               op0=OP.mult), reads=[sm], writes=[sm])
                        sc.op("dve", lambda h: h.tensor_scalar(out=esel.ap[:, :], in0=lg.ap[:, 4:12], scalar1=oh.ap[:, 0:1], scalar2=None,
                                                               op0=OP.mult), reads=[lg, oh], writes=[esel])
                        for gi in range(1, 4):
                            sc.op("dve", lambda h, gi=gi: h.scalar_tensor_tensor(
                                out=esel.ap[:, :], in0=lg.ap[:, 4 + 8 * gi:12 + 8 * gi], scalar=oh.ap[:, gi:gi + 1], in1=esel.ap[:, :],
                                op0=OP.mult, op1=OP.add), reads=[lg, oh, esel], writes=[esel])
                        sc.op("dve", lambda h: h.max(out=t8.ap[:, :], in_=esel.ap[:, :]), reads=[esel], writes=[t8])
                        for k in range(2):
                            sc.op("dve", lambda h, k=k: h.tensor_scalar(out=mk.ap[:, k, :], in0=esel.ap[:, :], scalar1=t8.ap[:, k:k + 1],
                                                                        scalar2=None, op0=OP.is_equal), reads=[esel, t8], writes=[mk])
                        sc.op("dve", lambda h: h.tensor_tensor(out=sm.ap[:, 4:5], in0=t8.ap[:, 1:2], in1=t8.ap[:, 0:1], op=OP.subtract),
                              reads=[t8], writes=[sm])
                        yield
                        sc.op("act", lambda h: h.activation(out=sm.ap[:, 10:14], in_=lg.ap[:, 0:4], func=AF.Exp, bias=sm.ap[:, 1:2],
                                                            scale=1.0), reads=[lg, sm], writes=[sm])
                        sc.op("act", lambda h: h.activation(out=sm.ap[:, 5:6], in_=sm.ap[:, 4:5], func=AF.Exp), reads=[sm], writes=[sm])
                        yield
                        sc.op("dve", lambda h: h.tensor_reduce(out=sm.ap[:, 2:3], in_=sm.ap[:, 10:14], axis=AX.X, op=OP.add),
                              reads=[sm], writes=[sm])
                        sc.op("dve", lambda h: h.reciprocal(out=sm.ap[:, 3:4], in_=sm.ap[:, 2:3]), reads=[sm], writes=[sm])
                        sc.op("dve", lambda h: h.tensor_scalar(out=sm.ap[:, 6:7], in0=sm.ap[:, 5:6], scalar1=1.0, scalar2=None, op0=OP.add),
                              reads=[sm], writes=[sm])
                        sc.op("dve", lambda h: h.reciprocal(out=sm.ap[:, 6:7], in_=sm.ap[:, 6:7]), reads=[sm], writes=[sm])
                        sc.op("dve", lambda h: h.tensor_tensor(out=sm.ap[:, 7:8], in0=sm.ap[:, 6:7], in1=sm.ap[:, 3:4], op=OP.mult),
                              reads=[sm], writes=[sm])
                        sc.op("dve", lambda h: h.tensor_tensor(out=sm.ap[:, 8:9], in0=sm.ap[:, 7:8], in1=sm.ap[:, 5:6], op=OP.mult),
                              reads=[sm], writes=[sm])
                        for k in range(2):
                            for gi in range(4):
                                sc.op("dve", lambda h, k=k, gi=gi: h.tensor_scalar(
                                    out=Ak.ap[:, k, gi * 8:(gi + 1) * 8], in0=mk.ap[:, k, :], scalar1=oh.ap[:, gi:gi + 1], scalar2=None,
                                    op0=OP.mult), reads=[mk, oh], writes=[Ak])
                        sc.op("dve", lambda h: h.tensor_tensor(out=Asum.ap[:, :], in0=Ak.ap[:, 0, :], in1=Ak.ap[:, 1, :], op=OP.add),
                              reads=[Ak], writes=[Asum])
                        yield

                    def tail2(t):
                        g = c * 4 + t
                        d = small[t]
                        sm, Ak, Asum, val, ov, dst, dsti, pay, tokf = (d[k] for k in (
                            "sm", "Ak", "Asum", "val", "ov", "dst", "dsti", "pay", "tokf"))
                        pp = psS[1]
                        sc.op("pe", lambda h: h.matmul(pp.ap[:, 0:32], lhsT=U32, rhs=Asum.ap[:, :], start=True, stop=False),
                              reads=[Asum, T_cf], writes=[pp])
                        sc.op("pe", lambda h: h.matmul(pp.ap[:, 0:32], lhsT=ones32, rhs=Acum.ap[:, :], start=False, stop=True),
                              reads=[Acum, T_cf], writes=[pp])
                        sc.op("dve", lambda h: h.tensor_scalar(out=ov.ap[:, :], in0=pp.ap[:, 0:32], scalar1=float(CAP), scalar2=1.0e6,
                                                               op0=OP.is_ge, op1=OP.mult), reads=[pp], writes=[ov])
                        sc.op("dve", lambda h: h.tensor_tensor(out=val.ap[:, :], in0=pp.ap[:, 0:32], in1=slotbase, op=OP.add),
                              reads=[pp, T_cf], writes=[val])
                        sc.op("dve", lambda h: h.tensor_tensor(out=Acum.ap[:, :], in0=Acum.ap[:, :], in1=Asum.ap[:, :], op=OP.add),
                              reads=[Acum, Asum], writes=[Acum])
                        sc.op("dve", lambda h: h.tensor_tensor(out=val.ap[:, :], in0=val.ap[:, :], in1=ov.ap[:, :], op=OP.add),
                              reads=[val, ov], writes=[val])
                        for k in range(2):
                            sc.op("dve", lambda h, k=k: h.tensor_tensor(out=Ak.ap[:, k, :], in0=Ak.ap[:, k, :], in1=val.ap[:, :], op=OP.mult),
                                  reads=[Ak, val], writes=[Ak])
                            sc.op("dve", lambda h, k=k: h.tensor_reduce(out=dst.ap[:, k:k + 1], in_=Ak.ap[:, k, :], axis=AX.X, op=OP.add),
                                  reads=[Ak], writes=[dst])
                        sc.op("dve", lambda h: h.tensor_copy(out=dsti.ap[:, :], in_=dst.ap[:, :]), reads=[dst], writes=[dsti])
                        sc.op("pool", lambda h: h.tensor_scalar(out=tokf.ap[:, 0:1], in0=iota_p, scalar1=float(g * 128), scalar2=None,
                                                                op0=OP.add), reads=[T_cf], writes=[tokf])
                        sc.op("pool", lambda h: h.tensor_scalar(out=tokf.ap[:, 1:2], in0=iota_p, scalar1=float(g * 128 + S), scalar2=None,
                                                                op0=OP.add), reads=[T_cf], writes=[tokf])
                        sc.op("pool", lambda h: h.memset(pay.ap[:, :, :], 0), writes=[pay])
                        sc.op("pool", lambda h: h.tensor_copy(out=pay.ap[:, 0, 0:1], in_=tokf.ap[:, 0:1]), reads=[tokf], writes=[pay])
                        sc.op("pool", lambda h: h.tensor_copy(out=pay.ap[:, 0, 1:2], in_=tokf.ap[:, 0:1]), reads=[tokf], writes=[pay])
                        sc.op("pool", lambda h: h.tensor_copy(out=pay.ap[:, 1, 0:1], in_=tokf.ap[:, 0:1]), reads=[tokf], writes=[pay])
                        sc.op("pool", lambda h: h.tensor_copy(out=pay.ap[:, 1, 1:2], in_=tokf.ap[:, 1:2]), reads=[tokf], writes=[pay])
                        for k in range(2):
                            sc.op("dve", lambda h, k=k: h.tensor_copy(out=pay.ap[:, k, 2:3].bitcast(F32), in_=sm.ap[:, 7 + k:8 + k]),
                                  reads=[sm], writes=[pay])
                        for k in range(2):
                            sc.dma("pool", lambda h, k=k: h.indirect_dma_start(
                                out=stab_s[:, :], out_offset=bass.IndirectOffsetOnAxis(ap=dsti.ap[:, k:k + 1], axis=0),
                                in_=pay.ap[:, k, :], in_offset=None, bounds_check=reg_ns, oob_is_err=False),
                                reads=[pay, dsti], writes=[T_stab])

                    gens = [tail1(t) for t in range(4)]
                    live = list(gens)
                    while live:
                        for gnr in list(live):
                            try:
                                next(gnr)
                            except StopIteration:
                                live.remove(gnr)
                    for t in range(4):
                        tail2(t)

                sc.fence()
        sc.fence()
    if "D" in phases:
        with ExitStack() as ed:
            def sbd(name, shape, dt):
                return ed.enter_context(nc.sbuf_tensor(f"s{next(_uid)}_" + name, list(shape), dt))
            str_ = Ring([T(sbd(f"stb{i}", [128, 2, 4], I32)) for i in range(3)])
            hgr = Ring([T(sbd(f"hg{i}", [128, 2048], F32)) for i in range(2)])
            hgTr = Ring([T(sbd(f"hgT{i}", [128, 16, 256], F32R)) for i in range(2)])
            wer = Ring([T(sbd(f"web{i}", [128, 4096], F32R)) for i in range(7)])
            sgr = Ring([T(sbd(f"sgd{i}", [128, 256], F32)) for i in range(2)])
            actT = sbd("actT", [128, 8, 256], F32R)
            T_act = [T() for _ in range(8)]
            your = Ring([T(sbd(f"yout{i}", [128, 2048], F32)) for i in range(2)])
            psT = Ring(PS[0:2])
            psa = Ring(PS[2:4])
            psb = Ring(PS[4:6])
            psy = Ring(PS[6:8])

            def gather(e):
                stb = str_.next()
                sc.dma("pool", lambda h: h.dma_start(
                    out=stb.ap[:, :, :], in_=stab_s[e * CAP:(e + 1) * CAP, :].rearrange("(s p) c -> p s c", p=128)),
                    reads=[T_stab], writes=[stb])
                hgs = []
                for s in range(2):
                    hg = hgr.next()
                    sc.dma("pool", lambda h, hg=hg, s=s: h.indirect_dma_start(
                        out=hg.ap[:, :], out_offset=None, in_=h_s[:, :],
                        in_offset=bass.IndirectOffsetOnAxis(ap=stb.ap[:, s, 0:1], axis=0)),
                        reads=[stb] + T_h, writes=[hg])
                    hgs.append(hg)
                return stb, hgs

            def transposes(hgs):
                hgT = hgTr.next()
                for s in range(2):
                    hg = hgs[s]
                    for k4 in range(4):
                        tp = psT.next()
                        for ii in range(4):
                            kc = k4 * 4 + ii
                            sc.op("pe", lambda h, tp=tp, ii=ii, kc=kc, hg=hg: h.transpose(
                                out=tp.ap[:, ii * 128:(ii + 1) * 128], in_=hg.ap[:, kc * 128:(kc + 1) * 128], identity=ident32),
                                reads=[hg, T_cf], writes=[tp])
                        if k4 % 2 == 0:
                            sc.op("dve", lambda h, tp=tp, k4=k4, s=s: h.tensor_copy(
                                out=hgT.ap[:, k4 * 4:(k4 + 1) * 4, s * 128:(s + 1) * 128],
                                in_=tp.ap[:, :].rearrange("p (a b) -> p a b", b=128)), reads=[tp], writes=[hgT])
                        else:
                            sc.op("act", lambda h, tp=tp, k4=k4, s=s: h.copy(
                                out=hgT.ap[:, k4 * 4:(k4 + 1) * 4, s * 128:(s + 1) * 128],
                                in_=tp.ap[:, :].rearrange("p (a b) -> p a b", b=128)), reads=[tp], writes=[hgT])
                return hgT

            nxt = gather(0)
            hgT_next = transposes(nxt[1])
            for e in range(NE):
                stb, hgs = nxt
                hgT = hgT_next
                if e + 1 < NE:
                    nxt = gather(e + 1)
                for fb in range(4):
                    W1, W3 = wer.next(), wer.next()
                    sc.dma("sp", lambda h, W1=W1, fb=fb: h.dma_start(
                        out=W1.ap[:, :], in_=w1_d[e, fb].rearrange("p k c -> p (k c)")), writes=[W1])
                    sc.dma("sp", lambda h, W3=W3, fb=fb: h.dma_start(
                        out=W3.ap[:, :], in_=w3_d[e, fb].rearrange("p k c -> p (k c)")), writes=[W3])
                    for half in range(2):
                        fc = fb * 2 + half
                        pa, pb = psa.next(), psb.next()
                        for kc in range(16):
                            sc.op("pe", lambda h, pa=pa, W1=W1, kc=kc, half=half: h.matmul(
                                pa.ap[:, 0:256], lhsT=r32(W1.ap[:, kc * 256 + half * 128:kc * 256 + (half + 1) * 128]), rhs=r32(hgT.ap[:, kc, :]),
                                start=(kc == 0), stop=(kc == 15)), reads=[W1, hgT], writes=[pa])
                        for kc in range(16):
                            sc.op("pe", lambda h, pb=pb, W3=W3, kc=kc, half=half: h.matmul(
                                pb.ap[:, 0:256], lhsT=r32(W3.ap[:, kc * 256 + half * 128:kc * 256 + (half + 1) * 128]), rhs=r32(hgT.ap[:, kc, :]),
                                start=(kc == 0), stop=(kc == 15)), reads=[W3, hgT], writes=[pb])
                        sg = sgr.next()
                        sc.op("act", lambda h, sg=sg, pa=pa: h.activation(out=sg.ap[:, :], in_=pa.ap[:, 0:256], func=AF.Silu),
                              reads=[pa], writes=[sg])
                        sc.op("dve", lambda h, sg=sg, pb=pb, fc=fc: h.tensor_tensor(
                            out=actT[:, fc, :], in0=pb.ap[:, 0:256], in1=sg.ap[:, :], op=OP.mult),
                            reads=[pb, sg], writes=[T_act[fc]])
                if e + 1 < NE:
                    hgT_next = transposes(nxt[1])
                youts = [your.next(), your.next()]
                for cb in range(4):
                    W2 = wer.next()
                    sc.dma("sp", lambda h, W2=W2, cb=cb: h.dma_start(
                        out=W2.ap[:, :], in_=w2_d[e, cb].rearrange("p k c -> p (k c)")), writes=[W2])
                    for s in range(2):
                        py = psy.next()
                        for fc in range(8):
                            sc.op("pe", lambda h, py=py, W2=W2, fc=fc, s=s: h.matmul(
                                py.ap[:, :], lhsT=r32(actT[:, fc, s * 128:(s + 1) * 128]), rhs=r32(W2.ap[:, fc * 512:(fc + 1) * 512]),
                                start=(fc == 0), stop=(fc == 7)), reads=[W2, T_act[fc]], writes=[py])
                        yo = youts[s]
                        sc.op("act", lambda h, py=py, yo=yo, cb=cb, s=s: h.activation(
                            out=yo.ap[:, cb * 512:(cb + 1) * 512], in_=py.ap[:, :], func=AF.Copy,
                            scale=stb.ap[:, s, 2:3].bitcast(F32)), reads=[py, stb], writes=[yo])
                for s in range(2):
                    yo = youts[s]
                    sc.dma("pool", lambda h, yo=yo, s=s: h.indirect_dma_start(
                        out=y_s[:, :], out_offset=bass.IndirectOffsetOnAxis(ap=stb.ap[:, s, 1:2], axis=0),
                        in_=yo.ap[:, :], in_offset=None, bounds_check=reg_y, oob_is_err=False),
                        reads=[yo, stb], writes=[T_y])
                if e % 8 == 7 and e != NE - 1:
                    sc.fence()
        sc.fence()
    if "E" in phases:
        with ExitStack() as ee:
            def sbe(name, shape, dt):
                return ee.enter_context(nc.sbuf_tensor(f"s{next(_uid)}_" + name, list(shape), dt))
            lng = sbe("lng2", [128, 2048], F32)
            lnb = sbe("lnb2", [128, 2048], F32)
            T_ln = T()
            sc.dma("sp", lambda h: h.dma_start(out=lng[:], in_=lnv_d[2:3, :].partition_broadcast(128)), writes=[T_ln])
            sc.dma("sp", lambda h: h.dma_start(out=lnb[:], in_=lnv_d[3:4, :].partition_broadcast(128)), writes=[T_ln])
            hr = Ring([T(sbe(f"he{i}", [128, 2048], F32)) for i in range(4)])
            y0r = Ring([T(sbe(f"y0e{i}", [128, 2048], F32)) for i in range(3)])
            y1r = Ring([T(sbe(f"y1e{i}", [128, 2048], F32)) for i in range(3)])
            smr = Ring([(T(sbe(f"st6e{i}", [128, 4, 6], F32)), T(sbe(f"mve{i}", [128, 2], F32)), T(sbe(f"rse{i}", [128, 2], F32)))
                        for i in range(4)])
            T_out = T()

            def etile(g):
                ht, y0, y1 = hr.next(), y0r.next(), y1r.next()
                st6, mv, rs = smr.next()
                sc.dma("sp", lambda h: h.dma_start(out=ht.ap[:, :], in_=h_s[g * 128:(g + 1) * 128, :]), reads=[T_h[g]], writes=[ht])
                sc.dma("sp", lambda h: h.dma_start(out=y0.ap[:, :], in_=y_s[g * 128:(g + 1) * 128, :]), reads=[T_y], writes=[y0])
                sc.dma("sp", lambda h: h.dma_start(out=y1.ap[:, :], in_=y_s[S + g * 128:S + (g + 1) * 128, :]), reads=[T_y], writes=[y1])
                yield
                sc.op("pool", lambda h: h.tensor_tensor(out=y0.ap[:, :], in0=y0.ap[:, :], in1=y1.ap[:, :], op=OP.add),
                      reads=[y0, y1], writes=[y0])
                yield
                sc.op("dve", lambda h: h.scalar_tensor_tensor(
                    out=ht.ap[:, :], in0=ht.ap[:, :], scalar=ALPHA, in1=y0.ap[:, :], op0=OP.mult, op1=OP.add),
                    reads=[ht, y0], writes=[ht])
                for q4 in range(4):
                    sc.op("dve", lambda h, q4=q4: h.bn_stats(out=st6.ap[:, q4, :], in_=ht.ap[:, q4 * 512:(q4 + 1) * 512]),
                          reads=[ht], writes=[st6])
                sc.op("dve", lambda h: h.bn_aggr(out=mv.ap[:, :], in_=st6.ap[:, :, :].rearrange("p a b -> p (a b)")),
                      reads=[st6], writes=[mv])
                sc.op("dve", lambda h: h.tensor_scalar(out=rs.ap[:, 0:1], in0=mv.ap[:, 1:2], scalar1=LN_EPS, scalar2=None, op0=OP.add),
                      reads=[mv], writes=[rs])
                yield
                sc.op("act", lambda h: h.activation(out=rs.ap[:, 0:1], in_=rs.ap[:, 0:1], func=AF.Sqrt), reads=[rs], writes=[rs])
                yield
                sc.op("dve", lambda h: h.reciprocal(out=rs.ap[:, 0:1], in_=rs.ap[:, 0:1]), reads=[rs], writes=[rs])
                sc.op("dve", lambda h: h.tensor_scalar(out=rs.ap[:, 1:2], in0=mv.ap[:, 0:1], scalar1=rs.ap[:, 0:1], scalar2=-1.0,
                                                       op0=OP.mult, op1=OP.mult), reads=[mv, rs], writes=[rs])
                yield
                sc.op("act", lambda h: h.activation(out=ht.ap[:, :], in_=ht.ap[:, :], func=AF.Identity,
                                                    scale=rs.ap[:, 0:1], bias=rs.ap[:, 1:2]), reads=[ht, rs], writes=[ht])
                yield
                sc.op("pool", lambda h: h.tensor_tensor(out=ht.ap[:, :], in0=ht.ap[:, :], in1=lng[:, :], op=OP.mult),
                      reads=[ht, T_ln], writes=[ht])
                yield
                sc.op("dve", lambda h: h.tensor_tensor(out=ht.ap[:, :], in0=ht.ap[:, :], in1=lnb[:, :], op=OP.add),
                      reads=[ht, T_ln], writes=[ht])
                sc.dma("pool", lambda h: h.dma_start(out=out_d[g * 128:(g + 1) * 128, :], in_=ht.ap[:, :]),
                       reads=[ht], writes=[T()])
                yield

            pending = list(range(16))
            live = []
            while pending or live:
                if pending and len(live) < 3:
                    live.append(etile(pending.pop(0)))
                for gnr in list(live):
                    try:
                        next(gnr)
                    except StopIteration:
                        live.remove(gnr)
        sc.fence()
        sc.fence()
    sc.finish()
    es.close()
    return nc


def _st_blocks(w, cols=128):
    K, N = w.shape
    return np.ascontiguousarray(w.reshape(K // 128, 128, N // cols, cols).transpose(2, 1, 0, 3))


def _consts():
    half = 64
    inv_freq = np.power(np.float32(10000.0), -np.arange(half, dtype=np.float32) * np.float32(2.0 / 128)).astype(np.float32)
    ang = np.arange(S, dtype=np.float32)[:, None] * inv_freq[None, :]
    cos = np.cos(ang).astype(np.float32).T
    sin = np.sin(ang).astype(np.float32).T
    rope = np.concatenate([np.concatenate([cos, cos], 0), np.concatenate([sin, sin], 0)], axis=1)
    cf = np.zeros((128, 1024), np.float32)
    cf[:, 0:128] = np.eye(128, dtype=np.float32)
    p = np.arange(128)
    for i in range(16):
        blk = i // 2
        for n in range(8):
            cf[:, 128 + i * 8 + n] = 0.0 if n < blk else -1.0e30
            cf[:, 256 + i * 8 + n] = 0.0 if n == blk else NEG
    cf[:, 384:512] = 1.0 / 2048.0
    cf[:, 512:640] = (p[:, None] < p[None, :]).astype(np.float32)
    cf[:, 640:768] = 1.0
    cf[:, 768:800] = (np.arange(32) * CAP).astype(np.float32)[None, :]
    cf[:, 800] = p.astype(np.float32)
    cb = np.zeros((128, 2688), np.float32)
    for d in range(64):
        cb[d + 64, d] = -1.0
        cb[d, 64 + d] = 1.0
    cb[:, 128:256] = np.eye(128)
    cb[:, 256:384] = np.where(p[:, None] <= p[None, :], 0.0, NEG)
    cb[:, 384:512] = 1.0
    k = np.arange(S)
    for n in range(8):
        cb[n, 640:2688] = (k // 256 == n).astype(np.float32)
    sinit = np.zeros((NS, 4), np.int32)
    sinit[:, 1] = OOBROW
    return rope, cf, cb.astype(ml_dtypes.bfloat16), sinit


def _prep(inp):
    f = lambda a: np.asarray(a, dtype=np.float32)
    w_in = f(inp["w_in"])[0]
    b_in = f(inp["b_in"])[0]
    sh = {}
    sh["wA"] = _st_blocks(w_in[:, 0:4096])
    sh["wV"] = _st_blocks(w_in[:, 4096:6144], 256)
    sh["wG"] = _st_blocks(w_in[:, 6144:14336])
    sh["bq"] = np.ascontiguousarray(b_in[0:4096].reshape(32, 128).T)
    sh["bv"] = np.ascontiguousarray(b_in[4096:6144].reshape(1, 2048))
    sh["bG"] = np.ascontiguousarray(b_in[6144:14336].reshape(64, 128).T)
    sh["wo"] = _st_blocks(f(inp["w_o_attn"])[0])
    sh["wp"] = _st_blocks(f(inp["w_pw2"])[0])
    sh["wout"] = _st_blocks(f(inp["w_out"])[0], 512)
    cvec = np.zeros((128, 16, 36), np.float32)
    cvec[:, :, 0:31] = f(inp["w_dw"])[0].T.reshape(16, 128, 31).transpose(1, 0, 2)
    for i, nm in enumerate(["b_dw", "conv_ln_g", "conv_ln_b", "b_pw2"]):
        cvec[:, :, 31 + i] = f(inp[nm])[0].reshape(16, 128).T
    sh["cvec"] = cvec
    sh["lnv"] = np.ascontiguousarray(np.stack([f(inp["ln1_g"])[0], f(inp["ln1_b"])[0], f(inp["ln2_g"])[0], f(inp["ln2_b"])[0]]))
    wr = np.concatenate([f(inp["w_rg"])[0]] + [f(inp["w_re"])[0, g] for g in range(4)], axis=1)
    sh["wr"] = np.ascontiguousarray(wr.reshape(16, 128, 36).transpose(1, 0, 2))
    sh["br"] = np.concatenate([f(inp["b_rg"])[0], f(inp["b_re"])[0].reshape(-1)]).reshape(1, 36).astype(np.float32)
    w1 = f(inp["w1"])[0]
    w3 = f(inp["w3"])[0]
    w2 = f(inp["w2"])[0]
    sh["w1"] = np.ascontiguousarray(w1.reshape(NE, 16, 128, 4, 256).transpose(0, 3, 2, 1, 4))
    sh["w3"] = np.ascontiguousarray(w3.reshape(NE, 16, 128, 4, 256).transpose(0, 3, 2, 1, 4))
    sh["w2"] = np.ascontiguousarray(w2.reshape(NE, 8, 128, 4, 512).transpose(0, 3, 2, 1, 4))
    rope, cf, cb, sinit = _consts()
    sh["rope"], sh["cf"], sh["cb"], sh["sinit"] = rope, cf, cb, sinit
    sh["onesn"] = np.full((128, 128), 1.0 / 2048.0, np.float32)
    return sh


def kernel(**inputs):
    x = np.asarray(inputs["x"], dtype=np.float32)
    shared = _prep(inputs)
    nc = build("ABCDE", dbg=False)
    in_maps = []
    for b in range(8):
        m = dict(shared)
        m["x"] = np.ascontiguousarray(x[b])
        m["xT"] = np.ascontiguousarray(x[b].T)
        in_maps.append(m)
    res = run_bass_kernel_spmd(nc, in_maps, core_ids=list(range(8)))
    return np.stack([np.asarray(r["out"], dtype=np.float32) for r in res.results], axis=0)
```

```python
import numpy as np
import ml_dtypes
import concourse.bass as bass
import concourse.mybir as mybir
from concourse.bass_utils import run_bass_kernel_spmd

F32 = mybir.dt.float32
F32R = mybir.dt.float32r
BF16 = mybir.dt.bfloat16
I32 = mybir.dt.int32
AF = mybir.ActivationFunctionType
OP = mybir.AluOpType
AX = mybir.AxisListType

S = 2048
D = 2048
NH = 16
NE = 32
FF = 1024
CAP = 256
NS = NE * CAP
LN_EPS = 1e-5
ALPHA = 2.0 ** 0.25
SCALE = 128.0 ** -0.5
NEG = -30000.0
OOBROW = 2 * S + 100


class T:
    __slots__ = ("ap", "w", "r")

    def __init__(self, ap=None):
        self.ap = ap
        self.w = {}
        self.r = {}


class Eng:
    def __init__(self, name, strict_self):
        self.name = name
        self.key = name
        self.insts = []
        self.count = 0
        self.seen = {}
        self.strict = strict_self
        self.ring_vals = None
        self.ring_pos = 0


class Sched:
    def __init__(self, nc, es, nring_sp=12, nring_pool=8):
        self.nc = nc
        self.es = es
        self.nrot = 0
        self.e = {
            "pe": Eng("pe", False),
            "act": Eng("act", True),
            "dve": Eng("dve", True),
            "pool": Eng("pool", True),
            "sp": Eng("sp", False),
        }
        self.h = {"pe": nc.tensor, "act": nc.scalar, "dve": nc.vector, "pool": nc.gpsimd, "sp": nc.sync}
        self.e["sp"].ring_vals = [0] * nring_sp
        self.e["pool"].ring_vals = [0] * nring_pool
        self.sems = {}
        for en in ("pe", "act", "dve", "pool", "sp"):
            self.sems[en] = es.enter_context(nc.semaphore(f"s_{en}"))
        for qn in ("sp", "pool"):
            for idx in range(len(self.e[qn].ring_vals)):
                self.sems[(qn, idx)] = es.enter_context(nc.semaphore(f"r_{qn}{idx}"))

    def _waits(self, eng, reads, writes, extra=()):
        deps = {}

        def add(k, v):
            if deps.get(k, 0) < v:
                deps[k] = v

        for t in reads:
            for k, v in t.w.items():
                add(k, v)
        for t in writes:
            for k, v in t.w.items():
                add(k, v)
            for k, v in t.r.items():
                add(k, v)
        for k, v in extra:
            add(k, v)
        for k, v in deps.items():
            if k == eng.key and not eng.strict:
                continue
            if eng.seen.get(k, 0) < v:
                eng.seen[k] = v
                self.h[eng.name].wait_ge(self.sems[k], v)

    def op(self, en, fn, reads=(), writes=()):
        eng = self.e[en]
        self._waits(eng, reads, writes)
        eng.count += 1
        v = eng.count
        fn(self.h[en]).then_inc(self.sems[eng.key], 1)
        for t in writes:
            t.w = {eng.key: v}
            t.r = {}
        for t in reads:
            t.r[eng.key] = v

    def dma(self, qn, fn, reads=(), writes=()):
        eng = self.e[qn]
        idx = eng.ring_pos % len(eng.ring_vals)
        eng.ring_pos += 1
        key = (qn, idx)
        prev = eng.ring_vals[idx]
        self._waits(eng, reads, writes, extra=((key, prev),) if prev else ())
        val = prev + 16
        eng.ring_vals[idx] = val
        fn(self.h[qn]).then_inc(self.sems[key], 16)
        for t in writes:
            t.w = {key: val}
            t.r = {}
        for t in reads:
            t.r[key] = val

    def fence(self):
        for en, eng in self.e.items():
            for fn_, f in self.e.items():
                if fn_ != en and f.count and eng.seen.get(f.key, 0) < f.count:
                    eng.seen[f.key] = f.count
                    self.h[en].wait_ge(self.sems[f.key], f.count)
            for qn in ("sp", "pool"):
                for idx, v in enumerate(self.e[qn].ring_vals):
                    if v and eng.seen.get((qn, idx), 0) < v:
                        eng.seen[(qn, idx)] = v
                        self.h[en].wait_ge(self.sems[(qn, idx)], v)
        for en, eng in self.e.items():
            if eng.count > 1500:
                eng.seen[eng.key] = eng.count
                self.nrot += 1
                eng.key = (en, "ep", self.nrot)
                self.sems[eng.key] = self.es.enter_context(self.nc.semaphore(f"s_{en}_{self.nrot}"))
                eng.count = 0

    def finish(self):
        for qn in ("sp", "pool"):
            eng = self.e[qn]
            for idx, v in enumerate(eng.ring_vals):
                if v and eng.seen.get((qn, idx), 0) < v:
                    self.h[qn].wait_ge(self.sems[(qn, idx)], v)


import itertools
_uid = itertools.count()


class Ring:
    def __init__(self, tiles):
        self.t = tiles
        self.i = 0

    def next(self):
        t = self.t[self.i % len(self.t)]
        self.i += 1
        return t


def r32(ap):
    return ap.bitcast(F32R)


def build(phases="ABCDE", dbg=False):
    nc = bass.Bass("TRN2", target_bir_lowering=False)
    nc.dge_precook = False
    from contextlib import ExitStack
    es = ExitStack()
    sc = Sched(nc, es)
    reg_ns = nc.gpsimd.to_reg(NS - 1)
    reg_y = nc.gpsimd.to_reg(2 * S - 1)

    def din(name, shape, dt):
        return nc.dram_tensor(name, list(shape), dt, kind="ExternalInput").ap()

    def dscr(name, shape, dt):
        kind = "ExternalOutput" if dbg else "Internal"
        return nc.dram_tensor(name, list(shape), dt, kind=kind).ap()

    xT_d = din("xT", [D, S], F32R)
    x_d = din("x", [S, D], F32)
    wA_d = din("wA", [32, 128, 16, 128], F32R)
    wV_d = din("wV", [8, 128, 16, 256], F32R)
    wG_d = din("wG", [64, 128, 16, 128], F32R)
    bq_d = din("bq", [128, 32], F32)
    bv_d = din("bv", [1, 2048], F32)
    bG_d = din("bG", [128, 64], F32)
    wo_d = din("wo", [16, 128, 16, 128], F32R)
    wp_d = din("wp", [16, 128, 16, 128], F32R)
    wout_d = din("wout", [4, 128, 16, 512], F32R)
    cvec_d = din("cvec", [128, 16, 36], F32)
    lnv_d = din("lnv", [4, 2048], F32)
    wr_d = din("wr", [128, 16, 36], F32)
    br_d = din("br", [1, 36], F32)
    ned = NE if "D" in phases else 1
    w1_d = din("w1", [ned, 4, 128, 16, 256], F32R)
    w3_d = din("w3", [ned, 4, 128, 16, 256], F32R)
    w2_d = din("w2", [ned, 4, 128, 8, 512], F32R)
    rope_d = din("rope", [128, 4096], F32)
    cf_d = din("cf", [128, 1024], F32)
    on_d = din("onesn", [128, 128], F32R)
    cb_d = din("cb", [128, 2688], BF16)
    sinit_d = din("sinit", [NS, 4], I32)
    out_d = nc.dram_tensor("out", [S, D], F32, kind="ExternalOutput").ap()

    qk_s = dscr("qk_s", [32, 128, S], BF16)
    v_s = dscr("v_s", [S, D], BF16)
    ot_s = dscr("ot_s", [16, 128, S], F32R)
    h_s = dscr("h_s", [S, D], F32)
    y_s = dscr("y_s", [2 * S, D], F32)
    stab_s = dscr("stab_s", [NS, 4], I32)
    T_qk = [T() for _ in range(32)]
    T_v = [T() for _ in range(8)]
    T_ot = [T() for _ in range(16)]
    T_h = [T() for _ in range(16)]
    T_y = T()
    T_stab = T()

    def sb(name, shape, dt):
        return es.enter_context(nc.sbuf_tensor(f"s{next(_uid)}_" + name, list(shape), dt))

    psum = [es.enter_context(nc.psum_tensor(f"ps{i}", [128, 512], F32)) for i in range(8)]
    PS = [T(p) for p in psum]

    cf = sb("cf", [128, 1024], F32)
    cbt = sb("cb", [128, 2688], BF16)
    T_cf, T_cb = T(cf), T(cbt)
    sc.dma("sp", lambda h: h.dma_start(out=cf[:], in_=cf_d), writes=[T_cf])
    sc.dma("sp", lambda h: h.dma_start(out=cbt[:], in_=cb_d), writes=[T_cb])
    onesn_t = sb("onesn", [128, 128], F32R)
    sc.dma("sp", lambda h: h.dma_start(out=onesn_t[:], in_=on_d), writes=[T_cf])
    ident32 = cf[:, 0:128]
    pastbias = cf[:, 128:256]
    notown = cf[:, 256:384]
    onesn = onesn_t[:, :]
    U32 = cf[:, 512:640]
    ones32 = cf[:, 640:768]
    slotbase = cf[:, 768:800]
    iota_p = cf[:, 800:801]
    rotT = cbt[:, 0:128]
    identb = cbt[:, 128:256]
    tri = cbt[:, 256:384]
    onesb = cbt[:, 384:512]
    blkind = cbt[:, 640:2688]

    if "A" in phases:
        with ExitStack() as ea:
            def sba(name, shape, dt):
                return ea.enter_context(nc.sbuf_tensor(f"s{next(_uid)}_" + name, list(shape), dt))
            xT = sba("xT", [128, 16, S], F32R)
            T_xT = [T() for _ in range(4)]
            for tc in range(4):
                sc.dma("sp", lambda h, tc=tc: h.dma_start(
                    out=xT[:, :, tc * 512:(tc + 1) * 512],
                    in_=xT_d.rearrange("(kc p) t -> p kc t", p=128)[:, :, tc * 512:(tc + 1) * 512]),
                    writes=[T_xT[tc]])
            with ExitStack() as e1:
                def sb1(name, shape, dt):
                    return e1.enter_context(nc.sbuf_tensor(f"s{next(_uid)}_" + name, list(shape), dt))
                rope = sb1("rope", [128, 4096], F32)
                T_rope = T()
                sc.dma("sp", lambda h: h.dma_start(out=rope[:], in_=rope_d), writes=[T_rope])
                bq = sb1("bq", [128, 32], F32)
                T_bq = T()
                sc.dma("sp", lambda h: h.dma_start(out=bq[:], in_=bq_d), writes=[T_bq])
                wring = Ring([T(sb1(f"wA{i}", [128, 16, 128], F32R)) for i in range(4)])
                qbr = Ring([T(sb1(f"qb{i}", [128, 512], BF16)) for i in range(3)])
                t1r = Ring([T(sb1(f"t1{i}", [128, 512], F32)) for i in range(2)])
                t2r = Ring([T(sb1(f"t2{i}", [128, 512], F32)) for i in range(2)])
                str_ = Ring([T(sb1(f"st{i}", [128, S], BF16)) for i in range(2)])
                psA = Ring(PS[0:3])
                psR = Ring(PS[3:5])
                def rope_tail(nb, tc, stg, qb, last):
                    ps2 = psR.next()
                    sc.op("pe", lambda h: h.matmul(ps2.ap[:, :], lhsT=rotT, rhs=qb.ap[:, :], start=True, stop=True),
                          reads=[qb, T_cb], writes=[ps2])
                    t1 = t1r.next()
                    t2 = t2r.next()
                    sc.op("pool", lambda h: h.tensor_tensor(
                        out=t1.ap[:, :], in0=qb.ap[:, :], in1=rope[:, tc * 512:(tc + 1) * 512], op=OP.mult),
                        reads=[qb, T_rope], writes=[t1])
                    sc.op("dve", lambda h: h.tensor_tensor(
                        out=t2.ap[:, :], in0=ps2.ap[:, :], in1=rope[:, 2048 + tc * 512:2048 + (tc + 1) * 512],
                        op=OP.mult), reads=[ps2, T_rope], writes=[t2])
                    sc.op("dve", lambda h: h.tensor_tensor(
                        out=stg.ap[:, tc * 512:(tc + 1) * 512], in0=t1.ap[:, :], in1=t2.ap[:, :], op=OP.add),
                        reads=[t1, t2], writes=[stg])
                    if last:
                        sc.dma("pool", lambda h: h.dma_start(out=qk_s[nb], in_=stg.ap[:, :]), reads=[stg], writes=[T_qk[nb]])

                pend = None
                for nb in range(32):
                    W = wring.next()
                    sc.dma("sp", lambda h, W=W, nb=nb: h.dma_start(out=W.ap[:], in_=wA_d[nb]), writes=[W])
                    stg = str_.next()
                    for tc in range(4):
                        ps = psA.next()
                        for kc in range(16):
                            sc.op("pe", lambda h, ps=ps, W=W, kc=kc, tc=tc: h.matmul(
                                ps.ap[:, :], lhsT=r32(W.ap[:, kc, :]), rhs=r32(xT[:, kc, tc * 512:(tc + 1) * 512]),
                                start=(kc == 0), stop=(kc == 15)),
                                reads=[W, T_xT[tc]], writes=[ps])
                        qb = qbr.next()
                        sc.op("act", lambda h, qb=qb, ps=ps, nb=nb: h.activation(
                            out=qb.ap[:, :], in_=ps.ap[:, :], func=AF.Identity, bias=bq[:, nb:nb + 1], scale=1.0),
                            reads=[ps, T_bq], writes=[qb])
                        if pend is not None:
                            rope_tail(*pend)
                        pend = (nb, tc, stg, qb, tc == 3)
                rope_tail(*pend)
            sc.fence()
            with ExitStack() as e2:
                def sb2(name, shape, dt):
                    return e2.enter_context(nc.sbuf_tensor(f"s{next(_uid)}_" + name, list(shape), dt))
                bv = sb2("bv", [128, 2048], F32)
                T_bv = T()
                sc.dma("sp", lambda h: h.dma_start(out=bv[:], in_=bv_d.partition_broadcast(128)), writes=[T_bv])
                wvr = Ring([T(sb2(f"wV{i}", [128, 16, 256], F32R)) for i in range(2)])
                vst = Ring([T(sb2(f"vst{i}", [128, 16, 256], BF16)) for i in range(2)])
                psA = Ring(PS[0:4])
                for vb in range(8):
                    W = wvr.next()
                    sc.dma("sp", lambda h, W=W, vb=vb: h.dma_start(out=W.ap[:], in_=wV_d[vb]), writes=[W])
                    stg = vst.next()
                    for tt in range(16):
                        ps = psA.next()
                        for kc in range(16):
                            sc.op("pe", lambda h, ps=ps, W=W, kc=kc, tt=tt: h.matmul(
                                ps.ap[:, 0:256], lhsT=r32(xT[:, kc, tt * 128:(tt + 1) * 128]), rhs=r32(W.ap[:, kc, :]),
                                start=(kc == 0), stop=(kc == 15)),
                                reads=[W, T_xT[tt // 4]], writes=[ps])
                        sc.op("dve", lambda h, stg=stg, ps=ps, tt=tt, vb=vb: h.tensor_tensor(
                            out=stg.ap[:, tt, :], in0=ps.ap[:, 0:256], in1=bv[:, vb * 256:(vb + 1) * 256], op=OP.add),
                            reads=[ps, T_bv], writes=[stg])
                    sc.dma("pool", lambda h, stg=stg, vb=vb: h.dma_start(
                        out=v_s.rearrange("(tt p) c -> p tt c", p=128)[:, :, vb * 256:(vb + 1) * 256],
                        in_=stg.ap[:, :, :]), reads=[stg], writes=[T_v[vb]])

            sc.fence()
        sc.fence()
    if "B" in phases:
        with ExitStack() as eb:
            def sbb(name, shape, dt):
                return eb.enter_context(nc.sbuf_tensor(f"s{next(_uid)}_" + name, list(shape), dt))
            zt = T(sbb("zt", [128, 2048], F32))
            sc.op("pool", lambda h: h.memset(zt.ap[:, :], 0.0), writes=[zt])

            def zero_fill(hd):
                for i in (2 * hd, 2 * hd + 1):
                    sc.dma("sp", lambda h, i=i: h.dma_start(out=y_s[i * 128:(i + 1) * 128, :], in_=zt.ap[:, :]),
                           reads=[zt], writes=[T()])
            qr = Ring([T(sbb(f"q{i}", [128, S], BF16)) for i in range(2)])
            kr = Ring([T(sbb(f"k{i}", [128, S], BF16)) for i in range(2)])
            vr = Ring([T(sbb(f"v{i}", [128, 16, 132], BF16)) for i in range(2)])
            for t in vr.t:
                sc.op("pool", lambda h, t=t: h.memset(t.ap[:, :, 128:132], 1.0), writes=[t])
            ks32 = T(sbb("ks32", [128, 8], F32))
            ksb = T(sbb("ksb", [128, 8], BF16))
            gm = T(sbb("gm", [128, 128], F32))
            top8 = T(sbb("top8", [128, 128], F32))
            lt = T(sbb("lt", [128, 128], F32))
            Bpr = Ring([T(sbb(f"Bpad{i}", [128, 16, 128], F32)) for i in range(2)])
            for t in Bpr.t:
                sc.op("pool", lambda h, t=t: h.memset(t.ap[:, :, :], 0.0), writes=[t])
            BTr = Ring([T(sbb(f"BT{i}", [128, 512], BF16)) for i in range(2)])
            PT = [[T() for _ in range(16)] for _ in range(2)]
            PTbuf = [sbb(f"PT{i}", [128, 16, 512], BF16) for i in range(2)]
            rinvr = Ring([T(sbb(f"rinv{i}", [128, 512], F32)) for i in range(2)])
            OTr = Ring([T(sbb(f"OTst{i}", [128, S], F32R)) for i in range(2)])
            psST = Ring(PS[0:4])
            psO = Ring(PS[4:5])
            psOT = Ring(PS[5:6])
            psG = PS[6]
            psBT = PS[7]
            def prep_load(hd):
                qT, kT, V = qr.next(), kr.next(), vr.next()
                sc.dma("sp", lambda h: h.dma_start(out=qT.ap[:, :], in_=qk_s[hd]), reads=[T_qk[hd]], writes=[qT])
                sc.dma("sp", lambda h: h.dma_start(out=kT.ap[:, :], in_=qk_s[16 + hd]), reads=[T_qk[16 + hd]], writes=[kT])
                sc.dma("sp", lambda h: h.dma_start(
                    out=V.ap[:, :, 0:128],
                    in_=v_s.rearrange("(tt p) c -> p tt c", p=128)[:, :, hd * 128:(hd + 1) * 128]),
                    reads=[T_v[hd // 2]], writes=[V])
                zero_fill(hd)
                return qT, kT, V

            def prep(hd, loaded):
                qT, kT, V = loaded
                Bpad = Bpr.next()
                sc.op("dve", lambda h: h.tensor_reduce(
                    out=ks32.ap[:, :], in_=kT.ap[:, :].rearrange("p (n k) -> p n k", k=256), axis=AX.X, op=OP.add),
                    reads=[kT], writes=[ks32])
                sc.op("dve", lambda h: h.tensor_copy(out=ksb.ap[:, :], in_=ks32.ap[:, :]), reads=[ks32], writes=[ksb])
                for i in range(16):
                    sc.op("pe", lambda h, i=i: h.matmul(
                        psG.ap[:, i * 8:(i + 1) * 8], lhsT=qT.ap[:, i * 128:(i + 1) * 128], rhs=ksb.ap[:, :],
                        start=True, stop=True), reads=[qT, ksb], writes=[psG])
                sc.op("dve", lambda h: h.tensor_tensor(out=gm.ap[:, :], in0=psG.ap[:, 0:128], in1=pastbias, op=OP.add),
                      reads=[psG, T_cf], writes=[gm])
                for i in range(16):
                    sc.op("dve", lambda h, i=i: h.max(out=top8.ap[:, i * 8:(i + 1) * 8], in_=gm.ap[:, i * 8:(i + 1) * 8]),
                          reads=[gm], writes=[top8])
                sc.op("dve", lambda h: h.tensor_tensor(
                    out=lt.ap[:, :].rearrange("p (i n) -> p i n", n=8),
                    in0=gm.ap[:, :].rearrange("p (i n) -> p i n", n=8),
                    in1=top8.ap[:, :].rearrange("p (i n) -> p i n", n=8)[:, :, 2:3].to_broadcast([128, 16, 8]),
                    op=OP.is_lt), reads=[gm, top8], writes=[lt])
                sc.op("dve", lambda h: h.tensor_tensor(
                    out=Bpad.ap[:, :, 0:8],
                    in0=lt.ap[:, :].rearrange("p (i n) -> p i n", n=8),
                    in1=notown.rearrange("p (i n) -> p i n", n=8), op=OP.mult),
                    reads=[lt, T_cf], writes=[Bpad])
                return qT, kT, V, OTr.next(), Bpad

            def stage1(ctx, c, par):
                qT, kT, V, OTst, Bpad = ctx
                for ii in range(4):
                    sc.op("pe", lambda h, ii=ii: h.transpose(
                        out=psBT.ap[:, ii * 128:(ii + 1) * 128], in_=Bpad.ap[:, 4 * c + ii, :], identity=ident32),
                        reads=[Bpad, T_cf], writes=[psBT])
                BT = BTr.next()
                sc.op("act", lambda h: h.copy(out=BT.ap[:, :], in_=psBT.ap[:, :]), reads=[psBT], writes=[BT])
                for j in range(4 * c + 4):
                    q0 = max(512 * c, 128 * j)
                    off = q0 - 512 * c
                    diag = j >= 4 * c
                    st = psST.next()
                    sc.op("pe", lambda h, st=st, j=j, q0=q0, off=off: h.matmul(
                        st.ap[:, off:512], lhsT=kT.ap[:, j * 128:(j + 1) * 128], rhs=qT.ap[:, q0:512 * c + 512],
                        start=True, stop=False), reads=[kT, qT], writes=[st])
                    sc.op("pe", lambda h, st=st, j=j, off=off, diag=diag: h.matmul(
                        st.ap[:, off:512], lhsT=blkind[:, j * 128:(j + 1) * 128], rhs=BT.ap[:, off:512],
                        start=False, stop=(not diag)), reads=[BT, T_cb], writes=[st])
                    if diag:
                        sc.op("pe", lambda h, st=st, off=off: h.matmul(
                            st.ap[:, off:off + 128], lhsT=identb, rhs=tri, start=False, stop=True,
                            skip_group_check=True), reads=[T_cb], writes=[st])
                    sc.op("act", lambda h, st=st, j=j, off=off: h.activation(
                        out=PTbuf[par][:, j, off:512], in_=st.ap[:, off:512], func=AF.Exp, scale=SCALE),
                        reads=[st], writes=[PT[par][j]])

            def stage2(ctx, c, par, hd):
                qT, kT, V, OTst, Bpad = ctx
                otp = psO.next()
                rsp = psOT.next()
                nj = 4 * c + 4
                for j in range(nj):
                    off = max(512 * c, 128 * j) - 512 * c
                    sc.op("pe", lambda h, j=j, off=off: h.matmul(
                        otp.ap[:, off:512], lhsT=V.ap[:, j, 0:128], rhs=PTbuf[par][:, j, off:512],
                        start=(j == 0), stop=(j == nj - 1), skip_group_check=True), reads=[PT[par][j], V], writes=[otp])
                    sc.op("pe", lambda h, j=j, off=off: h.matmul(
                        rsp.ap[:, off:512], lhsT=onesb, rhs=PTbuf[par][:, j, off:512],
                        start=(j == 0), stop=(j == nj - 1), skip_group_check=True), reads=[PT[par][j], T_cb], writes=[rsp])
                rinv = rinvr.next()
                sc.op("dve", lambda h: h.reciprocal(out=rinv.ap[:, :], in_=rsp.ap[:, :]), reads=[rsp], writes=[rinv])
                sc.op("dve", lambda h: h.tensor_tensor(
                    out=OTst.ap[:, c * 512:(c + 1) * 512], in0=otp.ap[:, :], in1=rinv.ap[:, :], op=OP.mult),
                    reads=[otp, rinv], writes=[OTst])
                if c == 3:
                    sc.dma("pool", lambda h: h.dma_start(out=ot_s[hd], in_=OTst.ap[:, :]), reads=[OTst], writes=[T_ot[hd]])

            items = [(hd, c) for hd in range(NH) for c in range(4)]
            ctxs = {}
            prev = None
            ctxs[0] = prep(0, prep_load(0))
            loaded = None
            for k, (hd, c) in enumerate(items):
                stage1(ctxs[hd], c, k % 2)
                if prev is not None:
                    stage2(ctxs[prev[0]], prev[1], (k - 1) % 2, prev[0])
                if c == 0 and hd + 1 < NH:
                    loaded = prep_load(hd + 1)
                if c == 1 and hd + 1 < NH:
                    ctxs[hd + 1] = prep(hd + 1, loaded)
                prev = (hd, c)
            stage2(ctxs[prev[0]], prev[1], (len(items) - 1) % 2, prev[0])
        sc.fence()
    if "C" in phases:
        with ExitStack() as ec:
            def sbc(name, shape, dt):
                return ec.enter_context(nc.sbuf_tensor(f"s{next(_uid)}_" + name, list(shape), dt))
            cvec = sbc("cvec", [128, 16, 36], F32)
            bG = sbc("bG", [128, 64], F32)
            wr = sbc("wr", [128, 16, 36], F32)
            brt = sbc("br", [128, 36], F32)
            T_cc = T()
            sc.dma("sp", lambda h: h.dma_start(out=cvec[:], in_=cvec_d), writes=[T_cc])
            sc.dma("sp", lambda h: h.dma_start(out=bG[:], in_=bG_d), writes=[T_cc])
            sc.dma("sp", lambda h: h.dma_start(out=wr[:], in_=wr_d), writes=[T_cc])
            sc.dma("sp", lambda h: h.dma_start(out=brt[:], in_=br_d.partition_broadcast(128)), writes=[T_cc])
            sc.dma("pool", lambda h: h.dma_start(out=stab_s, in_=sinit_d), writes=[T_stab])
            halo = T(sbc("halo", [128, 16, 32], F32))
            sc.op("pool", lambda h: h.memset(halo.ap[:, :, :], 0.0), writes=[halo])
            Acum = T(sbc("Acum", [128, 32], F32))
            sc.op("pool", lambda h: h.memset(Acum.ap[:, :], 0.0), writes=[Acum])
            mT = sbc("mT", [128, 16, 512], F32)
            T_mT = [T() for _ in range(16)]
            psA = Ring(PS[0:2])
            psB = Ring(PS[2:4])
            psS = [PS[4], PS[5]]
            psX = Ring(PS[6:8])

            def gemm_fm(wring, wsrc, ps, act, T_act):
                W = wring.next()
                sc.dma("sp", lambda h, W=W: h.dma_start(out=W.ap[:], in_=wsrc), writes=[W])
                for kc in range(16):
                    sc.op("pe", lambda h, W=W, kc=kc: h.matmul(
                        ps.ap[:, :], lhsT=r32(W.ap[:, kc, :]), rhs=r32(act[:, kc, :]),
                        start=(kc == 0), stop=(kc == 15)),
                        reads=[W, (T_act[kc * len(T_act) // 16] if isinstance(T_act, list) else T_act)], writes=[ps])

            for c in range(4):
                t0 = c * 512
                e12 = ExitStack()
                def sb12(name, shape, dt):
                    return e12.enter_context(nc.sbuf_tensor(f"s{next(_uid)}_" + name, list(shape), dt))
                xTc = sb12("xTc", [128, 16, 512], F32R)
                T_xTc = [T() for _ in range(4)]
                OTc = sb12("OTc", [128, 16, 512], F32R)
                T_OTc = T()
                wring = Ring([T(sb12(f"wC{i}", [128, 16, 128], F32R)) for i in range(3)])
                sgr = Ring([T(sb12(f"sg{i}", [128, 512], F32)) for i in range(2)])
                tmr = Ring([T(sb12(f"tm{i}", [128, 512], F32)) for i in range(2)])
                cT = sb12("cT", [128, 16, 512], F32)
                T_cT = [T() for _ in range(16)]
                hTr = Ring([T(sb12(f"hT{i}", [128, 544], F32)) for i in range(4)])
                dgr = Ring([T(sb12(f"dg{i}", [128, 16, 128], F32)) for i in range(2)])
                mean = T(sb12("mean", [128, 512], F32))
                rstd = T(sb12("rstd", [128, 512], F32))
                for kq in range(4):
                    sc.dma("sp", lambda h, kq=kq: h.dma_start(
                        out=xTc[:, 4 * kq:4 * kq + 4, :],
                        in_=xT_d.rearrange("(kc p) t -> p kc t", p=128)[:, 4 * kq:4 * kq + 4, t0:t0 + 512]),
                        writes=[T_xTc[kq]])
                sc.dma("sp", lambda h: h.dma_start(
                    out=OTc[:, :, :], in_=ot_s.rearrange("k p t -> p k t")[:, :, t0:t0 + 512]),
                    reads=T_ot, writes=[T_OTc])

                def conv_pe(cc, hT):
                    halves = []
                    for hh in range(2):
                        j0 = 16 * hh
                        n = 16 if hh == 0 else 15
                        dg = dgr.next()
                        sc.op("pool", lambda h, dg=dg, j0=j0, n=n: h.tensor_tensor(
                            out=r32(dg.ap[:, 0:n, :]), in0=ident32.unsqueeze(1).to_broadcast([128, n, 128]),
                            in1=cvec[:, cc, j0:j0 + n].unsqueeze(2).to_broadcast([128, n, 128]), op=OP.mult),
                            reads=[T_cf, T_cc], writes=[dg])
                        halves.append(dg)
                    ps = psX.next()
                    for j in range(31):
                        dg = halves[j // 16]
                        sc.op("pe", lambda h, j=j, dg=dg: h.matmul(
                            ps.ap[:, :], lhsT=r32(dg.ap[:, j % 16, :]), rhs=r32(hT.ap[:, 2 + j:514 + j]),
                            start=(j == 0), stop=(j == 30)), reads=[dg, hT], writes=[ps])
                    sc.op("act", lambda h: h.activation(
                        out=r32(cT[:, cc, :]), in_=ps.ap[:, :], func=AF.Identity, bias=cvec[:, cc, 31:32], scale=1.0),
                        reads=[ps, T_cc], writes=[T_cT[cc]])
                    sc.op("pool", lambda h: h.tensor_copy(out=halo.ap[:, cc, :], in_=hT.ap[:, 512:544]),
                          reads=[hT], writes=[halo])

                pend = None
                for cc in range(16):
                    hT = hTr.next()
                    sc.op("pool", lambda h, cc=cc, hT=hT: h.tensor_copy(out=r32(hT.ap[:, 0:32]), in_=halo.ap[:, cc, :]),
                          reads=[halo], writes=[hT])
                    pa, pg = psA.next(), psB.next()
                    gemm_fm(wring, wG_d[cc], pa, xTc, T_xTc)
                    gemm_fm(wring, wG_d[16 + cc], pg, xTc, T_xTc)
                    sg = sgr.next()
                    sc.op("act", lambda h, sg=sg, pg=pg, cc=cc: h.activation(
                        out=sg.ap[:, :], in_=pg.ap[:, :], func=AF.Sigmoid, bias=bG[:, 16 + cc:17 + cc], scale=1.0),
                        reads=[pg, T_cc], writes=[sg])
                    sc.op("dve", lambda h, sg=sg, pa=pa, cc=cc, hT=hT: h.scalar_tensor_tensor(
                        out=r32(hT.ap[:, 32:544]), in0=pa.ap[:, :], scalar=bG[:, cc:cc + 1], in1=sg.ap[:, :],
                        op0=OP.add, op1=OP.mult), reads=[pa, sg, T_cc, hT], writes=[hT])
                    if pend is not None:
                        conv_pe(*pend)
                    pend = (cc, hT)
                conv_pe(*pend)

                pm, pq = psS
                for cc in range(16):
                    sc.op("pe", lambda h, cc=cc: h.matmul(pm.ap[:, :], lhsT=r32(onesn), rhs=r32(cT[:, cc, :]),
                                                         start=(cc == 0), stop=(cc == 15)),
                          reads=[T_cT[cc], T_cf], writes=[pm])
                for cc in range(16):
                    sq = hTr.next()
                    sc.op("act", lambda h, sq=sq, cc=cc: h.activation(out=r32(sq.ap[:, 0:512]), in_=cT[:, cc, :], func=AF.Square),
                          reads=[T_cT[cc]], writes=[sq])
                    sc.op("pe", lambda h, sq=sq, cc=cc: h.matmul(pq.ap[:, :], lhsT=r32(onesn), rhs=r32(sq.ap[:, 0:512]),
                                                                start=(cc == 0), stop=(cc == 15)),
                          reads=[sq, T_cf], writes=[pq])
                sc.op("dve", lambda h: h.tensor_copy(out=mean.ap[:, :], in_=pm.ap[:, :]), reads=[pm], writes=[mean])
                sc.op("dve", lambda h: h.tensor_tensor(out=rstd.ap[:, :], in0=mean.ap[:, :], in1=mean.ap[:, :], op=OP.mult),
                      reads=[mean], writes=[rstd])
                sc.op("dve", lambda h: h.tensor_tensor(out=rstd.ap[:, :], in0=pq.ap[:, :], in1=rstd.ap[:, :], op=OP.subtract),
                      reads=[pq, rstd], writes=[rstd])
                sc.op("dve", lambda h: h.tensor_scalar(out=rstd.ap[:, :], in0=rstd.ap[:, :], scalar1=LN_EPS, scalar2=None,
                                                       op0=OP.add), reads=[rstd], writes=[rstd])
                sc.op("act", lambda h: h.activation(out=rstd.ap[:, :], in_=rstd.ap[:, :], func=AF.Sqrt),
                      reads=[rstd], writes=[rstd])
                sc.op("dve", lambda h: h.reciprocal(out=rstd.ap[:, :], in_=rstd.ap[:, :]), reads=[rstd], writes=[rstd])

                for j in range(16):
                    cc = j
                    en = "dve" if cc % 2 == 0 else "pool"
                    nt = hTr.next()
                    sc.op(en, lambda h, cc=cc, nt=nt: h.tensor_tensor(out=r32(nt.ap[:, 0:512]), in0=cT[:, cc, :], in1=mean.ap[:, :],
                                                                      op=OP.subtract), reads=[T_cT[cc], mean], writes=[nt])
                    sc.op(en, lambda h, cc=cc, nt=nt: h.tensor_tensor(out=r32(nt.ap[:, 0:512]), in0=nt.ap[:, 0:512], in1=rstd.ap[:, :],
                                                                      op=OP.mult), reads=[nt, rstd], writes=[nt])
                    sc.op("act", lambda h, cc=cc, nt=nt: h.activation(
                        out=r32(cT[:, cc, :]), in_=nt.ap[:, 0:512], func=AF.Silu, bias=cvec[:, cc, 33:34], scale=cvec[:, cc, 32:33]),
                        reads=[nt, T_cc], writes=[T_cT[cc]])
                    py, pg = psA.next(), psB.next()
                    gemm_fm(wring, wo_d[j], py, OTc, T_OTc)
                    gemm_fm(wring, wG_d[32 + j], pg, xTc, T_xTc)
                    sg = sgr.next()
                    sc.op("act", lambda h, sg=sg, pg=pg, j=j: h.activation(
                        out=sg.ap[:, :], in_=pg.ap[:, :], func=AF.Sigmoid, bias=bG[:, 32 + j:33 + j], scale=1.0),
                        reads=[pg, T_cc], writes=[sg])
                    sc.op("dve", lambda h, sg=sg, py=py, j=j: h.tensor_tensor(
                        out=r32(mT[:, j, :]), in0=py.ap[:, :], in1=sg.ap[:, :], op=OP.mult),
                        reads=[py, sg], writes=[T_mT[j]])
                for j in range(16):
                    py, pg = psA.next(), psB.next()
                    gemm_fm(wring, wp_d[j], py, cT, T_cT)
                    gemm_fm(wring, wG_d[48 + j], pg, xTc, T_xTc)
                    sg = sgr.next()
                    sc.op("act", lambda h, sg=sg, pg=pg, j=j: h.activation(
                        out=sg.ap[:, :], in_=pg.ap[:, :], func=AF.Sigmoid, bias=bG[:, 48 + j:49 + j], scale=1.0),
                        reads=[pg, T_cc], writes=[sg])
                    tm = tmr.next()
                    sc.op("dve", lambda h, sg=sg, py=py, j=j, tm=tm: h.scalar_tensor_tensor(
                        out=tm.ap[:, :], in0=py.ap[:, :], scalar=cvec[:, j, 34:35], in1=sg.ap[:, :],
                        op0=OP.add, op1=OP.mult), reads=[py, sg, T_cc], writes=[tm])
                    sc.op("pool", lambda h, tm=tm, j=j: h.tensor_tensor(
                        out=r32(mT[:, j, :]), in0=mT[:, j, :], in1=tm.ap[:, :], op=OP.add),
                        reads=[tm, T_mT[j]], writes=[T_mT[j]])
                e12.close()
                sc.fence()
                with ExitStack() as e3:
                    def sb3(name, shape, dt):
                        return e3.enter_context(nc.sbuf_tensor(f"s{next(_uid)}_" + name, list(shape), dt))
                    wor = Ring([T(sb3(f"wout{i}", [128, 16, 512], F32R)) for i in range(2)])
                    lng = sb3("lng", [128, 2048], F32)
                    lnb = sb3("lnb", [128, 2048], F32)
                    T_ln = T()
                    sc.dma("sp", lambda h: h.dma_start(out=lng[:], in_=lnv_d[0:1, :].partition_broadcast(128)), writes=[T_ln])
                    sc.dma("sp", lambda h: h.dma_start(out=lnb[:], in_=lnv_d[1:2, :].partition_broadcast(128)), writes=[T_ln])
                    xr = [T(sb3(f"xt{i}", [128, 2048], F32)) for i in range(4)]
                    zr = xr
                    for t in range(4):
                        sc.dma("sp", lambda h, t=t, t0=t0: h.dma_start(
                            out=xr[t].ap[:, :], in_=x_d[t0 + t * 128:t0 + (t + 1) * 128, :]), writes=[xr[t]])
                    for cb in range(4):
                        W = wor.next()
                        sc.dma("sp", lambda h, W=W, cb=cb: h.dma_start(out=W.ap[:], in_=wout_d[cb]), writes=[W])
                        for t in range(4):
                            ps = psA.next() if t % 2 == 0 else psB.next()
                            for kc in range(16):
                                sc.op("pe", lambda h, ps=ps, W=W, kc=kc, t=t: h.matmul(
                                    ps.ap[:, :], lhsT=r32(mT[:, kc, t * 128:(t + 1) * 128]), rhs=r32(W.ap[:, kc, :]),
                                    start=(kc == 0), stop=(kc == 15)), reads=[W, T_mT[kc]], writes=[ps])
                            sc.op("dve", lambda h, ps=ps, t=t, cb=cb: h.scalar_tensor_tensor(
                                out=zr[t].ap[:, cb * 512:(cb + 1) * 512], in0=xr[t].ap[:, cb * 512:(cb + 1) * 512],
                                scalar=ALPHA, in1=ps.ap[:, :], op0=OP.mult, op1=OP.add),
                                reads=[ps, xr[t]], writes=[zr[t]])
                    def mk_small(t):
                        d = {}
                        for nm, shp, dt in (("st6", [128, 4, 6], F32), ("mv", [128, 2], F32), ("rs", [128, 2], F32),
                                            ("lg", [128, 36], F32), ("sm", [128, 64], F32), ("oh", [128, 4], F32),
                                            ("esel", [128, 8], F32), ("t8", [128, 8], F32), ("mk", [128, 2, 8], F32),
                                            ("Ak", [128, 2, 32], F32), ("Asum", [128, 32], F32), ("val", [128, 32], F32),
                                            ("ov", [128, 32], F32), ("dst", [128, 2], F32), ("dsti", [128, 2], I32),
                                            ("pay", [128, 2, 4], I32), ("tokf", [128, 2], F32)):
                            d[nm] = T(sb3(f"{nm}{t}", shp, dt))
                        return d
                    small = [mk_small(t) for t in range(4)]
                    hTts = [T(sb3(f"hTt{i}", [128, 16, 128], F32)) for i in range(2)]

                    def tail1(t):
                        g = c * 4 + t
                        z = zr[t]
                        d = small[t]
                        st6, mv, rs, lg, sm, oh, esel, t8, mk, Ak, Asum = (d[k] for k in (
                            "st6", "mv", "rs", "lg", "sm", "oh", "esel", "t8", "mk", "Ak", "Asum"))
                        hTt = hTts[t % 2]
                        for q4 in range(4):
                            sc.op("dve", lambda h, q4=q4: h.bn_stats(out=st6.ap[:, q4, :], in_=z.ap[:, q4 * 512:(q4 + 1) * 512]),
                                  reads=[z], writes=[st6])
                        sc.op("dve", lambda h: h.bn_aggr(out=mv.ap[:, :], in_=st6.ap[:, :, :].rearrange("p a b -> p (a b)")),
                              reads=[st6], writes=[mv])
                        sc.op("dve", lambda h: h.tensor_scalar(out=rs.ap[:, 0:1], in0=mv.ap[:, 1:2], scalar1=LN_EPS, scalar2=None,
                                                               op0=OP.add), reads=[mv], writes=[rs])
                        yield
                        sc.op("act", lambda h: h.activation(out=rs.ap[:, 0:1], in_=rs.ap[:, 0:1], func=AF.Sqrt), reads=[rs], writes=[rs])
                        yield
                        sc.op("dve", lambda h: h.reciprocal(out=rs.ap[:, 0:1], in_=rs.ap[:, 0:1]), reads=[rs], writes=[rs])
                        sc.op("dve", lambda h: h.tensor_scalar(out=rs.ap[:, 1:2], in0=mv.ap[:, 0:1], scalar1=rs.ap[:, 0:1], scalar2=-1.0,
                                                               op0=OP.mult, op1=OP.mult), reads=[mv, rs], writes=[rs])
                        yield
                        sc.op("act", lambda h: h.activation(out=z.ap[:, :], in_=z.ap[:, :], func=AF.Identity,
                                                            scale=rs.ap[:, 0:1], bias=rs.ap[:, 1:2]), reads=[z, rs], writes=[z])
                        yield
                        sc.op("dve", lambda h: h.tensor_tensor(out=z.ap[:, :], in0=z.ap[:, :], in1=lng[:, :], op=OP.mult),
                              reads=[z, T_ln], writes=[z])
                        sc.op("dve", lambda h: h.tensor_tensor(out=z.ap[:, :], in0=z.ap[:, :], in1=lnb[:, :], op=OP.add),
                              reads=[z, T_ln], writes=[z])
                        sc.dma("pool", lambda h: h.dma_start(out=h_s[g * 128:(g + 1) * 128, :], in_=z.ap[:, :]),
                               reads=[z], writes=[T_h[g]])
                        yield
                        for k4 in range(4):
                            tp = psX.next()
                            for ii in range(4):
                                kc = k4 * 4 + ii
                                sc.op("pe", lambda h, tp=tp, ii=ii, kc=kc: h.transpose(
                                    out=tp.ap[:, ii * 128:(ii + 1) * 128], in_=z.ap[:, kc * 128:(kc + 1) * 128], identity=ident32),
                                    reads=[z, T_cf], writes=[tp])
                            sc.op("act", lambda h, tp=tp, k4=k4: h.copy(
                                out=hTt.ap[:, k4 * 4:(k4 + 1) * 4, :].rearrange("p a b -> p (a b)"), in_=tp.ap[:, :]),
                                reads=[tp], writes=[hTt])
                        pl = psS[0]
                        for kc in range(16):
                            sc.op("pe", lambda h, kc=kc: h.matmul(pl.ap[:, t * 64:t * 64 + 36], lhsT=hTt.ap[:, kc, :], rhs=wr[:, kc, :],
                                                                  start=(kc == 0), stop=(kc == 15)),
                                  reads=[hTt, T_cc], writes=[pl])
                        yield
                        sc.op("dve", lambda h: h.tensor_tensor(out=lg.ap[:, :], in0=pl.ap[:, t * 64:t * 64 + 36], in1=brt[:, :], op=OP.add),
                              reads=[pl, T_cc], writes=[lg])
                        sc.op("dve", lambda h: h.tensor_reduce(out=sm.ap[:, 0:1], in_=lg.ap[:, 0:4], axis=AX.X, op=OP.max),
                              reads=[lg], writes=[sm])
                        sc.op("dve", lambda h: h.tensor_scalar(out=oh.ap[:, :], in0=lg.ap[:, 0:4], scalar1=sm.ap[:, 0:1], scalar2=None,
                                                               op0=OP.is_equal), reads=[lg, sm], writes=[oh])
                        sc.op("dve", lambda h: h.tensor_scalar(out=sm.ap[:, 1:2], in0=sm.ap[:, 0:1], scalar1=-1.0, scalar2=None,
                                                               op0=OP.mult), reads=[sm], writes=[sm])
                        sc.op("dve", lambda h: h.tensor_scalar(out=esel.ap[:, :], in0=lg.ap[:, 4:12], scalar1=oh.ap[:, 0:1], scalar2=None,
                                                               op0=OP.mult), reads=[lg, oh], writes=[esel])
                        for gi in range(1, 4):
                            sc.op("dve", lambda h, gi=gi: h.scalar_tensor_tensor(
                                out=esel.ap[:, :], in0=lg.ap[:, 4 + 8 * gi:12 + 8 * gi], scalar=oh.ap[:, gi:gi + 1], in1=esel.ap[:, :],
                                op0=OP.mult, op1=OP.add), reads=[lg, oh, esel], writes=[esel])
                        sc.op("dve", lambda h: h.max(out=t8.ap[:, :], in_=esel.ap[:, :]), reads=[esel], writes=[t8])
                        for k in range(2):
                            sc.op("dve", lambda h, k=k: h.tensor_scalar(out=mk.ap[:, k, :], in0=esel.ap[:, :], scalar1=t8.ap[:, k:k + 1],
                                                                        scalar2=None, op0=OP.is_equal), reads=[esel, t8], writes=[mk])
                        sc.op("dve", lambda h: h.tensor_tensor(out=sm.ap[:, 4:5], in0=t8.ap[:, 1:2], in1=t8.ap[:, 0:1], op=OP.subtract),
                              reads=[t8], writes=[sm])
                        yield
                        sc.op("act", lambda h: h.activation(out=sm.ap[:, 10:14], in_=lg.ap[:, 0:4], func=AF.Exp, bias=sm.ap[:, 1:2],
                                                            scale=1.0), reads=[lg, sm], writes=[sm])
                        sc.op("act", lambda h: h.activation(out=sm.ap[:, 5:6], in_=sm.ap[:, 4:5], func=AF.Exp), reads=[sm], writes=[sm])
                        yield
                        sc.op("dve", lambda h: h.tensor_reduce(out=sm.ap[:, 2:3], in_=sm.ap[:, 10:14], axis=AX.X, op=OP.add),
                              reads=[sm], writes=[sm])
                        sc.op("dve", lambda h: h.reciprocal(out=sm.ap[:, 3:4], in_=sm.ap[:, 2:3]), reads=[sm], writes=[sm])
                        sc.op("dve", lambda h: h.tensor_scalar(out=sm.ap[:, 6:7], in0=sm.ap[:, 5:6], scalar1=1.0, scalar2=None, op0=OP.add),
                              reads=[sm], writes=[sm])
                        sc.op("dve", lambda h: h.reciprocal(out=sm.ap[:, 6:7], in_=sm.ap[:, 6:7]), reads=[sm], writes=[sm])
                        sc.op("dve", lambda h: h.tensor_tensor(out=sm.ap[:, 7:8], in0=sm.ap[:, 6:7], in1=sm.ap[:, 3:4], op=OP.mult),
                              reads=[sm], writes=[sm])
                        sc.op("dve", lambda h: h.tensor_tensor(out=sm.ap[:, 8:9], in0=sm.ap[:, 7:8], in1=sm.ap[:, 5:6], op=OP.mult),
                              reads=[sm], writes=[sm])
                        for k in range(2):
                            for gi in range(4):
                                sc.op("dve", lambda h, k=k, gi=gi: h.tensor_scalar(
                                    out=Ak.ap[:, k, gi * 8:(gi + 1) * 8], in0=mk.ap[:, k, :], scalar1=oh.ap[:, gi:gi + 1], scalar2=None,
                                    op0=OP.mult), reads=[mk, oh], writes=[Ak])
                        sc.op("dve", lambda h: h.tensor_tensor(out=Asum.ap[:, :], in0=Ak.ap[:, 0, :], in1=Ak.ap[:, 1, :], op=OP.add),
                              reads=[Ak], writes=[Asum])
                        yield

                    def tail2(t):
                        g = c * 4 + t
                        d = small[t]
                        sm, Ak, Asum, val, ov, dst, dsti, pay, tokf = (d[k] for k in (
                            "sm", "Ak", "Asum", "val", "ov", "dst", "dsti", "pay", "tokf"))
                        pp = psS[1]
                        sc.op("pe", lambda h: h.matmul(pp.ap[:, 0:32], lhsT=U32, rhs=Asum.ap[:, :], start=True, stop=False),
                              reads=[Asum, T_cf], writes=[pp])
                        sc.op("pe", lambda h: h.matmul(pp.ap[:, 0:32], lhsT=ones32, rhs=Acum.ap[:, :], start=False, stop=True),
                              reads=[Acum, T_cf], writes=[pp])
                        sc.op("dve", lambda h: h.tensor_scalar(out=ov.ap[:, :], in0=pp.ap[:, 0:32], scalar1=float(CAP), scalar2=1.0e6,
                                                               op0=OP.is_ge, op1=OP.mult), reads=[pp], writes=[ov])
                        sc.op("dve", lambda h: h.tensor_tensor(out=val.ap[:, :], in0=pp.ap[:, 0:32], in1=slotbase, op=OP.add),
                              reads=[pp, T_cf], writes=[val])
                        sc.op("dve", lambda h: h.tensor_tensor(out=Acum.ap[:, :], in0=Acum.ap[:, :], in1=Asum.ap[:, :], op=OP.add),
                              reads=[Acum, Asum], writes=[Acum])
                        sc.op("dve", lambda h: h.tensor_tensor(out=val.ap[:, :], in0=val.ap[:, :], in1=ov.ap[:, :], op=OP.add),
                              reads=[val, ov], writes=[val])
                        for k in range(2):
                            sc.op("dve", lambda h, k=k: h.tensor_tensor(out=Ak.ap[:, k, :], in0=Ak.ap[:, k, :], in1=val.ap[:, :], op=OP.mult),
                                  reads=[Ak, val], writes=[Ak])
                            sc.op("dve", lambda h, k=k: h.tensor_reduce(out=dst.ap[:, k:k + 1], in_=Ak.ap[:, k, :], axis=AX.X, op=OP.add),
                                  reads=[Ak], writes=[dst])
                        sc.op("dve", lambda h: h.tensor_copy(out=dsti.ap[:, :], in_=dst.ap[:, :]), reads=[dst], writes=[dsti])
                        sc.op("pool", lambda h: h.tensor_scalar(out=tokf.ap[:, 0:1], in0=iota_p, scalar1=float(g * 128), scalar2=None,
                                                                op0=OP.add), reads=[T_cf], writes=[tokf])
                        sc.op("pool", lambda h: h.tensor_scalar(out=tokf.ap[:, 1:2], in0=iota_p, scalar1=float(g * 128 + S), scalar2=None,
                                                                op0=OP.add), reads=[T_cf], writes=[tokf])
                        sc.op("pool", lambda h: h.memset(pay.ap[:, :, :], 0), writes=[pay])
                        sc.op("pool", lambda h: h.tensor_copy(out=pay.ap[:, 0, 0:1], in_=tokf.ap[:, 0:1]), reads=[tokf], writes=[pay])
                        sc.op("pool", lambda h: h.tensor_copy(out=pay.ap[:, 0, 1:2], in_=tokf.ap[:, 0:1]), reads=[tokf], writes=[pay])
                        sc.op("pool", lambda h: h.tensor_copy(out=pay.ap[:, 1, 0:1], in_=tokf.ap[:, 0:1]), reads=[tokf], writes=[pay])
                        sc.op("pool", lambda h: h.tensor_copy(out=pay.ap[:, 1, 1:2], in_=tokf.ap[:, 1:2]), reads=[tokf], writes=[pay])
                        for k in range(2):
                            sc.op("dve", lambda h, k=k: h.tensor_copy(out=pay.ap[:, k, 2:3].bitcast(F32), in_=sm.ap[:, 7 + k:8 + k]),
                                  reads=[sm], writes=[pay])
                        for k in range(2):
                            sc.dma("pool", lambda h, k=k: h.indirect_dma_start(
                                out=stab_s[:, :], out_offset=bass.IndirectOffsetOnAxis(ap=dsti.ap[:, k:k + 1], axis=0),
                                in_=pay.ap[:, k, :], in_offset=None, bounds_check=reg_ns, oob_is_err=False),
                                reads=[pay, dsti], writes=[T_stab])

                    gens = [tail1(t) for t in range(4)]
                    live = list(gens)
                    while live:
                        for gnr in list(live):
                            try:
                                next(gnr)
                            except StopIteration:
                                live.remove(gnr)
                    for t in range(4):
                        tail2(t)

                sc.fence()
        sc.fence()
    if "D" in phases:
        with ExitStack() as ed:
            def sbd(name, shape, dt):
                return ed.enter_context(nc.sbuf_tensor(f"s{next(_uid)}_" + name, list(shape), dt))
            str_ = Ring([T(sbd(f"stb{i}", [128, 2, 4], I32)) for i in range(3)])
            hgr = Ring([T(sbd(f"hg{i}", [128, 2048], F32)) for i in range(2)])
            hgTr = Ring([T(sbd(f"hgT{i}", [128, 16, 256], F32R)) for i in range(2)])
            wer = Ring([T(sbd(f"web{i}", [128, 4096], F32R)) for i in range(7)])
            sgr = Ring([T(sbd(f"sgd{i}", [128, 256], F32)) for i in range(2)])
            actT = sbd("actT", [128, 8, 256], F32R)
            T_act = [T() for _ in range(8)]
            your = Ring([T(sbd(f"yout{i}", [128, 2048], F32)) for i in range(2)])
            psT = Ring(PS[0:2])
            psa = Ring(PS[2:4])
            psb = Ring(PS[4:6])
            psy = Ring(PS[6:8])

            def gather(e):
                stb = str_.next()
                sc.dma("pool", lambda h: h.dma_start(
                    out=stb.ap[:, :, :], in_=stab_s[e * CAP:(e + 1) * CAP, :].rearrange("(s p) c -> p s c", p=128)),
                    reads=[T_stab], writes=[stb])
                hgs = []
                for s in range(2):
                    hg = hgr.next()
                    sc.dma("pool", lambda h, hg=hg, s=s: h.indirect_dma_start(
                        out=hg.ap[:, :], out_offset=None, in_=h_s[:, :],
                        in_offset=bass.IndirectOffsetOnAxis(ap=stb.ap[:, s, 0:1], axis=0)),
                        reads=[stb] + T_h, writes=[hg])
                    hgs.append(hg)
                return stb, hgs

            def transposes(hgs):
                hgT = hgTr.next()
                for s in range(2):
                    hg = hgs[s]
                    for k4 in range(4):
                        tp = psT.next()
                        for ii in range(4):
                            kc = k4 * 4 + ii
                            sc.op("pe", lambda h, tp=tp, ii=ii, kc=kc, hg=hg: h.transpose(
                                out=tp.ap[:, ii * 128:(ii + 1) * 128], in_=hg.ap[:, kc * 128:(kc + 1) * 128], identity=ident32),
                                reads=[hg, T_cf], writes=[tp])
                        if k4 % 2 == 0:
                            sc.op("dve", lambda h, tp=tp, k4=k4, s=s: h.tensor_copy(
                                out=hgT.ap[:, k4 * 4:(k4 + 1) * 4, s * 128:(s + 1) * 128],
                                in_=tp.ap[:, :].rearrange("p (a b) -> p a b", b=128)), reads=[tp], writes=[hgT])
                        else:
                            sc.op("act", lambda h, tp=tp, k4=k4, s=s: h.copy(
                                out=hgT.ap[:, k4 * 4:(k4 + 1) * 4, s * 128:(s + 1) * 128],
                                in_=tp.ap[:, :].rearrange("p (a b) -> p a b", b=128)), reads=[tp], writes=[hgT])
                return hgT

            nxt = gather(0)
            hgT_next = transposes(nxt[1])
            for e in range(NE):
                stb, hgs = nxt
                hgT = hgT_next
                if e + 1 < NE:
                    nxt = gather(e + 1)
                for fb in range(4):
                    W1, W3 = wer.next(), wer.next()
                    sc.dma("sp", lambda h, W1=W1, fb=fb: h.dma_start(
                        out=W1.ap[:, :], in_=w1_d[e, fb].rearrange("p k c -> p (k c)")), writes=[W1])
                    sc.dma("sp", lambda h, W3=W3, fb=fb: h.dma_start(
                        out=W3.ap[:, :], in_=w3_d[e, fb].rearrange("p k c -> p (k c)")), writes=[W3])
                    for half in range(2):
                        fc = fb * 2 + half
                        pa, pb = psa.next(), psb.next()
                        for kc in range(16):
                            sc.op("pe", lambda h, pa=pa, W1=W1, kc=kc, half=half: h.matmul(
                                pa.ap[:, 0:256], lhsT=r32(W1.ap[:, kc * 256 + half * 128:kc * 256 + (half + 1) * 128]), rhs=r32(hgT.ap[:, kc, :]),
                                start=(kc == 0), stop=(kc == 15)), reads=[W1, hgT], writes=[pa])
                        for kc in range(16):
                            sc.op("pe", lambda h, pb=pb, W3=W3, kc=kc, half=half: h.matmul(
                                pb.ap[:, 0:256], lhsT=r32(W3.ap[:, kc * 256 + half * 128:kc * 256 + (half + 1) * 128]), rhs=r32(hgT.ap[:, kc, :]),
                                start=(kc == 0), stop=(kc == 15)), reads=[W3, hgT], writes=[pb])
                        sg = sgr.next()
                        sc.op("act", lambda h, sg=sg, pa=pa: h.activation(out=sg.ap[:, :], in_=pa.ap[:, 0:256], func=AF.Silu),
                              reads=[pa], writes=[sg])
                        sc.op("dve", lambda h, sg=sg, pb=pb, fc=fc: h.tensor_tensor(
                            out=actT[:, fc, :], in0=pb.ap[:, 0:256], in1=sg.ap[:, :], op=OP.mult),
                            reads=[pb, sg], writes=[T_act[fc]])
                if e + 1 < NE:
                    hgT_next = transposes(nxt[1])
                youts = [your.next(), your.next()]
                for cb in range(4):
                    W2 = wer.next()
                    sc.dma("sp", lambda h, W2=W2, cb=cb: h.dma_start(
                        out=W2.ap[:, :], in_=w2_d[e, cb].rearrange("p k c -> p (k c)")), writes=[W2])
                    for s in range(2):
                        py = psy.next()
                        for fc in range(8):
                            sc.op("pe", lambda h, py=py, W2=W2, fc=fc, s=s: h.matmul(
                                py.ap[:, :], lhsT=r32(actT[:, fc, s * 128:(s + 1) * 128]), rhs=r32(W2.ap[:, fc * 512:(fc + 1) * 512]),
                                start=(fc == 0), stop=(fc == 7)), reads=[W2, T_act[fc]], writes=[py])
                        yo = youts[s]
                        sc.op("act", lambda h, py=py, yo=yo, cb=cb, s=s: h.activation(
                            out=yo.ap[:, cb * 512:(cb + 1) * 512], in_=py.ap[:, :], func=AF.Copy,
                            scale=stb.ap[:, s, 2:3].bitcast(F32)), reads=[py, stb], writes=[yo])
                for s in range(2):
                    yo = youts[s]
                    sc.dma("pool", lambda h, yo=yo, s=s: h.indirect_dma_start(
                        out=y_s[:, :], out_offset=bass.IndirectOffsetOnAxis(ap=stb.ap[:, s, 1:2], axis=0),
                        in_=yo.ap[:, :], in_offset=None, bounds_check=reg_y, oob_is_err=False),
                        reads=[yo, stb], writes=[T_y])
                if e % 8 == 7 and e != NE - 1:
                    sc.fence()
        sc.fence()
    if "E" in phases:
        with ExitStack() as ee:
            def sbe(name, shape, dt):
                return ee.enter_context(nc.sbuf_tensor(f"s{next(_uid)}_" + name, list(shape), dt))
            lng = sbe("lng2", [128, 2048], F32)
            lnb = sbe("lnb2", [128, 2048], F32)
            T_ln = T()
            sc.dma("sp", lambda h: h.dma_start(out=lng[:], in_=lnv_d[2:3, :].partition_broadcast(128)), writes=[T_ln])
            sc.dma("sp", lambda h: h.dma_start(out=lnb[:], in_=lnv_d[3:4, :].partition_broadcast(128)), writes=[T_ln])
            hr = Ring([T(sbe(f"he{i}", [128, 2048], F32)) for i in range(4)])
            y0r = Ring([T(sbe(f"y0e{i}", [128, 2048], F32)) for i in range(3)])
            y1r = Ring([T(sbe(f"y1e{i}", [128, 2048], F32)) for i in range(3)])
            smr = Ring([(T(sbe(f"st6e{i}", [128, 4, 6], F32)), T(sbe(f"mve{i}", [128, 2], F32)), T(sbe(f"rse{i}", [128, 2], F32)))
                        for i in range(4)])
            T_out = T()

            def etile(g):
                ht, y0, y1 = hr.next(), y0r.next(), y1r.next()
                st6, mv, rs = smr.next()
                sc.dma("sp", lambda h: h.dma_start(out=ht.ap[:, :], in_=h_s[g * 128:(g + 1) * 128, :]), reads=[T_h[g]], writes=[ht])
                sc.dma("sp", lambda h: h.dma_start(out=y0.ap[:, :], in_=y_s[g * 128:(g + 1) * 128, :]), reads=[T_y], writes=[y0])
                sc.dma("sp", lambda h: h.dma_start(out=y1.ap[:, :], in_=y_s[S + g * 128:S + (g + 1) * 128, :]), reads=[T_y], writes=[y1])
                yield
                sc.op("dve", lambda h: h.scalar_tensor_tensor(
                    out=ht.ap[:, :], in0=ht.ap[:, :], scalar=ALPHA, in1=y0.ap[:, :], op0=OP.mult, op1=OP.add),
                    reads=[ht, y0], writes=[ht])
                sc.op("dve", lambda h: h.tensor_tensor(out=ht.ap[:, :], in0=ht.ap[:, :], in1=y1.ap[:, :], op=OP.add),
                      reads=[ht, y1], writes=[ht])
                for q4 in range(4):
                    sc.op("dve", lambda h, q4=q4: h.bn_stats(out=st6.ap[:, q4, :], in_=ht.ap[:, q4 * 512:(q4 + 1) * 512]),
                          reads=[ht], writes=[st6])
                sc.op("dve", lambda h: h.bn_aggr(out=mv.ap[:, :], in_=st6.ap[:, :, :].rearrange("p a b -> p (a b)")),
                      reads=[st6], writes=[mv])
                sc.op("dve", lambda h: h.tensor_scalar(out=rs.ap[:, 0:1], in0=mv.ap[:, 1:2], scalar1=LN_EPS, scalar2=None, op0=OP.add),
                      reads=[mv], writes=[rs])
                yield
                sc.op("act", lambda h: h.activation(out=rs.ap[:, 0:1], in_=rs.ap[:, 0:1], func=AF.Sqrt), reads=[rs], writes=[rs])
                yield
                sc.op("dve", lambda h: h.reciprocal(out=rs.ap[:, 0:1], in_=rs.ap[:, 0:1]), reads=[rs], writes=[rs])
                sc.op("dve", lambda h: h.tensor_scalar(out=rs.ap[:, 1:2], in0=mv.ap[:, 0:1], scalar1=rs.ap[:, 0:1], scalar2=-1.0,
                                                       op0=OP.mult, op1=OP.mult), reads=[mv, rs], writes=[rs])
                yield
                sc.op("act", lambda h: h.activation(out=ht.ap[:, :], in_=ht.ap[:, :], func=AF.Identity,
                                                    scale=rs.ap[:, 0:1], bias=rs.ap[:, 1:2]), reads=[ht, rs], writes=[ht])
                yield
                sc.op("dve", lambda h: h.tensor_tensor(out=ht.ap[:, :], in0=ht.ap[:, :], in1=lng[:, :], op=OP.mult),
                      reads=[ht, T_ln], writes=[ht])
                yield
                sc.op("pool", lambda h: h.tensor_tensor(out=ht.ap[:, :], in0=ht.ap[:, :], in1=lnb[:, :], op=OP.add),
                      reads=[ht, T_ln], writes=[ht])
                sc.dma("pool", lambda h: h.dma_start(out=out_d[g * 128:(g + 1) * 128, :], in_=ht.ap[:, :]),
                       reads=[ht], writes=[T()])
                yield

            pending = list(range(16))
            live = []
            while pending or live:
                if pending and len(live) < 3:
                    live.append(etile(pending.pop(0)))
                for gnr in list(live):
                    try:
                        next(gnr)
                    except StopIteration:
                        live.remove(gnr)
        sc.fence()
        sc.fence()
    sc.finish()
    es.close()
    return nc


def _st_blocks(w, cols=128):
    K, N = w.shape
    return np.ascontiguousarray(w.reshape(K // 128, 128, N // cols, cols).transpose(2, 1, 0, 3))


def _consts():
    half = 64
    inv_freq = np.power(np.float32(10000.0), -np.arange(half, dtype=np.float32) * np.float32(2.0 / 128)).astype(np.float32)
    ang = np.arange(S, dtype=np.float32)[:, None] * inv_freq[None, :]
    cos = np.cos(ang).astype(np.float32).T
    sin = np.sin(ang).astype(np.float32).T
    rope = np.concatenate([np.concatenate([cos, cos], 0), np.concatenate([sin, sin], 0)], axis=1)
    cf = np.zeros((128, 1024), np.float32)
    cf[:, 0:128] = np.eye(128, dtype=np.float32)
    p = np.arange(128)
    for i in range(16):
        blk = i // 2
        for n in range(8):
            cf[:, 128 + i * 8 + n] = 0.0 if n < blk else -1.0e30
            cf[:, 256 + i * 8 + n] = 0.0 if n == blk else NEG
    cf[:, 384:512] = 1.0 / 2048.0
    cf[:, 512:640] = (p[:, None] < p[None, :]).astype(np.float32)
    cf[:, 640:768] = 1.0
    cf[:, 768:800] = (np.arange(32) * CAP).astype(np.float32)[None, :]
    cf[:, 800] = p.astype(np.float32)
    cb = np.zeros((128, 2688), np.float32)
    for d in range(64):
        cb[d + 64, d] = -1.0
        cb[d, 64 + d] = 1.0
    cb[:, 128:256] = np.eye(128)
    cb[:, 256:384] = np.where(p[:, None] <= p[None, :], 0.0, NEG)
    cb[:, 384:512] = 1.0
    k = np.arange(S)
    for n in range(8):
        cb[n, 640:2688] = (k // 256 == n).astype(np.float32)
    sinit = np.zeros((NS, 4), np.int32)
    sinit[:, 1] = OOBROW
    return rope, cf, cb.astype(ml_dtypes.bfloat16), sinit


def _prep(inp):
    f = lambda a: np.asarray(a, dtype=np.float32)
    w_in = f(inp["w_in"])[0]
    b_in = f(inp["b_in"])[0]
    sh = {}
    sh["wA"] = _st_blocks(w_in[:, 0:4096])
    sh["wV"] = _st_blocks(w_in[:, 4096:6144], 256)
    sh["wG"] = _st_blocks(w_in[:, 6144:14336])
    sh["bq"] = np.ascontiguousarray(b_in[0:4096].reshape(32, 128).T)
    sh["bv"] = np.ascontiguousarray(b_in[4096:6144].reshape(1, 2048))
    sh["bG"] = np.ascontiguousarray(b_in[6144:14336].reshape(64, 128).T)
    sh["wo"] = _st_blocks(f(inp["w_o_attn"])[0])
    sh["wp"] = _st_blocks(f(inp["w_pw2"])[0])
    sh["wout"] = _st_blocks(f(inp["w_out"])[0], 512)
    cvec = np.zeros((128, 16, 36), np.float32)
    cvec[:, :, 0:31] = f(inp["w_dw"])[0].T.reshape(16, 128, 31).transpose(1, 0, 2)
    for i, nm in enumerate(["b_dw", "conv_ln_g", "conv_ln_b", "b_pw2"]):
        cvec[:, :, 31 + i] = f(inp[nm])[0].reshape(16, 128).T
    sh["cvec"] = cvec
    sh["lnv"] = np.ascontiguousarray(np.stack([f(inp["ln1_g"])[0], f(inp["ln1_b"])[0], f(inp["ln2_g"])[0], f(inp["ln2_b"])[0]]))
    wr = np.concatenate([f(inp["w_rg"])[0]] + [f(inp["w_re"])[0, g] for g in range(4)], axis=1)
    sh["wr"] = np.ascontiguousarray(wr.reshape(16, 128, 36).transpose(1, 0, 2))
    sh["br"] = np.concatenate([f(inp["b_rg"])[0], f(inp["b_re"])[0].reshape(-1)]).reshape(1, 36).astype(np.float32)
    w1 = f(inp["w1"])[0]
    w3 = f(inp["w3"])[0]
    w2 = f(inp["w2"])[0]
    sh["w1"] = np.ascontiguousarray(w1.reshape(NE, 16, 128, 4, 256).transpose(0, 3, 2, 1, 4))
    sh["w3"] = np.ascontiguousarray(w3.reshape(NE, 16, 128, 4, 256).transpose(0, 3, 2, 1, 4))
    sh["w2"] = np.ascontiguousarray(w2.reshape(NE, 8, 128, 4, 512).transpose(0, 3, 2, 1, 4))
    rope, cf, cb, sinit = _consts()
    sh["rope"], sh["cf"], sh["cb"], sh["sinit"] = rope, cf, cb, sinit
    sh["onesn"] = np.full((128, 128), 1.0 / 2048.0, np.float32)
    return sh


def kernel(**inputs):
    x = np.asarray(inputs["x"], dtype=np.float32)
    shared = _prep(inputs)
    nc = build("ABCDE", dbg=False)
    in_maps = []
    for b in range(8):
        m = dict(shared)
        m["x"] = np.ascontiguousarray(x[b])
        m["xT"] = np.ascontiguousarray(x[b].T)
        in_maps.append(m)
    res = run_bass_kernel_spmd(nc, in_maps, core_ids=list(range(8)))
    return np.stack([np.asarray(r["out"], dtype=np.float32) for r in res.results], axis=0)
```

```python
import numpy as np
import ml_dtypes
import concourse.bass as bass
import concourse.mybir as mybir
from concourse.bass_utils import run_bass_kernel_spmd

F32 = mybir.dt.float32
F32R = mybir.dt.float32r
BF16 = mybir.dt.bfloat16
I32 = mybir.dt.int32
AF = mybir.ActivationFunctionType
OP = mybir.AluOpType
AX = mybir.AxisListType

S = 2048
D = 2048
NH = 16
NE = 32
FF = 1024
CAP = 256
NS = NE * CAP
LN_EPS = 1e-5
ALPHA = 2.0 ** 0.25
SCALE = 128.0 ** -0.5
NEG = -30000.0
OOBROW = 2 * S + 100


class T:
    __slots__ = ("ap", "w", "r")

    def __init__(self, ap=None):
        self.ap = ap
        self.w = {}
        self.r = {}


class Eng:
    def __init__(self, name, strict_self):
        self.name = name
        self.key = name
        self.insts = []
        self.count = 0
        self.seen = {}
        self.strict = strict_self
        self.ring_vals = None
        self.ring_pos = 0


class Sched:
    def __init__(self, nc, es, nring_sp=12, nring_pool=8):
        self.nc = nc
        self.es = es
        self.nrot = 0
        self.e = {
            "pe": Eng("pe", False),
            "act": Eng("act", True),
            "dve": Eng("dve", True),
            "pool": Eng("pool", True),
            "sp": Eng("sp", False),
        }
        self.h = {"pe": nc.tensor, "act": nc.scalar, "dve": nc.vector, "pool": nc.gpsimd, "sp": nc.sync}
        self.e["sp"].ring_vals = [0] * nring_sp
        self.e["pool"].ring_vals = [0] * nring_pool
        self.sems = {}
        for en in ("pe", "act", "dve", "pool", "sp"):
            self.sems[en] = es.enter_context(nc.semaphore(f"s_{en}"))
        for qn in ("sp", "pool"):
            for idx in range(len(self.e[qn].ring_vals)):
                self.sems[(qn, idx)] = es.enter_context(nc.semaphore(f"r_{qn}{idx}"))

    def _waits(self, eng, reads, writes, extra=()):
        deps = {}

        def add(k, v):
            if deps.get(k, 0) < v:
                deps[k] = v

        for t in reads:
            for k, v in t.w.items():
                add(k, v)
        for t in writes:
            for k, v in t.w.items():
                add(k, v)
            for k, v in t.r.items():
                add(k, v)
        for k, v in extra:
            add(k, v)
        for k, v in deps.items():
            if k == eng.key and not eng.strict:
                continue
            if eng.seen.get(k, 0) < v:
                eng.seen[k] = v
                self.h[eng.name].wait_ge(self.sems[k], v)

    def op(self, en, fn, reads=(), writes=()):
        eng = self.e[en]
        self._waits(eng, reads, writes)
        if eng.count >= 6000:
            eng.seen[eng.key] = eng.count
            self.nrot += 1
            eng.key = (en, "ep", self.nrot)
            self.sems[eng.key] = self.es.enter_context(self.nc.semaphore(f"s_{en}_{self.nrot}"))
            eng.count = 0
        eng.count += 1
        v = eng.count
        fn(self.h[en]).then_inc(self.sems[eng.key], 1)
        for t in writes:
            t.w = {eng.key: v}
            t.r = {}
        for t in reads:
            t.r[eng.key] = v

    def dma(self, qn, fn, reads=(), writes=()):
        eng = self.e[qn]
        idx = eng.ring_pos % len(eng.ring_vals)
        eng.ring_pos += 1
        key = (qn, idx)
        prev = eng.ring_vals[idx]
        self._waits(eng, reads, writes, extra=((key, prev),) if prev else ())
        val = prev + 16
        eng.ring_vals[idx] = val
        fn(self.h[qn]).then_inc(self.sems[key], 16)
        for t in writes:
            t.w = {key: val}
            t.r = {}
        for t in reads:
            t.r[key] = val

    def fence(self):
        for en, eng in self.e.items():
            for fn_, f in self.e.items():
                if fn_ != en and f.count and eng.seen.get(f.key, 0) < f.count:
                    eng.seen[f.key] = f.count
                    self.h[en].wait_ge(self.sems[f.key], f.count)
            for qn in ("sp", "pool"):
                for idx, v in enumerate(self.e[qn].ring_vals):
                    if v and eng.seen.get((qn, idx), 0) < v:
                        eng.seen[(qn, idx)] = v
                        self.h[en].wait_ge(self.sems[(qn, idx)], v)
        for en, eng in self.e.items():
            if eng.count > 1500:
                eng.seen[eng.key] = eng.count
                self.nrot += 1
                eng.key = (en, "ep", self.nrot)
                self.sems[eng.key] = self.es.enter_context(self.nc.semaphore(f"s_{en}_{self.nrot}"))
                eng.count = 0

    def finish(self):
        for qn in ("sp", "pool"):
            eng = self.e[qn]
            for idx, v in enumerate(eng.ring_vals):
                if v and eng.seen.get((qn, idx), 0) < v:
                    self.h[qn].wait_ge(self.sems[(qn, idx)], v)


import itertools
_uid = itertools.count()


class Ring:
    def __init__(self, tiles):
        self.t = tiles
        self.i = 0

    def next(self):
        t = self.t[self.i % len(self.t)]
        self.i += 1
        return t


def r32(ap):
    return ap.bitcast(F32R)


def build(phases="ABCDE", dbg=False):
    nc = bass.Bass("TRN2", target_bir_lowering=False)
    nc.dge_precook = False
    from contextlib import ExitStack
    es = ExitStack()
    sc = Sched(nc, es)
    reg_ns = nc.gpsimd.to_reg(NS - 1)
    reg_y = nc.gpsimd.to_reg(2 * S - 1)

    def din(name, shape, dt):
        return nc.dram_tensor(name, list(shape), dt, kind="ExternalInput").ap()

    def dscr(name, shape, dt):
        kind = "ExternalOutput" if dbg else "Internal"
        return nc.dram_tensor(name, list(shape), dt, kind=kind).ap()

    xT_d = din("xT", [D, S], F32R)
    x_d = din("x", [S, D], F32)
    wA_d = din("wA", [32, 128, 16, 128], F32R)
    wV_d = din("wV", [8, 128, 16, 256], F32R)
    wG_d = din("wG", [64, 128, 16, 128], F32R)
    bq_d = din("bq", [128, 32], F32)
    bv_d = din("bv", [1, 2048], F32)
    bG_d = din("bG", [128, 64], F32)
    wo_d = din("wo", [16, 128, 16, 128], F32R)
    wp_d = din("wp", [16, 128, 16, 128], F32R)
    wout_d = din("wout", [4, 128, 16, 512], F32R)
    cvec_d = din("cvec", [128, 16, 36], F32)
    lnv_d = din("lnv", [4, 2048], F32)
    wr_d = din("wr", [128, 16, 36], F32)
    br_d = din("br", [1, 36], F32)
    ned = NE if "D" in phases else 1
    w1_d = din("w1", [ned, 4, 128, 16, 256], F32R)
    w3_d = din("w3", [ned, 4, 128, 16, 256], F32R)
    w2_d = din("w2", [ned, 4, 128, 8, 512], F32R)
    rope_d = din("rope", [128, 4096], F32)
    cf_d = din("cf", [128, 1024], F32)
    on_d = din("onesn", [128, 128], F32R)
    cb_d = din("cb", [128, 2688], BF16)
    sinit_d = din("sinit", [NS, 4], I32)
    out_d = nc.dram_tensor("out", [S, D], F32, kind="ExternalOutput").ap()

    qk_s = dscr("qk_s", [32, 128, S], BF16)
    v_s = dscr("v_s", [S, D], BF16)
    ot_s = dscr("ot_s", [16, 128, S], F32R)
    h_s = dscr("h_s", [S, D], F32)
    y_s = dscr("y_s", [2 * S, D], F32)
    stab_s = dscr("stab_s", [NS, 4], I32)
    T_qk = [T() for _ in range(32)]
    T_v = [T() for _ in range(8)]
    T_ot = [T() for _ in range(16)]
    T_h = [T() for _ in range(16)]
    T_y = T()
    T_stab = T()

    def sb(name, shape, dt):
        return es.enter_context(nc.sbuf_tensor(f"s{next(_uid)}_" + name, list(shape), dt))

    psum = [es.enter_context(nc.psum_tensor(f"ps{i}", [128, 512], F32)) for i in range(8)]
    PS = [T(p) for p in psum]

    cf = sb("cf", [128, 1024], F32)
    cbt = sb("cb", [128, 2688], BF16)
    T_cf, T_cb = T(cf), T(cbt)
    sc.dma("sp", lambda h: h.dma_start(out=cf[:], in_=cf_d), writes=[T_cf])
    sc.dma("sp", lambda h: h.dma_start(out=cbt[:], in_=cb_d), writes=[T_cb])
    onesn_t = sb("onesn", [128, 128], F32R)
    sc.dma("sp", lambda h: h.dma_start(out=onesn_t[:], in_=on_d), writes=[T_cf])
    ident32 = cf[:, 0:128]
    pastbias = cf[:, 128:256]
    notown = cf[:, 256:384]
    onesn = onesn_t[:, :]
    U32 = cf[:, 512:640]
    ones32 = cf[:, 640:768]
    slotbase = cf[:, 768:800]
    iota_p = cf[:, 800:801]
    rotT = cbt[:, 0:128]
    identb = cbt[:, 128:256]
    tri = cbt[:, 256:384]
    onesb = cbt[:, 384:512]
    blkind = cbt[:, 640:2688]

    if "A" in phases:
        with ExitStack() as ea:
            def sba(name, shape, dt):
                return ea.enter_context(nc.sbuf_tensor(f"s{next(_uid)}_" + name, list(shape), dt))
            xT = sba("xT", [128, 16, S], F32R)
            T_xT = [T() for _ in range(4)]
            for tc in range(4):
                sc.dma("sp", lambda h, tc=tc: h.dma_start(
                    out=xT[:, :, tc * 512:(tc + 1) * 512],
                    in_=xT_d.rearrange("(kc p) t -> p kc t", p=128)[:, :, tc * 512:(tc + 1) * 512]),
                    writes=[T_xT[tc]])
            with ExitStack() as e1:
                def sb1(name, shape, dt):
                    return e1.enter_context(nc.sbuf_tensor(f"s{next(_uid)}_" + name, list(shape), dt))
                rope = sb1("rope", [128, 4096], F32)
                T_rope = T()
                sc.dma("sp", lambda h: h.dma_start(out=rope[:], in_=rope_d), writes=[T_rope])
                bq = sb1("bq", [128, 32], F32)
                T_bq = T()
                sc.dma("sp", lambda h: h.dma_start(out=bq[:], in_=bq_d), writes=[T_bq])
                wring = Ring([T(sb1(f"wA{i}", [128, 16, 128], F32R)) for i in range(4)])
                qbr = Ring([T(sb1(f"qb{i}", [128, 512], BF16)) for i in range(3)])
                t1r = Ring([T(sb1(f"t1{i}", [128, 512], F32)) for i in range(2)])
                t2r = Ring([T(sb1(f"t2{i}", [128, 512], F32)) for i in range(2)])
                str_ = Ring([T(sb1(f"st{i}", [128, S], BF16)) for i in range(2)])
                psA = Ring(PS[0:3])
                psR = Ring(PS[3:5])
                def rope_tail(nb, tc, stg, qb, last):
                    ps2 = psR.next()
                    sc.op("pe", lambda h: h.matmul(ps2.ap[:, :], lhsT=rotT, rhs=qb.ap[:, :], start=True, stop=True),
                          reads=[qb, T_cb], writes=[ps2])
                    t1 = t1r.next()
                    t2 = t2r.next()
                    sc.op("pool", lambda h: h.tensor_tensor(
                        out=t1.ap[:, :], in0=qb.ap[:, :], in1=rope[:, tc * 512:(tc + 1) * 512], op=OP.mult),
                        reads=[qb, T_rope], writes=[t1])
                    sc.op("dve", lambda h: h.tensor_tensor(
                        out=t2.ap[:, :], in0=ps2.ap[:, :], in1=rope[:, 2048 + tc * 512:2048 + (tc + 1) * 512],
                        op=OP.mult), reads=[ps2, T_rope], writes=[t2])
                    sc.op("dve", lambda h: h.tensor_tensor(
                        out=stg.ap[:, tc * 512:(tc + 1) * 512], in0=t1.ap[:, :], in1=t2.ap[:, :], op=OP.add),
                        reads=[t1, t2], writes=[stg])
                    if last:
                        sc.dma("pool", lambda h: h.dma_start(out=qk_s[nb], in_=stg.ap[:, :]), reads=[stg], writes=[T_qk[nb]])

                pend = None
                for nb in range(32):
                    W = wring.next()
                    sc.dma("sp", lambda h, W=W, nb=nb: h.dma_start(out=W.ap[:], in_=wA_d[nb]), writes=[W])
                    stg = str_.next()
                    for tc in range(4):
                        ps = psA.next()
                        for kc in range(16):
                            sc.op("pe", lambda h, ps=ps, W=W, kc=kc, tc=tc: h.matmul(
                                ps.ap[:, :], lhsT=r32(W.ap[:, kc, :]), rhs=r32(xT[:, kc, tc * 512:(tc + 1) * 512]),
                                start=(kc == 0), stop=(kc == 15)),
                                reads=[W, T_xT[tc]], writes=[ps])
                        qb = qbr.next()
                        sc.op("act", lambda h, qb=qb, ps=ps, nb=nb: h.activation(
                            out=qb.ap[:, :], in_=ps.ap[:, :], func=AF.Identity, bias=bq[:, nb:nb + 1], scale=1.0),
                            reads=[ps, T_bq], writes=[qb])
                        if pend is not None:
                            rope_tail(*pend)
                        pend = (nb, tc, stg, qb, tc == 3)
                rope_tail(*pend)
            sc.fence()
            with ExitStack() as e2:
                def sb2(name, shape, dt):
                    return e2.enter_context(nc.sbuf_tensor(f"s{next(_uid)}_" + name, list(shape), dt))
                bv = sb2("bv", [128, 2048], F32)
                T_bv = T()
                sc.dma("sp", lambda h: h.dma_start(out=bv[:], in_=bv_d.partition_broadcast(128)), writes=[T_bv])
                wvr = Ring([T(sb2(f"wV{i}", [128, 16, 256], F32R)) for i in range(2)])
                vst = Ring([T(sb2(f"vst{i}", [128, 16, 256], BF16)) for i in range(2)])
                psA = Ring(PS[0:4])
                for vb in range(8):
                    W = wvr.next()
                    sc.dma("sp", lambda h, W=W, vb=vb: h.dma_start(out=W.ap[:], in_=wV_d[vb]), writes=[W])
                    stg = vst.next()
                    for tt in range(16):
                        ps = psA.next()
                        for kc in range(16):
                            sc.op("pe", lambda h, ps=ps, W=W, kc=kc, tt=tt: h.matmul(
                                ps.ap[:, 0:256], lhsT=r32(xT[:, kc, tt * 128:(tt + 1) * 128]), rhs=r32(W.ap[:, kc, :]),
                                start=(kc == 0), stop=(kc == 15)),
                                reads=[W, T_xT[tt // 4]], writes=[ps])
                        sc.op("dve", lambda h, stg=stg, ps=ps, tt=tt, vb=vb: h.tensor_tensor(
                            out=stg.ap[:, tt, :], in0=ps.ap[:, 0:256], in1=bv[:, vb * 256:(vb + 1) * 256], op=OP.add),
                            reads=[ps, T_bv], writes=[stg])
                    sc.dma("pool", lambda h, stg=stg, vb=vb: h.dma_start(
                        out=v_s.rearrange("(tt p) c -> p tt c", p=128)[:, :, vb * 256:(vb + 1) * 256],
                        in_=stg.ap[:, :, :]), reads=[stg], writes=[T_v[vb]])

            sc.fence()
        sc.fence()
    if "B" in phases:
        with ExitStack() as eb:
            def sbb(name, shape, dt):
                return eb.enter_context(nc.sbuf_tensor(f"s{next(_uid)}_" + name, list(shape), dt))
            zt = T(sbb("zt", [128, 2048], F32))
            sc.op("pool", lambda h: h.memset(zt.ap[:, :], 0.0), writes=[zt])

            def zero_fill(hd):
                for i in (2 * hd, 2 * hd + 1):
                    sc.dma("sp", lambda h, i=i: h.dma_start(out=y_s[i * 128:(i + 1) * 128, :], in_=zt.ap[:, :]),
                           reads=[zt], writes=[T()])
            qr = Ring([T(sbb(f"q{i}", [128, S], BF16)) for i in range(2)])
            kr = Ring([T(sbb(f"k{i}", [128, S], BF16)) for i in range(2)])
            vr = Ring([T(sbb(f"v{i}", [128, 16, 132], BF16)) for i in range(2)])
            for t in vr.t:
                sc.op("pool", lambda h, t=t: h.memset(t.ap[:, :, 128:132], 1.0), writes=[t])
            ks32 = T(sbb("ks32", [128, 8], F32))
            ksb = T(sbb("ksb", [128, 8], BF16))
            gm = T(sbb("gm", [128, 128], F32))
            top8 = T(sbb("top8", [128, 128], F32))
            lt = T(sbb("lt", [128, 128], F32))
            Bpr = Ring([T(sbb(f"Bpad{i}", [128, 16, 128], F32)) for i in range(2)])
            for t in Bpr.t:
                sc.op("pool", lambda h, t=t: h.memset(t.ap[:, :, :], 0.0), writes=[t])
            BTr = Ring([T(sbb(f"BT{i}", [128, 512], BF16)) for i in range(2)])
            PT = [[T() for _ in range(16)] for _ in range(2)]
            PTbuf = [sbb(f"PT{i}", [128, 16, 512], BF16) for i in range(2)]
            rinvr = Ring([T(sbb(f"rinv{i}", [128, 512], F32)) for i in range(2)])
            OTr = Ring([T(sbb(f"OTst{i}", [128, S], F32R)) for i in range(2)])
            psST = Ring(PS[0:4])
            psO = Ring(PS[4:5])
            psOT = Ring(PS[5:6])
            psG = PS[6]
            psBT = PS[7]
            def prep_load(hd):
                qT, kT, V = qr.next(), kr.next(), vr.next()
                sc.dma("sp", lambda h: h.dma_start(out=qT.ap[:, :], in_=qk_s[hd]), reads=[T_qk[hd]], writes=[qT])
                sc.dma("sp", lambda h: h.dma_start(out=kT.ap[:, :], in_=qk_s[16 + hd]), reads=[T_qk[16 + hd]], writes=[kT])
                sc.dma("sp", lambda h: h.dma_start(
                    out=V.ap[:, :, 0:128],
                    in_=v_s.rearrange("(tt p) c -> p tt c", p=128)[:, :, hd * 128:(hd + 1) * 128]),
                    reads=[T_v[hd // 2]], writes=[V])
                zero_fill(hd)
                return qT, kT, V

            def prep(hd, loaded):
                qT, kT, V = loaded
                Bpad = Bpr.next()
                sc.op("dve", lambda h: h.tensor_reduce(
                    out=ks32.ap[:, :], in_=kT.ap[:, :].rearrange("p (n k) -> p n k", k=256), axis=AX.X, op=OP.add),
                    reads=[kT], writes=[ks32])
                sc.op("dve", lambda h: h.tensor_copy(out=ksb.ap[:, :], in_=ks32.ap[:, :]), reads=[ks32], writes=[ksb])
                for i in range(16):
                    sc.op("pe", lambda h, i=i: h.matmul(
                        psG.ap[:, i * 8:(i + 1) * 8], lhsT=qT.ap[:, i * 128:(i + 1) * 128], rhs=ksb.ap[:, :],
                        start=True, stop=True), reads=[qT, ksb], writes=[psG])
                sc.op("dve", lambda h: h.tensor_tensor(out=gm.ap[:, :], in0=psG.ap[:, 0:128], in1=pastbias, op=OP.add),
                      reads=[psG, T_cf], writes=[gm])
                for i in range(16):
                    sc.op("dve", lambda h, i=i: h.max(out=top8.ap[:, i * 8:(i + 1) * 8], in_=gm.ap[:, i * 8:(i + 1) * 8]),
                          reads=[gm], writes=[top8])
                sc.op("dve", lambda h: h.tensor_tensor(
                    out=lt.ap[:, :].rearrange("p (i n) -> p i n", n=8),
                    in0=gm.ap[:, :].rearrange("p (i n) -> p i n", n=8),
                    in1=top8.ap[:, :].rearrange("p (i n) -> p i n", n=8)[:, :, 2:3].to_broadcast([128, 16, 8]),
                    op=OP.is_lt), reads=[gm, top8], writes=[lt])
                sc.op("dve", lambda h: h.tensor_tensor(
                    out=Bpad.ap[:, :, 0:8],
                    in0=lt.ap[:, :].rearrange("p (i n) -> p i n", n=8),
                    in1=notown.rearrange("p (i n) -> p i n", n=8), op=OP.mult),
                    reads=[lt, T_cf], writes=[Bpad])
                return qT, kT, V, OTr.next(), Bpad

            def stage1(ctx, c, par):
                qT, kT, V, OTst, Bpad = ctx
                for ii in range(4):
                    sc.op("pe", lambda h, ii=ii: h.transpose(
                        out=psBT.ap[:, ii * 128:(ii + 1) * 128], in_=Bpad.ap[:, 4 * c + ii, :], identity=ident32),
                        reads=[Bpad, T_cf], writes=[psBT])
                BT = BTr.next()
                sc.op("act", lambda h: h.copy(out=BT.ap[:, :], in_=psBT.ap[:, :]), reads=[psBT], writes=[BT])
                for j in range(4 * c + 4):
                    q0 = max(512 * c, 128 * j)
                    off = q0 - 512 * c
                    diag = j >= 4 * c
                    st = psST.next()
                    sc.op("pe", lambda h, st=st, j=j, q0=q0, off=off: h.matmul(
                        st.ap[:, off:512], lhsT=kT.ap[:, j * 128:(j + 1) * 128], rhs=qT.ap[:, q0:512 * c + 512],
                        start=True, stop=False), reads=[kT, qT], writes=[st])
                    sc.op("pe", lambda h, st=st, j=j, off=off, diag=diag: h.matmul(
                        st.ap[:, off:512], lhsT=blkind[:, j * 128:(j + 1) * 128], rhs=BT.ap[:, off:512],
                        start=False, stop=(not diag)), reads=[BT, T_cb], writes=[st])
                    if diag:
                        sc.op("pe", lambda h, st=st, off=off: h.matmul(
                            st.ap[:, off:off + 128], lhsT=identb, rhs=tri, start=False, stop=True,
                            skip_group_check=True), reads=[T_cb], writes=[st])
                    sc.op("act", lambda h, st=st, j=j, off=off: h.activation(
                        out=PTbuf[par][:, j, off:512], in_=st.ap[:, off:512], func=AF.Exp, scale=SCALE),
                        reads=[st], writes=[PT[par][j]])

            def stage2(ctx, c, par, hd):
                qT, kT, V, OTst, Bpad = ctx
                otp = psO.next()
                rsp = psOT.next()
                nj = 4 * c + 4
                for j in range(nj):
                    off = max(512 * c, 128 * j) - 512 * c
                    sc.op("pe", lambda h, j=j, off=off: h.matmul(
                        otp.ap[:, off:512], lhsT=V.ap[:, j, 0:128], rhs=PTbuf[par][:, j, off:512],
                        start=(j == 0), stop=(j == nj - 1), skip_group_check=True), reads=[PT[par][j], V], writes=[otp])
                    sc.op("pe", lambda h, j=j, off=off: h.matmul(
                        rsp.ap[:, off:512], lhsT=onesb, rhs=PTbuf[par][:, j, off:512],
                        start=(j == 0), stop=(j == nj - 1), skip_group_check=True), reads=[PT[par][j], T_cb], writes=[rsp])
                rinv = rinvr.next()
                sc.op("dve", lambda h: h.reciprocal(out=rinv.ap[:, :], in_=rsp.ap[:, :]), reads=[rsp], writes=[rinv])
                sc.op("dve", lambda h: h.tensor_tensor(
                    out=OTst.ap[:, c * 512:(c + 1) * 512], in0=otp.ap[:, :], in1=rinv.ap[:, :], op=OP.mult),
                    reads=[otp, rinv], writes=[OTst])
                if c == 3:
                    sc.dma("pool", lambda h: h.dma_start(out=ot_s[hd], in_=OTst.ap[:, :]), reads=[OTst], writes=[T_ot[hd]])

            items = [(hd, c) for hd in range(NH) for c in range(4)]
            ctxs = {}
            prev = None
            ctxs[0] = prep(0, prep_load(0))
            loaded = None
            for k, (hd, c) in enumerate(items):
                stage1(ctxs[hd], c, k % 2)
                if prev is not None:
                    stage2(ctxs[prev[0]], prev[1], (k - 1) % 2, prev[0])
                if c == 0 and hd + 1 < NH:
                    loaded = prep_load(hd + 1)
                if c == 1 and hd + 1 < NH:
                    ctxs[hd + 1] = prep(hd + 1, loaded)
                prev = (hd, c)
            stage2(ctxs[prev[0]], prev[1], (len(items) - 1) % 2, prev[0])
        sc.fence()
    if "C" in phases:
        with ExitStack() as ec:
            def sbc(name, shape, dt):
                return ec.enter_context(nc.sbuf_tensor(f"s{next(_uid)}_" + name, list(shape), dt))
            cvec = sbc("cvec", [128, 16, 36], F32)
            bG = sbc("bG", [128, 64], F32)
            wr = sbc("wr", [128, 16, 36], F32)
            brt = sbc("br", [128, 36], F32)
            T_cc = T()
            sc.dma("sp", lambda h: h.dma_start(out=cvec[:], in_=cvec_d), writes=[T_cc])
            sc.dma("sp", lambda h: h.dma_start(out=bG[:], in_=bG_d), writes=[T_cc])
            sc.dma("sp", lambda h: h.dma_start(out=wr[:], in_=wr_d), writes=[T_cc])
            sc.dma("sp", lambda h: h.dma_start(out=brt[:], in_=br_d.partition_broadcast(128)), writes=[T_cc])
            sc.dma("pool", lambda h: h.dma_start(out=stab_s, in_=sinit_d), writes=[T_stab])
            halo = T(sbc("halo", [128, 16, 32], F32))
            sc.op("pool", lambda h: h.memset(halo.ap[:, :, :], 0.0), writes=[halo])
            Acum = T(sbc("Acum", [128, 32], F32))
            sc.op("pool", lambda h: h.memset(Acum.ap[:, :], 0.0), writes=[Acum])
            mT = sbc("mT", [128, 16, 512], F32)
            T_mT = [T() for _ in range(16)]
            psA = Ring(PS[0:2])
            psB = Ring(PS[2:4])
            psS = [PS[4], PS[5]]
            psX = Ring(PS[6:8])

            def gemm_fm(wring, wsrc, ps, act, T_act, W=None):
                if W is None:
                    W = wring.next()
                    sc.dma("sp", lambda h, W=W: h.dma_start(out=W.ap[:], in_=wsrc), writes=[W])
                for kc in range(16):
                    sc.op("pe", lambda h, W=W, kc=kc: h.matmul(
                        ps.ap[:, :], lhsT=r32(W.ap[:, kc, :]), rhs=r32(act[:, kc, :]),
                        start=(kc == 0), stop=(kc == 15)),
                        reads=[W, (T_act[kc * len(T_act) // 16] if isinstance(T_act, list) else T_act)], writes=[ps])

            for c in range(4):
                t0 = c * 512
                e12 = ExitStack()
                def sb12(name, shape, dt):
                    return e12.enter_context(nc.sbuf_tensor(f"s{next(_uid)}_" + name, list(shape), dt))
                xTc = sb12("xTc", [128, 16, 512], F32R)
                T_xTc = [T() for _ in range(4)]
                OTc = sb12("OTc", [128, 16, 512], F32R)
                T_OTc = T()
                wring = Ring([T(sb12(f"wC{i}", [128, 16, 128], F32R)) for i in range(3)])
                sgr = Ring([T(sb12(f"sg{i}", [128, 512], F32)) for i in range(2)])
                tmr = Ring([T(sb12(f"tm{i}", [128, 512], F32)) for i in range(2)])
                cT = sb12("cT", [128, 16, 512], F32)
                T_cT = [T() for _ in range(16)]
                hTr = Ring([T(sb12(f"hT{i}", [128, 544], F32)) for i in range(4)])
                dgr = Ring([T(sb12(f"dg{i}", [128, 16, 128], F32)) for i in range(2)])
                mean = T(sb12("mean", [128, 512], F32))
                rstd = T(sb12("rstd", [128, 512], F32))
                Wfirst = []
                for kq in range(4):
                    sc.dma("sp", lambda h, kq=kq: h.dma_start(
                        out=xTc[:, 4 * kq:4 * kq + 4, :],
                        in_=xT_d.rearrange("(kc p) t -> p kc t", p=128)[:, 4 * kq:4 * kq + 4, t0:t0 + 512]),
                        writes=[T_xTc[kq]])
                    if kq == 0:
                        for src in (wG_d[0], wG_d[16]):
                            W = wring.next()
                            sc.dma("sp", lambda h, W=W, src=src: h.dma_start(out=W.ap[:], in_=src), writes=[W])
                            Wfirst.append(W)

                def conv_pe(cc, hT):
                    halves = []
                    for hh in range(2):
                        j0 = 16 * hh
                        n = 16 if hh == 0 else 15
                        dg = dgr.next()
                        sc.op("pool", lambda h, dg=dg, j0=j0, n=n: h.tensor_tensor(
                            out=r32(dg.ap[:, 0:n, :]), in0=ident32.unsqueeze(1).to_broadcast([128, n, 128]),
                            in1=cvec[:, cc, j0:j0 + n].unsqueeze(2).to_broadcast([128, n, 128]), op=OP.mult),
                            reads=[T_cf, T_cc], writes=[dg])
                        halves.append(dg)
                    ps = psX.next()
                    for j in range(31):
                        dg = halves[j // 16]
                        sc.op("pe", lambda h, j=j, dg=dg: h.matmul(
                            ps.ap[:, :], lhsT=r32(dg.ap[:, j % 16, :]), rhs=r32(hT.ap[:, 2 + j:514 + j]),
                            start=(j == 0), stop=(j == 30)), reads=[dg, hT], writes=[ps])
                    sc.op("act", lambda h: h.activation(
                        out=r32(cT[:, cc, :]), in_=ps.ap[:, :], func=AF.Identity, bias=cvec[:, cc, 31:32], scale=1.0),
                        reads=[ps, T_cc], writes=[T_cT[cc]])
                    sc.op("pool", lambda h: h.tensor_copy(out=halo.ap[:, cc, :], in_=hT.ap[:, 512:544]),
                          reads=[hT], writes=[halo])

                pend = None
                for cc in range(16):
                    hT = hTr.next()
                    sc.op("pool", lambda h, cc=cc, hT=hT: h.tensor_copy(out=r32(hT.ap[:, 0:32]), in_=halo.ap[:, cc, :]),
                          reads=[halo], writes=[hT])
                    pa, pg = psA.next(), psB.next()
                    gemm_fm(wring, wG_d[cc], pa, xTc, T_xTc, W=(Wfirst[0] if cc == 0 else None))
                    gemm_fm(wring, wG_d[16 + cc], pg, xTc, T_xTc, W=(Wfirst[1] if cc == 0 else None))
                    if cc == 2:
                        sc.dma("sp", lambda h: h.dma_start(
                            out=OTc[:, :, :], in_=ot_s.rearrange("k p t -> p k t")[:, :, t0:t0 + 512]),
                            reads=T_ot, writes=[T_OTc])
                    sg = sgr.next()
                    sc.op("act", lambda h, sg=sg, pg=pg, cc=cc: h.activation(
                        out=sg.ap[:, :], in_=pg.ap[:, :], func=AF.Sigmoid, bias=bG[:, 16 + cc:17 + cc], scale=1.0),
                        reads=[pg, T_cc], writes=[sg])
                    sc.op("dve", lambda h, sg=sg, pa=pa, cc=cc, hT=hT: h.scalar_tensor_tensor(
                        out=r32(hT.ap[:, 32:544]), in0=pa.ap[:, :], scalar=bG[:, cc:cc + 1], in1=sg.ap[:, :],
                        op0=OP.add, op1=OP.mult), reads=[pa, sg, T_cc, hT], writes=[hT])
                    if pend is not None:
                        conv_pe(*pend)
                    pend = (cc, hT)
                conv_pe(*pend)

                pm, pq = psS
                for cc in range(16):
                    sc.op("pe", lambda h, cc=cc: h.matmul(pm.ap[:, :], lhsT=r32(onesn), rhs=r32(cT[:, cc, :]),
                                                         start=(cc == 0), stop=(cc == 15)),
                          reads=[T_cT[cc], T_cf], writes=[pm])
                for cc in range(16):
                    sq = hTr.next()
                    sc.op("act", lambda h, sq=sq, cc=cc: h.activation(out=r32(sq.ap[:, 0:512]), in_=cT[:, cc, :], func=AF.Square),
                          reads=[T_cT[cc]], writes=[sq])
                    sc.op("pe", lambda h, sq=sq, cc=cc: h.matmul(pq.ap[:, :], lhsT=r32(onesn), rhs=r32(sq.ap[:, 0:512]),
                                                                start=(cc == 0), stop=(cc == 15)),
                          reads=[sq, T_cf], writes=[pq])
                sc.op("dve", lambda h: h.tensor_copy(out=mean.ap[:, :], in_=pm.ap[:, :]), reads=[pm], writes=[mean])
                sc.op("dve", lambda h: h.tensor_tensor(out=rstd.ap[:, :], in0=mean.ap[:, :], in1=mean.ap[:, :], op=OP.mult),
                      reads=[mean], writes=[rstd])
                sc.op("dve", lambda h: h.tensor_tensor(out=rstd.ap[:, :], in0=pq.ap[:, :], in1=rstd.ap[:, :], op=OP.subtract),
                      reads=[pq, rstd], writes=[rstd])
                sc.op("dve", lambda h: h.tensor_scalar(out=rstd.ap[:, :], in0=rstd.ap[:, :], scalar1=LN_EPS, scalar2=None,
                                                       op0=OP.add), reads=[rstd], writes=[rstd])
                sc.op("act", lambda h: h.activation(out=rstd.ap[:, :], in_=rstd.ap[:, :], func=AF.Sqrt),
                      reads=[rstd], writes=[rstd])
                sc.op("dve", lambda h: h.reciprocal(out=rstd.ap[:, :], in_=rstd.ap[:, :]), reads=[rstd], writes=[rstd])

                for j in range(16):
                    cc = j
                    en = "dve" if cc % 2 == 0 else "pool"
                    nt = hTr.next()
                    sc.op(en, lambda h, cc=cc, nt=nt: h.tensor_tensor(out=r32(nt.ap[:, 0:512]), in0=cT[:, cc, :], in1=mean.ap[:, :],
                                                                      op=OP.subtract), reads=[T_cT[cc], mean], writes=[nt])
                    sc.op(en, lambda h, cc=cc, nt=nt: h.tensor_tensor(out=r32(nt.ap[:, 0:512]), in0=nt.ap[:, 0:512], in1=rstd.ap[:, :],
                                                                      op=OP.mult), reads=[nt, rstd], writes=[nt])
                    sc.op("act", lambda h, cc=cc, nt=nt: h.activation(
                        out=r32(cT[:, cc, :]), in_=nt.ap[:, 0:512], func=AF.Silu, bias=cvec[:, cc, 33:34], scale=cvec[:, cc, 32:33]),
                        reads=[nt, T_cc], writes=[T_cT[cc]])
                    py, pg = psA.next(), psB.next()
                    gemm_fm(wring, wo_d[j], py, OTc, T_OTc)
                    gemm_fm(wring, wG_d[32 + j], pg, xTc, T_xTc)
                    sg = sgr.next()
                    sc.op("act", lambda h, sg=sg, pg=pg, j=j: h.activation(
                        out=sg.ap[:, :], in_=pg.ap[:, :], func=AF.Sigmoid, bias=bG[:, 32 + j:33 + j], scale=1.0),
                        reads=[pg, T_cc], writes=[sg])
                    sc.op("dve", lambda h, sg=sg, py=py, j=j: h.tensor_tensor(
                        out=r32(mT[:, j, :]), in0=py.ap[:, :], in1=sg.ap[:, :], op=OP.mult),
                        reads=[py, sg], writes=[T_mT[j]])
                for j in range(16):
                    py, pg = psA.next(), psB.next()
                    gemm_fm(wring, wp_d[j], py, cT, T_cT)
                    gemm_fm(wring, wG_d[48 + j], pg, xTc, T_xTc)
                    sg = sgr.next()
                    sc.op("act", lambda h, sg=sg, pg=pg, j=j: h.activation(
                        out=sg.ap[:, :], in_=pg.ap[:, :], func=AF.Sigmoid, bias=bG[:, 48 + j:49 + j], scale=1.0),
                        reads=[pg, T_cc], writes=[sg])
                    tm = tmr.next()
                    sc.op("dve", lambda h, sg=sg, py=py, j=j, tm=tm: h.scalar_tensor_tensor(
                        out=tm.ap[:, :], in0=py.ap[:, :], scalar=cvec[:, j, 34:35], in1=sg.ap[:, :],
                        op0=OP.add, op1=OP.mult), reads=[py, sg, T_cc], writes=[tm])
                    sc.op("pool", lambda h, tm=tm, j=j: h.tensor_tensor(
                        out=r32(mT[:, j, :]), in0=mT[:, j, :], in1=tm.ap[:, :], op=OP.add),
                        reads=[tm, T_mT[j]], writes=[T_mT[j]])
                e12.close()
                sc.fence()
                with ExitStack() as e3:
                    def sb3(name, shape, dt):
                        return e3.enter_context(nc.sbuf_tensor(f"s{next(_uid)}_" + name, list(shape), dt))
                    wor = Ring([T(sb3(f"wout{i}", [128, 16, 512], F32R)) for i in range(2)])
                    lng = sb3("lng", [128, 2048], F32)
                    lnb = sb3("lnb", [128, 2048], F32)
                    T_lng, T_lnb = T(), T()
                    xr = [T(sb3(f"xt{i}", [128, 2048], F32)) for i in range(4)]
                    zr = xr
                    Wpre = []
                    for cb in range(2):
                        W = wor.next()
                        sc.dma("sp", lambda h, W=W, cb=cb: h.dma_start(out=W.ap[:], in_=wout_d[cb]), writes=[W])
                        Wpre.append(W)
                        if cb == 0:
                            for t in range(4):
                                sc.dma("sp", lambda h, t=t, t0=t0: h.dma_start(
                                    out=xr[t].ap[:, :], in_=x_d[t0 + t * 128:t0 + (t + 1) * 128, :]), writes=[xr[t]])
                    sc.dma("sp", lambda h: h.dma_start(out=lng[:], in_=lnv_d[0:1, :].partition_broadcast(128)), writes=[T_lng])
                    sc.dma("sp", lambda h: h.dma_start(out=lnb[:], in_=lnv_d[1:2, :].partition_broadcast(128)), writes=[T_lnb])
                    for cb in range(4):
                        if cb < 2:
                            W = Wpre[cb]
                        else:
                            W = wor.next()
                            sc.dma("sp", lambda h, W=W, cb=cb: h.dma_start(out=W.ap[:], in_=wout_d[cb]), writes=[W])
                        for t in range(4):
                            ps = psA.next() if t % 2 == 0 else psB.next()
                            for kc in range(16):
                                sc.op("pe", lambda h, ps=ps, W=W, kc=kc, t=t: h.matmul(
                                    ps.ap[:, :], lhsT=r32(mT[:, kc, t * 128:(t + 1) * 128]), rhs=r32(W.ap[:, kc, :]),
                                    start=(kc == 0), stop=(kc == 15)), reads=[W, T_mT[kc]], writes=[ps])
                            sc.op("dve", lambda h, ps=ps, t=t, cb=cb: h.scalar_tensor_tensor(
                                out=zr[t].ap[:, cb * 512:(cb + 1) * 512], in0=xr[t].ap[:, cb * 512:(cb + 1) * 512],
                                scalar=ALPHA, in1=ps.ap[:, :], op0=OP.mult, op1=OP.add),
                                reads=[ps, xr[t]], writes=[zr[t]])
                    def mk_small(t):
                        d = {}
                        for nm, shp, dt in (("st6", [128, 4, 6], F32), ("mv", [128, 2], F32), ("rs", [128, 2], F32),
                                            ("lg", [128, 36], F32), ("sm", [128, 64], F32), ("oh", [128, 4], F32),
                                            ("esel", [128, 8], F32), ("t8", [128, 8], F32), ("mk", [128, 2, 8], F32),
                                            ("Ak", [128, 2, 32], F32), ("Asum", [128, 32], F32), ("val", [128, 32], F32),
                                            ("ov", [128, 32], F32), ("dst", [128, 2], F32), ("dsti", [128, 2], I32),
                                            ("pay", [128, 2, 4], I32), ("tokf", [128, 2], F32)):
                            d[nm] = T(sb3(f"{nm}{t}", shp, dt))
                        return d
                    small = [mk_small(t) for t in range(4)]
                    hTts = [T(sb3(f"hTt{i}", [128, 16, 128], F32)) for i in range(2)]

                    def tail1(t):
                        g = c * 4 + t
                        z = zr[t]
                        d = small[t]
                        st6, mv, rs, lg, sm, oh, esel, t8, mk, Ak, Asum = (d[k] for k in (
                            "st6", "mv", "rs", "lg", "sm", "oh", "esel", "t8", "mk", "Ak", "Asum"))
                        hTt = hTts[t % 2]
                        for q4 in range(4):
                            sc.op("dve", lambda h, q4=q4: h.bn_stats(out=st6.ap[:, q4, :], in_=z.ap[:, q4 * 512:(q4 + 1) * 512]),
                                  reads=[z], writes=[st6])
                        sc.op("dve", lambda h: h.bn_aggr(out=mv.ap[:, :], in_=st6.ap[:, :, :].rearrange("p a b -> p (a b)")),
                              reads=[st6], writes=[mv])
                        sc.op("dve", lambda h: h.tensor_scalar(out=rs.ap[:, 0:1], in0=mv.ap[:, 1:2], scalar1=LN_EPS, scalar2=None,
                                                               op0=OP.add), reads=[mv], writes=[rs])
                        yield
                        sc.op("act", lambda h: h.activation(out=rs.ap[:, 0:1], in_=rs.ap[:, 0:1], func=AF.Sqrt), reads=[rs], writes=[rs])
                        yield
                        sc.op("dve", lambda h: h.reciprocal(out=rs.ap[:, 0:1], in_=rs.ap[:, 0:1]), reads=[rs], writes=[rs])
                        sc.op("dve", lambda h: h.tensor_scalar(out=rs.ap[:, 1:2], in0=mv.ap[:, 0:1], scalar1=rs.ap[:, 0:1], scalar2=-1.0,
                                                               op0=OP.mult, op1=OP.mult), reads=[mv, rs], writes=[rs])
                        yield
                        sc.op("act", lambda h: h.activation(out=z.ap[:, :], in_=z.ap[:, :], func=AF.Identity,
                                                            scale=rs.ap[:, 0:1], bias=rs.ap[:, 1:2]), reads=[z, rs], writes=[z])
                        yield
                        sc.op("pool", lambda h: h.tensor_tensor(out=z.ap[:, :], in0=z.ap[:, :], in1=lng[:, :], op=OP.mult),
                              reads=[z, T_lng], writes=[z])
                        yield
                        sc.op("dve", lambda h: h.tensor_tensor(out=z.ap[:, :], in0=z.ap[:, :], in1=lnb[:, :], op=OP.add),
                              reads=[z, T_lnb], writes=[z])
                        sc.dma("pool", lambda h: h.dma_start(out=h_s[g * 128:(g + 1) * 128, :], in_=z.ap[:, :]),
                               reads=[z], writes=[T_h[g]])
                        yield
                        for k4 in range(4):
                            tp = psX.next()
                            for ii in range(4):
                                kc = k4 * 4 + ii
                                sc.op("pe", lambda h, tp=tp, ii=ii, kc=kc: h.transpose(
                                    out=tp.ap[:, ii * 128:(ii + 1) * 128], in_=z.ap[:, kc * 128:(kc + 1) * 128], identity=ident32),
                                    reads=[z, T_cf], writes=[tp])
                            sc.op("act", lambda h, tp=tp, k4=k4: h.copy(
                                out=hTt.ap[:, k4 * 4:(k4 + 1) * 4, :].rearrange("p a b -> p (a b)"), in_=tp.ap[:, :]),
                                reads=[tp], writes=[hTt])
                        pl = psS[0]
                        for kc in range(16):
                            sc.op("pe", lambda h, kc=kc: h.matmul(pl.ap[:, t * 64:t * 64 + 36], lhsT=hTt.ap[:, kc, :], rhs=wr[:, kc, :],
                                                                  start=(kc == 0), stop=(kc == 15)),
                                  reads=[hTt, T_cc], writes=[pl])
                        yield
                        sc.op("dve", lambda h: h.tensor_tensor(out=lg.ap[:, :], in0=pl.ap[:, t * 64:t * 64 + 36], in1=brt[:, :], op=OP.add),
                              reads=[pl, T_cc], writes=[lg])
                        sc.op("dve", lambda h: h.tensor_reduce(out=sm.ap[:, 0:1], in_=lg.ap[:, 0:4], axis=AX.X, op=OP.max),
                              reads=[lg], writes=[sm])
                        sc.op("dve", lambda h: h.tensor_scalar(out=oh.ap[:, :], in0=lg.ap[:, 0:4], scalar1=sm.ap[:, 0:1], scalar2=None,
                                                               op0=OP.is_equal), reads=[lg, sm], writes=[oh])
                        sc.op("dve", lambda h: h.tensor_scalar(out=sm.ap[:, 1:2], in0=sm.ap[:, 0:1], scalar1=-1.0, scalar2=None,
                                                               op0=OP.mult), reads=[sm], writes=[sm])
                        sc.op("dve", lambda h: h.tensor_scalar(out=esel.ap[:, :], in0=lg.ap[:, 4:12], scalar1=oh.ap[:, 0:1], scalar2=None,
                                                               op0=OP.mult), reads=[lg, oh], writes=[esel])
                        for gi in range(1, 4):
                            sc.op("dve", lambda h, gi=gi: h.scalar_tensor_tensor(
                                out=esel.ap[:, :], in0=lg.ap[:, 4 + 8 * gi:12 + 8 * gi], scalar=oh.ap[:, gi:gi + 1], in1=esel.ap[:, :],
                                op0=OP.mult, op1=OP.add), reads=[lg, oh, esel], writes=[esel])
                        sc.op("dve", lambda h: h.max(out=t8.ap[:, :], in_=esel.ap[:, :]), reads=[esel], writes=[t8])
                        for k in range(2):
                            sc.op("dve", lambda h, k=k: h.tensor_scalar(out=mk.ap[:, k, :], in0=esel.ap[:, :], scalar1=t8.ap[:, k:k + 1],
                                                                        scalar2=None, op0=OP.is_equal), reads=[esel, t8], writes=[mk])
                        sc.op("dve", lambda h: h.tensor_tensor(out=sm.ap[:, 4:5], in0=t8.ap[:, 1:2], in1=t8.ap[:, 0:1], op=OP.subtract),
                              reads=[t8], writes=[sm])
                        yield
                        sc.op("act", lambda h: h.activation(out=sm.ap[:, 10:14], in_=lg.ap[:, 0:4], func=AF.Exp, bias=sm.ap[:, 1:2],
                                                            scale=1.0), reads=[lg, sm], writes=[sm])
                        sc.op("act", lambda h: h.activation(out=sm.ap[:, 5:6], in_=sm.ap[:, 4:5], func=AF.Exp), reads=[sm], writes=[sm])
                        yield
                        sc.op("dve", lambda h: h.tensor_reduce(out=sm.ap[:, 2:3], in_=sm.ap[:, 10:14], axis=AX.X, op=OP.add),
                              reads=[sm], writes=[sm])
                        sc.op("dve", lambda h: h.reciprocal(out=sm.ap[:, 3:4], in_=sm.ap[:, 2:3]), reads=[sm], writes=[sm])
                        sc.op("dve", lambda h: h.tensor_scalar(out=sm.ap[:, 6:7], in0=sm.ap[:, 5:6], scalar1=1.0, scalar2=None, op0=OP.add),
                              reads=[sm], writes=[sm])
                        sc.op("dve", lambda h: h.reciprocal(out=sm.ap[:, 6:7], in_=sm.ap[:, 6:7]), reads=[sm], writes=[sm])
                        sc.op("dve", lambda h: h.tensor_tensor(out=sm.ap[:, 7:8], in0=sm.ap[:, 6:7], in1=sm.ap[:, 3:4], op=OP.mult),
                              reads=[sm], writes=[sm])
                        sc.op("dve", lambda h: h.tensor_tensor(out=sm.ap[:, 8:9], in0=sm.ap[:, 7:8], in1=sm.ap[:, 5:6], op=OP.mult),
                              reads=[sm], writes=[sm])
                        for k in range(2):
                            for gi in range(4):
                                sc.op("dve", lambda h, k=k, gi=gi: h.tensor_scalar(
                                    out=Ak.ap[:, k, gi * 8:(gi + 1) * 8], in0=mk.ap[:, k, :], scalar1=oh.ap[:, gi:gi + 1], scalar2=None,
                                    op0=OP.mult), reads=[mk, oh], writes=[Ak])
                        sc.op("dve", lambda h: h.tensor_tensor(out=Asum.ap[:, :], in0=Ak.ap[:, 0, :], in1=Ak.ap[:, 1, :], op=OP.add),
                              reads=[Ak], writes=[Asum])
                        yield

                    def tail2(t):
                        g = c * 4 + t
                        d = small[t]
                        sm, Ak, Asum, val, ov, dst, dsti, pay, tokf = (d[k] for k in (
                            "sm", "Ak", "Asum", "val", "ov", "dst", "dsti", "pay", "tokf"))
                        pp = psS[1]
                        sc.op("pe", lambda h: h.matmul(pp.ap[:, 0:32], lhsT=U32, rhs=Asum.ap[:, :], start=True, stop=False),
                              reads=[Asum, T_cf], writes=[pp])
                        sc.op("pe", lambda h: h.matmul(pp.ap[:, 0:32], lhsT=ones32, rhs=Acum.ap[:, :], start=False, stop=True),
                              reads=[Acum, T_cf], writes=[pp])
                        sc.op("dve", lambda h: h.tensor_scalar(out=ov.ap[:, :], in0=pp.ap[:, 0:32], scalar1=float(CAP), scalar2=1.0e6,
                                                               op0=OP.is_ge, op1=OP.mult), reads=[pp], writes=[ov])
                        sc.op("dve", lambda h: h.tensor_tensor(out=val.ap[:, :], in0=pp.ap[:, 0:32], in1=slotbase, op=OP.add),
                              reads=[pp, T_cf], writes=[val])
                        sc.op("dve", lambda h: h.tensor_tensor(out=Acum.ap[:, :], in0=Acum.ap[:, :], in1=Asum.ap[:, :], op=OP.add),
                              reads=[Acum, Asum], writes=[Acum])
                        sc.op("dve", lambda h: h.tensor_tensor(out=val.ap[:, :], in0=val.ap[:, :], in1=ov.ap[:, :], op=OP.add),
                              reads=[val, ov], writes=[val])
                        for k in range(2):
                            sc.op("dve", lambda h, k=k: h.tensor_tensor(out=Ak.ap[:, k, :], in0=Ak.ap[:, k, :], in1=val.ap[:, :], op=OP.mult),
                                  reads=[Ak, val], writes=[Ak])
                            sc.op("dve", lambda h, k=k: h.tensor_reduce(out=dst.ap[:, k:k + 1], in_=Ak.ap[:, k, :], axis=AX.X, op=OP.add),
                                  reads=[Ak], writes=[dst])
                        sc.op("dve", lambda h: h.tensor_copy(out=dsti.ap[:, :], in_=dst.ap[:, :]), reads=[dst], writes=[dsti])
                        sc.op("pool", lambda h: h.tensor_scalar(out=tokf.ap[:, 0:1], in0=iota_p, scalar1=float(g * 128), scalar2=None,
                                                                op0=OP.add), reads=[T_cf], writes=[tokf])
                        sc.op("pool", lambda h: h.tensor_scalar(out=tokf.ap[:, 1:2], in0=iota_p, scalar1=float(g * 128 + S), scalar2=None,
                                                                op0=OP.add), reads=[T_cf], writes=[tokf])
                        sc.op("pool", lambda h: h.memset(pay.ap[:, :, :], 0), writes=[pay])
                        sc.op("pool", lambda h: h.tensor_copy(out=pay.ap[:, 0, 0:1], in_=tokf.ap[:, 0:1]), reads=[tokf], writes=[pay])
                        sc.op("pool", lambda h: h.tensor_copy(out=pay.ap[:, 0, 1:2], in_=tokf.ap[:, 0:1]), reads=[tokf], writes=[pay])
                        sc.op("pool", lambda h: h.tensor_copy(out=pay.ap[:, 1, 0:1], in_=tokf.ap[:, 0:1]), reads=[tokf], writes=[pay])
                        sc.op("pool", lambda h: h.tensor_copy(out=pay.ap[:, 1, 1:2], in_=tokf.ap[:, 1:2]), reads=[tokf], writes=[pay])
                        for k in range(2):
                            sc.op("dve", lambda h, k=k: h.tensor_copy(out=pay.ap[:, k, 2:3].bitcast(F32), in_=sm.ap[:, 7 + k:8 + k]),
                                  reads=[sm], writes=[pay])
                        for k in range(2):
                            sc.dma("pool", lambda h, k=k: h.indirect_dma_start(
                                out=stab_s[:, :], out_offset=bass.IndirectOffsetOnAxis(ap=dsti.ap[:, k:k + 1], axis=0),
                                in_=pay.ap[:, k, :], in_offset=None, bounds_check=reg_ns, oob_is_err=False),
                                reads=[pay, dsti], writes=[T_stab])

                    gens = [tail1(t) for t in range(4)]
                    live = list(gens)
                    while live:
                        for gnr in list(live):
                            try:
                                next(gnr)
                            except StopIteration:
                                live.remove(gnr)
                    for t in range(4):
                        tail2(t)

                sc.fence()
        sc.fence()
    if "D" in phases:
        with ExitStack() as ed:
            def sbd(name, shape, dt):
                return ed.enter_context(nc.sbuf_tensor(f"s{next(_uid)}_" + name, list(shape), dt))
            str_ = Ring([T(sbd(f"stb{i}", [128, 2, 4], I32)) for i in range(3)])
            hgr = Ring([T(sbd(f"hg{i}", [128, 2048], F32)) for i in range(2)])
            hgTr = Ring([T(sbd(f"hgT{i}", [128, 16, 256], F32R)) for i in range(2)])
            wer = Ring([T(sbd(f"web{i}", [128, 4096], F32R)) for i in range(7)])
            sgr = Ring([T(sbd(f"sgd{i}", [128, 256], F32)) for i in range(2)])
            actT = sbd("actT", [128, 8, 256], F32R)
            T_act = [T() for _ in range(8)]
            your = Ring([T(sbd(f"yout{i}", [128, 2048], F32)) for i in range(2)])
            psT = Ring(PS[0:2])
            psa = Ring(PS[2:4])
            psb = Ring(PS[4:6])
            psy = Ring(PS[6:8])

            def gather(e):
                stb = str_.next()
                sc.dma("pool", lambda h: h.dma_start(
                    out=stb.ap[:, :, :], in_=stab_s[e * CAP:(e + 1) * CAP, :].rearrange("(s p) c -> p s c", p=128)),
                    reads=[T_stab], writes=[stb])
                hgs = []
                for s in range(2):
                    hg = hgr.next()
                    sc.dma("pool", lambda h, hg=hg, s=s: h.indirect_dma_start(
                        out=hg.ap[:, :], out_offset=None, in_=h_s[:, :],
                        in_offset=bass.IndirectOffsetOnAxis(ap=stb.ap[:, s, 0:1], axis=0)),
                        reads=[stb] + T_h, writes=[hg])
                    hgs.append(hg)
                return stb, hgs

            def transposes(hgs):
                hgT = hgTr.next()
                for s in range(2):
                    hg = hgs[s]
                    for k4 in range(4):
                        tp = psT.next()
                        for ii in range(4):
                            kc = k4 * 4 + ii
                            sc.op("pe", lambda h, tp=tp, ii=ii, kc=kc, hg=hg: h.transpose(
                                out=tp.ap[:, ii * 128:(ii + 1) * 128], in_=hg.ap[:, kc * 128:(kc + 1) * 128], identity=ident32),
                                reads=[hg, T_cf], writes=[tp])
                        if k4 % 2 == 0:
                            sc.op("dve", lambda h, tp=tp, k4=k4, s=s: h.tensor_copy(
                                out=hgT.ap[:, k4 * 4:(k4 + 1) * 4, s * 128:(s + 1) * 128],
                                in_=tp.ap[:, :].rearrange("p (a b) -> p a b", b=128)), reads=[tp], writes=[hgT])
                        else:
                            sc.op("act", lambda h, tp=tp, k4=k4, s=s: h.copy(
                                out=hgT.ap[:, k4 * 4:(k4 + 1) * 4, s * 128:(s + 1) * 128],
                                in_=tp.ap[:, :].rearrange("p (a b) -> p a b", b=128)), reads=[tp], writes=[hgT])
                return hgT

            nxt = gather(0)
            hgT_next = transposes(nxt[1])
            for e in range(NE):
                stb, hgs = nxt
                hgT = hgT_next
                if e + 1 < NE:
                    nxt = gather(e + 1)
                for fb in range(4):
                    W1, W3 = wer.next(), wer.next()
                    sc.dma("sp", lambda h, W1=W1, fb=fb: h.dma_start(
                        out=W1.ap[:, :], in_=w1_d[e, fb].rearrange("p k c -> p (k c)")), writes=[W1])
                    sc.dma("sp", lambda h, W3=W3, fb=fb: h.dma_start(
                        out=W3.ap[:, :], in_=w3_d[e, fb].rearrange("p k c -> p (k c)")), writes=[W3])
                    for half in range(2):
                        fc = fb * 2 + half
                        pa, pb = psa.next(), psb.next()
                        for kc in range(16):
                            sc.op("pe", lambda h, pa=pa, W1=W1, kc=kc, half=half: h.matmul(
                                pa.ap[:, 0:256], lhsT=r32(W1.ap[:, kc * 256 + half * 128:kc * 256 + (half + 1) * 128]), rhs=r32(hgT.ap[:, kc, :]),
                                start=(kc == 0), stop=(kc == 15)), reads=[W1, hgT], writes=[pa])
                        for kc in range(16):
                            sc.op("pe", lambda h, pb=pb, W3=W3, kc=kc, half=half: h.matmul(
                                pb.ap[:, 0:256], lhsT=r32(W3.ap[:, kc * 256 + half * 128:kc * 256 + (half + 1) * 128]), rhs=r32(hgT.ap[:, kc, :]),
                                start=(kc == 0), stop=(kc == 15)), reads=[W3, hgT], writes=[pb])
                        sg = sgr.next()
                        sc.op("act", lambda h, sg=sg, pa=pa: h.activation(out=sg.ap[:, :], in_=pa.ap[:, 0:256], func=AF.Silu),
                              reads=[pa], writes=[sg])
                        sc.op("dve", lambda h, sg=sg, pb=pb, fc=fc: h.tensor_tensor(
                            out=actT[:, fc, :], in0=pb.ap[:, 0:256], in1=sg.ap[:, :], op=OP.mult),
                            reads=[pb, sg], writes=[T_act[fc]])
                if e + 1 < NE:
                    hgT_next = transposes(nxt[1])
                youts = [your.next(), your.next()]
                for cb in range(4):
                    W2 = wer.next()
                    sc.dma("sp", lambda h, W2=W2, cb=cb: h.dma_start(
                        out=W2.ap[:, :], in_=w2_d[e, cb].rearrange("p k c -> p (k c)")), writes=[W2])
                    for s in range(2):
                        py = psy.next()
                        for fc in range(8):
                            sc.op("pe", lambda h, py=py, W2=W2, fc=fc, s=s: h.matmul(
                                py.ap[:, :], lhsT=r32(actT[:, fc, s * 128:(s + 1) * 128]), rhs=r32(W2.ap[:, fc * 512:(fc + 1) * 512]),
                                start=(fc == 0), stop=(fc == 7)), reads=[W2, T_act[fc]], writes=[py])
                        yo = youts[s]
                        sc.op("act", lambda h, py=py, yo=yo, cb=cb, s=s: h.activation(
                            out=yo.ap[:, cb * 512:(cb + 1) * 512], in_=py.ap[:, :], func=AF.Copy,
                            scale=stb.ap[:, s, 2:3].bitcast(F32)), reads=[py, stb], writes=[yo])
                for s in range(2):
                    yo = youts[s]
                    sc.dma("pool", lambda h, yo=yo, s=s: h.indirect_dma_start(
                        out=y_s[:, :], out_offset=bass.IndirectOffsetOnAxis(ap=stb.ap[:, s, 1:2], axis=0),
                        in_=yo.ap[:, :], in_offset=None, bounds_check=reg_y, oob_is_err=False),
                        reads=[yo, stb], writes=[T_y])
        sc.fence()
    if "E" in phases:
        with ExitStack() as ee:
            def sbe(name, shape, dt):
                return ee.enter_context(nc.sbuf_tensor(f"s{next(_uid)}_" + name, list(shape), dt))
            lng = sbe("lng2", [128, 2048], F32)
            lnb = sbe("lnb2", [128, 2048], F32)
            T_ln = T()
            sc.dma("sp", lambda h: h.dma_start(out=lng[:], in_=lnv_d[2:3, :].partition_broadcast(128)), writes=[T_ln])
            sc.dma("sp", lambda h: h.dma_start(out=lnb[:], in_=lnv_d[3:4, :].partition_broadcast(128)), writes=[T_ln])
            hr = Ring([T(sbe(f"he{i}", [128, 2048], F32)) for i in range(4)])
            y0r = Ring([T(sbe(f"y0e{i}", [128, 2048], F32)) for i in range(3)])
            y1r = Ring([T(sbe(f"y1e{i}", [128, 2048], F32)) for i in range(3)])
            smr = Ring([(T(sbe(f"st6e{i}", [128, 4, 6], F32)), T(sbe(f"mve{i}", [128, 2], F32)), T(sbe(f"rse{i}", [128, 2], F32)))
                        for i in range(4)])
            T_out = T()

            def etile(g):
                ht, y0, y1 = hr.next(), y0r.next(), y1r.next()
                st6, mv, rs = smr.next()
                sc.dma("sp", lambda h: h.dma_start(out=ht.ap[:, :], in_=h_s[g * 128:(g + 1) * 128, :]), reads=[T_h[g]], writes=[ht])
                sc.dma("sp", lambda h: h.dma_start(out=y0.ap[:, :], in_=y_s[g * 128:(g + 1) * 128, :]), reads=[T_y], writes=[y0])
                sc.dma("sp", lambda h: h.dma_start(out=y1.ap[:, :], in_=y_s[S + g * 128:S + (g + 1) * 128, :]), reads=[T_y], writes=[y1])
                yield
                sc.op("dve", lambda h: h.scalar_tensor_tensor(
                    out=ht.ap[:, :], in0=ht.ap[:, :], scalar=ALPHA, in1=y0.ap[:, :], op0=OP.mult, op1=OP.add),
                    reads=[ht, y0], writes=[ht])
                sc.op("dve", lambda h: h.tensor_tensor(out=ht.ap[:, :], in0=ht.ap[:, :], in1=y1.ap[:, :], op=OP.add),
                      reads=[ht, y1], writes=[ht])
                for q4 in range(4):
                    sc.op("dve", lambda h, q4=q4: h.bn_stats(out=st6.ap[:, q4, :], in_=ht.ap[:, q4 * 512:(q4 + 1) * 512]),
                          reads=[ht], writes=[st6])
                sc.op("dve", lambda h: h.bn_aggr(out=mv.ap[:, :], in_=st6.ap[:, :, :].rearrange("p a b -> p (a b)")),
                      reads=[st6], writes=[mv])
                sc.op("dve", lambda h: h.tensor_scalar(out=rs.ap[:, 0:1], in0=mv.ap[:, 1:2], scalar1=LN_EPS, scalar2=None, op0=OP.add),
                      reads=[mv], writes=[rs])
                yield
                sc.op("act", lambda h: h.activation(out=rs.ap[:, 0:1], in_=rs.ap[:, 0:1], func=AF.Sqrt), reads=[rs], writes=[rs])
                yield
                sc.op("dve", lambda h: h.reciprocal(out=rs.ap[:, 0:1], in_=rs.ap[:, 0:1]), reads=[rs], writes=[rs])
                sc.op("dve", lambda h: h.tensor_scalar(out=rs.ap[:, 1:2], in0=mv.ap[:, 0:1], scalar1=rs.ap[:, 0:1], scalar2=-1.0,
                                                       op0=OP.mult, op1=OP.mult), reads=[mv, rs], writes=[rs])
                yield
                sc.op("act", lambda h: h.activation(out=ht.ap[:, :], in_=ht.ap[:, :], func=AF.Identity,
                                                    scale=rs.ap[:, 0:1], bias=rs.ap[:, 1:2]), reads=[ht, rs], writes=[ht])
                yield
                sc.op("pool", lambda h: h.tensor_tensor(out=ht.ap[:, :], in0=ht.ap[:, :], in1=lng[:, :], op=OP.mult),
                      reads=[ht, T_ln], writes=[ht])
                yield
                sc.op("dve", lambda h: h.tensor_tensor(out=ht.ap[:, :], in0=ht.ap[:, :], in1=lnb[:, :], op=OP.add),
                      reads=[ht, T_ln], writes=[ht])
                sc.dma("pool", lambda h: h.dma_start(out=out_d[g * 128:(g + 1) * 128, :], in_=ht.ap[:, :]),
                       reads=[ht], writes=[T()])
                yield

            pending = list(range(16))
            live = []
            while pending or live:
                if pending and len(live) < 3:
                    live.append(etile(pending.pop(0)))
                for gnr in list(live):
                    try:
                        next(gnr)
                    except StopIteration:
                        live.remove(gnr)
        sc.fence()
        sc.fence()
    sc.finish()
    es.close()
    return nc


def _st_blocks(w, cols=128):
    K, N = w.shape
    return np.ascontiguousarray(w.reshape(K // 128, 128, N // cols, cols).transpose(2, 1, 0, 3))


def _consts():
    half = 64
    inv_freq = np.power(np.float32(10000.0), -np.arange(half, dtype=np.float32) * np.float32(2.0 / 128)).astype(np.float32)
    ang = np.arange(S, dtype=np.float32)[:, None] * inv_freq[None, :]
    cos = np.cos(ang).astype(np.float32).T
    sin = np.sin(ang).astype(np.float32).T
    rope = np.concatenate([np.concatenate([cos, cos], 0), np.concatenate([sin, sin], 0)], axis=1)
    cf = np.zeros((128, 1024), np.float32)
    cf[:, 0:128] = np.eye(128, dtype=np.float32)
    p = np.arange(128)
    for i in range(16):
        blk = i // 2
        for n in range(8):
            cf[:, 128 + i * 8 + n] = 0.0 if n < blk else -1.0e30
            cf[:, 256 + i * 8 + n] = 0.0 if n == blk else NEG
    cf[:, 384:512] = 1.0 / 2048.0
    cf[:, 512:640] = (p[:, None] < p[None, :]).astype(np.float32)
    cf[:, 640:768] = 1.0
    cf[:, 768:800] = (np.arange(32) * CAP).astype(np.float32)[None, :]
    cf[:, 800] = p.astype(np.float32)
    cb = np.zeros((128, 2688), np.float32)
    for d in range(64):
        cb[d + 64, d] = -1.0
        cb[d, 64 + d] = 1.0
    cb[:, 128:256] = np.eye(128)
    cb[:, 256:384] = np.where(p[:, None] <= p[None, :], 0.0, NEG)
    cb[:, 384:512] = 1.0
    k = np.arange(S)
    for n in range(8):
        cb[n, 640:2688] = (k // 256 == n).astype(np.float32)
    sinit = np.zeros((NS, 4), np.int32)
    sinit[:, 1] = OOBROW
    return rope, cf, cb.astype(ml_dtypes.bfloat16), sinit


def _prep(inp):
    f = lambda a: np.asarray(a, dtype=np.float32)
    w_in = f(inp["w_in"])[0]
    b_in = f(inp["b_in"])[0]
    sh = {}
    sh["wA"] = _st_blocks(w_in[:, 0:4096])
    sh["wV"] = _st_blocks(w_in[:, 4096:6144], 256)
    sh["wG"] = _st_blocks(w_in[:, 6144:14336])
    sh["bq"] = np.ascontiguousarray(b_in[0:4096].reshape(32, 128).T)
    sh["bv"] = np.ascontiguousarray(b_in[4096:6144].reshape(1, 2048))
    sh["bG"] = np.ascontiguousarray(b_in[6144:14336].reshape(64, 128).T)
    sh["wo"] = _st_blocks(f(inp["w_o_attn"])[0])
    sh["wp"] = _st_blocks(f(inp["w_pw2"])[0])
    sh["wout"] = _st_blocks(f(inp["w_out"])[0], 512)
    cvec = np.zeros((128, 16, 36), np.float32)
    cvec[:, :, 0:31] = f(inp["w_dw"])[0].T.reshape(16, 128, 31).transpose(1, 0, 2)
    for i, nm in enumerate(["b_dw", "conv_ln_g", "conv_ln_b", "b_pw2"]):
        cvec[:, :, 31 + i] = f(inp[nm])[0].reshape(16, 128).T
    sh["cvec"] = cvec
    sh["lnv"] = np.ascontiguousarray(np.stack([f(inp["ln1_g"])[0], f(inp["ln1_b"])[0], f(inp["ln2_g"])[0], f(inp["ln2_b"])[0]]))
    wr = np.concatenate([f(inp["w_rg"])[0]] + [f(inp["w_re"])[0, g] for g in range(4)], axis=1)
    sh["wr"] = np.ascontiguousarray(wr.reshape(16, 128, 36).transpose(1, 0, 2))
    sh["br"] = np.concatenate([f(inp["b_rg"])[0], f(inp["b_re"])[0].reshape(-1)]).reshape(1, 36).astype(np.float32)
    w1 = f(inp["w1"])[0]
    w3 = f(inp["w3"])[0]
    w2 = f(inp["w2"])[0]
    sh["w1"] = np.ascontiguousarray(w1.reshape(NE, 16, 128, 4, 256).transpose(0, 3, 2, 1, 4))
    sh["w3"] = np.ascontiguousarray(w3.reshape(NE, 16, 128, 4, 256).transpose(0, 3, 2, 1, 4))
    sh["w2"] = np.ascontiguousarray(w2.reshape(NE, 8, 128, 4, 512).transpose(0, 3, 2, 1, 4))
    rope, cf, cb, sinit = _consts()
    sh["rope"], sh["cf"], sh["cb"], sh["sinit"] = rope, cf, cb, sinit
    sh["onesn"] = np.full((128, 128), 1.0 / 2048.0, np.float32)
    return sh


def kernel(**inputs):
    x = np.asarray(inputs["x"], dtype=np.float32)
    shared = _prep(inputs)
    nc = build("ABCDE", dbg=False)
    in_maps = []
    for b in range(8):
        m = dict(shared)
        m["x"] = np.ascontiguousarray(x[b])
        m["xT"] = np.ascontiguousarray(x[b].T)
        in_maps.append(m)
    res = run_bass_kernel_spmd(nc, in_maps, core_ids=list(range(8)))
    return np.stack([np.asarray(r["out"], dtype=np.float32) for r in res.results], axis=0)
```

```python
import numpy as np
import ml_dtypes
import concourse.bass as bass
import concourse.mybir as mybir
from concourse.bass_utils import run_bass_kernel_spmd

F32 = mybir.dt.float32
F32R = mybir.dt.float32r
BF16 = mybir.dt.bfloat16
I32 = mybir.dt.int32
AF = mybir.ActivationFunctionType
OP = mybir.AluOpType
AX = mybir.AxisListType

S = 2048
D = 2048
NH = 16
NE = 32
FF = 1024
CAP = 256
NS = NE * CAP
LN_EPS = 1e-5
ALPHA = 2.0 ** 0.25
SCALE = 128.0 ** -0.5
NEG = -30000.0
OOBROW = 2 * S + 100


class T:
    __slots__ = ("ap", "w", "r")

    def __init__(self, ap=None):
        self.ap = ap
        self.w = {}
        self.r = {}


class Eng:
    def __init__(self, name, strict_self):
        self.name = name
        self.key = name
        self.insts = []
        self.count = 0
        self.seen = {}
        self.strict = strict_self
        self.ring_vals = None
        self.ring_pos = 0


class Sched:
    def __init__(self, nc, es, nring_sp=12, nring_pool=8):
        self.nc = nc
        self.es = es
        self.nrot = 0
        self.e = {
            "pe": Eng("pe", False),
            "act": Eng("act", True),
            "dve": Eng("dve", True),
            "pool": Eng("pool", True),
            "sp": Eng("sp", False),
        }
        self.h = {"pe": nc.tensor, "act": nc.scalar, "dve": nc.vector, "pool": nc.gpsimd, "sp": nc.sync}
        self.e["sp"].ring_vals = [0] * nring_sp
        self.e["pool"].ring_vals = [0] * nring_pool
        self.sems = {}
        for en in ("pe", "act", "dve", "pool", "sp"):
            self.sems[en] = es.enter_context(nc.semaphore(f"s_{en}"))
        for qn in ("sp", "pool"):
            for idx in range(len(self.e[qn].ring_vals)):
                self.sems[(qn, idx)] = es.enter_context(nc.semaphore(f"r_{qn}{idx}"))

    def _waits(self, eng, reads, writes, extra=()):
        deps = {}

        def add(k, v):
            if deps.get(k, 0) < v:
                deps[k] = v

        for t in reads:
            for k, v in t.w.items():
                add(k, v)
        for t in writes:
            for k, v in t.w.items():
                add(k, v)
            for k, v in t.r.items():
                add(k, v)
        for k, v in extra:
            add(k, v)
        for k, v in deps.items():
            if k == eng.key and not eng.strict:
                continue
            if eng.seen.get(k, 0) < v:
                eng.seen[k] = v
                self.h[eng.name].wait_ge(self.sems[k], v)

    def op(self, en, fn, reads=(), writes=()):
        eng = self.e[en]
        self._waits(eng, reads, writes)
        if eng.count >= 6000:
            eng.seen[eng.key] = eng.count
            self.nrot += 1
            eng.key = (en, "ep", self.nrot)
            self.sems[eng.key] = self.es.enter_context(self.nc.semaphore(f"s_{en}_{self.nrot}"))
            eng.count = 0
        eng.count += 1
        v = eng.count
        fn(self.h[en]).then_inc(self.sems[eng.key], 1)
        for t in writes:
            t.w = {eng.key: v}
            t.r = {}
        for t in reads:
            t.r[eng.key] = v

    def dma(self, qn, fn, reads=(), writes=()):
        eng = self.e[qn]
        idx = eng.ring_pos % len(eng.ring_vals)
        eng.ring_pos += 1
        key = (qn, idx)
        prev = eng.ring_vals[idx]
        self._waits(eng, reads, writes, extra=((key, prev),) if prev else ())
        val = prev + 16
        eng.ring_vals[idx] = val
        fn(self.h[qn]).then_inc(self.sems[key], 16)
        for t in writes:
            t.w = {key: val}
            t.r = {}
        for t in reads:
            t.r[key] = val

    def fence(self):
        for en, eng in self.e.items():
            for fn_, f in self.e.items():
                if fn_ != en and f.count and eng.seen.get(f.key, 0) < f.count:
                    eng.seen[f.key] = f.count
                    self.h[en].wait_ge(self.sems[f.key], f.count)
            for qn in ("sp", "pool"):
                for idx, v in enumerate(self.e[qn].ring_vals):
                    if v and eng.seen.get((qn, idx), 0) < v:
                        eng.seen[(qn, idx)] = v
                        self.h[en].wait_ge(self.sems[(qn, idx)], v)
        for en, eng in self.e.items():
            if eng.count > 1500:
                eng.seen[eng.key] = eng.count
                self.nrot += 1
                eng.key = (en, "ep", self.nrot)
                self.sems[eng.key] = self.es.enter_context(self.nc.semaphore(f"s_{en}_{self.nrot}"))
                eng.count = 0

    def finish(self):
        for qn in ("sp", "pool"):
            eng = self.e[qn]
            for idx, v in enumerate(eng.ring_vals):
                if v and eng.seen.get((qn, idx), 0) < v:
                    self.h[qn].wait_ge(self.sems[(qn, idx)], v)


import itertools
_uid = itertools.count()


class Ring:
    def __init__(self, tiles):
        self.t = tiles
        self.i = 0

    def next(self):
        t = self.t[self.i % len(self.t)]
        self.i += 1
        return t


def r32(ap):
    return ap.bitcast(F32R)


def build(phases="ABCDE", dbg=False):
    nc = bass.Bass("TRN2", target_bir_lowering=False)
    nc.dge_precook = False
    from contextlib import ExitStack
    es = ExitStack()
    sc = Sched(nc, es)
    reg_ns = nc.gpsimd.to_reg(NS - 1)
    reg_y = nc.gpsimd.to_reg(2 * S - 1)

    def din(name, shape, dt):
        return nc.dram_tensor(name, list(shape), dt, kind="ExternalInput").ap()

    def dscr(name, shape, dt):
        kind = "ExternalOutput" if dbg else "Internal"
        return nc.dram_tensor(name, list(shape), dt, kind=kind).ap()

    xT_d = din("xT", [D, S], F32R)
    x_d = din("x", [S, D], F32)
    wA_d = din("wA", [32, 128, 16, 128], F32R)
    wV_d = din("wV", [8, 128, 16, 256], F32R)
    wG_d = din("wG", [64, 128, 16, 128], F32R)
    bq_d = din("bq", [128, 32], F32)
    bv_d = din("bv", [1, 2048], F32)
    bG_d = din("bG", [128, 64], F32)
    wo_d = din("wo", [16, 128, 16, 128], F32R)
    wp_d = din("wp", [16, 128, 16, 128], F32R)
    wout_d = din("wout", [4, 128, 16, 512], F32R)
    cvec_d = din("cvec", [128, 16, 36], F32)
    lnv_d = din("lnv", [4, 2048], F32)
    wr_d = din("wr", [128, 16, 36], F32)
    br_d = din("br", [1, 36], F32)
    ned = NE if "D" in phases else 1
    w1_d = din("w1", [ned, 4, 128, 16, 256], F32R)
    w3_d = din("w3", [ned, 4, 128, 16, 256], F32R)
    w2_d = din("w2", [ned, 4, 128, 8, 512], F32R)
    rope_d = din("rope", [128, 4096], F32)
    cf_d = din("cf", [128, 1024], F32)
    on_d = din("onesn", [128, 128], F32R)
    cb_d = din("cb", [128, 2688], BF16)
    sinit_d = din("sinit", [NS, 4], I32)
    out_d = nc.dram_tensor("out", [S, D], F32, kind="ExternalOutput").ap()

    qk_s = dscr("qk_s", [32, 128, S], BF16)
    v_s = dscr("v_s", [S, D], BF16)
    ot_s = dscr("ot_s", [16, 128, S], F32R)
    h_s = dscr("h_s", [S, D], F32)
    y_s = dscr("y_s", [2 * S, D], F32)
    stab_s = dscr("stab_s", [NS, 4], I32)
    T_qk = [T() for _ in range(32)]
    T_v = [T() for _ in range(8)]
    T_ot = [T() for _ in range(16)]
    T_h = [T() for _ in range(16)]
    T_y = T()
    T_stab = T()

    def sb(name, shape, dt):
        return es.enter_context(nc.sbuf_tensor(f"s{next(_uid)}_" + name, list(shape), dt))

    psum = [es.enter_context(nc.psum_tensor(f"ps{i}", [128, 512], F32)) for i in range(8)]
    PS = [T(p) for p in psum]

    cf = sb("cf", [128, 1024], F32)
    cbt = sb("cb", [128, 2688], BF16)
    T_cf, T_cb = T(cf), T(cbt)
    sc.dma("sp", lambda h: h.dma_start(out=cf[:], in_=cf_d), writes=[T_cf])
    sc.dma("sp", lambda h: h.dma_start(out=cbt[:], in_=cb_d), writes=[T_cb])
    onesn_t = sb("onesn", [128, 128], F32R)
    sc.dma("sp", lambda h: h.dma_start(out=onesn_t[:], in_=on_d), writes=[T_cf])
    ident32 = cf[:, 0:128]
    pastbias = cf[:, 128:256]
    notown = cf[:, 256:384]
    onesn = onesn_t[:, :]
    U32 = cf[:, 512:640]
    ones32 = cf[:, 640:768]
    slotbase = cf[:, 768:800]
    iota_p = cf[:, 800:801]
    rotT = cbt[:, 0:128]
    identb = cbt[:, 128:256]
    tri = cbt[:, 256:384]
    onesb = cbt[:, 384:512]
    blkind = cbt[:, 640:2688]

    if "A" in phases:
        with ExitStack() as ea:
            def sba(name, shape, dt):
                return ea.enter_context(nc.sbuf_tensor(f"s{next(_uid)}_" + name, list(shape), dt))
            xT = sba("xT", [128, 16, S], F32R)
            T_xT = [T() for _ in range(4)]
            for tc in range(4):
                sc.dma("sp", lambda h, tc=tc: h.dma_start(
                    out=xT[:, :, tc * 512:(tc + 1) * 512],
                    in_=xT_d.rearrange("(kc p) t -> p kc t", p=128)[:, :, tc * 512:(tc + 1) * 512]),
                    writes=[T_xT[tc]])
            with ExitStack() as e1:
                def sb1(name, shape, dt):
                    return e1.enter_context(nc.sbuf_tensor(f"s{next(_uid)}_" + name, list(shape), dt))
                rope = sb1("rope", [128, 4096], F32)
                T_rope = T()
                sc.dma("sp", lambda h: h.dma_start(out=rope[:], in_=rope_d), writes=[T_rope])
                bq = sb1("bq", [128, 32], F32)
                T_bq = T()
                sc.dma("sp", lambda h: h.dma_start(out=bq[:], in_=bq_d), writes=[T_bq])
                wring = Ring([T(sb1(f"wA{i}", [128, 16, 128], F32R)) for i in range(4)])
                qbr = Ring([T(sb1(f"qb{i}", [128, 512], BF16)) for i in range(3)])
                t1r = Ring([T(sb1(f"t1{i}", [128, 512], F32)) for i in range(2)])
                t2r = Ring([T(sb1(f"t2{i}", [128, 512], F32)) for i in range(2)])
                str_ = Ring([T(sb1(f"st{i}", [128, S], BF16)) for i in range(2)])
                psA = Ring(PS[0:3])
                psR = Ring(PS[3:5])
                def rope_tail(nb, tc, stg, qb, last):
                    ps2 = psR.next()
                    sc.op("pe", lambda h: h.matmul(ps2.ap[:, :], lhsT=rotT, rhs=qb.ap[:, :], start=True, stop=True),
                          reads=[qb, T_cb], writes=[ps2])
                    t1 = t1r.next()
                    t2 = t2r.next()
                    sc.op("pool", lambda h: h.tensor_tensor(
                        out=t1.ap[:, :], in0=qb.ap[:, :], in1=rope[:, tc * 512:(tc + 1) * 512], op=OP.mult),
                        reads=[qb, T_rope], writes=[t1])
                    sc.op("dve", lambda h: h.tensor_tensor(
                        out=t2.ap[:, :], in0=ps2.ap[:, :], in1=rope[:, 2048 + tc * 512:2048 + (tc + 1) * 512],
                        op=OP.mult), reads=[ps2, T_rope], writes=[t2])
                    sc.op("dve", lambda h: h.tensor_tensor(
                        out=stg.ap[:, tc * 512:(tc + 1) * 512], in0=t1.ap[:, :], in1=t2.ap[:, :], op=OP.add),
                        reads=[t1, t2], writes=[stg])
                    if last:
                        sc.dma("pool", lambda h: h.dma_start(out=qk_s[nb], in_=stg.ap[:, :]), reads=[stg], writes=[T_qk[nb]])

                pend = None
                for nb in range(32):
                    W = wring.next()
                    sc.dma("sp", lambda h, W=W, nb=nb: h.dma_start(out=W.ap[:], in_=wA_d[nb]), writes=[W])
                    stg = str_.next()
                    for tc in range(4):
                        ps = psA.next()
                        for kc in range(16):
                            sc.op("pe", lambda h, ps=ps, W=W, kc=kc, tc=tc: h.matmul(
                                ps.ap[:, :], lhsT=r32(W.ap[:, kc, :]), rhs=r32(xT[:, kc, tc * 512:(tc + 1) * 512]),
                                start=(kc == 0), stop=(kc == 15)),
                                reads=[W, T_xT[tc]], writes=[ps])
                        qb = qbr.next()
                        sc.op("act", lambda h, qb=qb, ps=ps, nb=nb: h.activation(
                            out=qb.ap[:, :], in_=ps.ap[:, :], func=AF.Identity, bias=bq[:, nb:nb + 1], scale=1.0),
                            reads=[ps, T_bq], writes=[qb])
                        if pend is not None:
                            rope_tail(*pend)
                        pend = (nb, tc, stg, qb, tc == 3)
                rope_tail(*pend)
            sc.fence()
            with ExitStack() as e2:
                def sb2(name, shape, dt):
                    return e2.enter_context(nc.sbuf_tensor(f"s{next(_uid)}_" + name, list(shape), dt))
                bv = sb2("bv", [128, 2048], F32)
                T_bv = T()
                sc.dma("sp", lambda h: h.dma_start(out=bv[:], in_=bv_d.partition_broadcast(128)), writes=[T_bv])
                wvr = Ring([T(sb2(f"wV{i}", [128, 16, 256], F32R)) for i in range(2)])
                vst = Ring([T(sb2(f"vst{i}", [128, 16, 256], BF16)) for i in range(2)])
                psA = Ring(PS[0:4])
                for vb in range(8):
                    W = wvr.next()
                    sc.dma("sp", lambda h, W=W, vb=vb: h.dma_start(out=W.ap[:], in_=wV_d[vb]), writes=[W])
                    stg = vst.next()
                    for tt in range(16):
                        ps = psA.next()
                        for kc in range(16):
                            sc.op("pe", lambda h, ps=ps, W=W, kc=kc, tt=tt: h.matmul(
                                ps.ap[:, 0:256], lhsT=r32(xT[:, kc, tt * 128:(tt + 1) * 128]), rhs=r32(W.ap[:, kc, :]),
                                start=(kc == 0), stop=(kc == 15)),
                                reads=[W, T_xT[tt // 4]], writes=[ps])
                        sc.op("dve", lambda h, stg=stg, ps=ps, tt=tt, vb=vb: h.tensor_tensor(
                            out=stg.ap[:, tt, :], in0=ps.ap[:, 0:256], in1=bv[:, vb * 256:(vb + 1) * 256], op=OP.add),
                            reads=[ps, T_bv], writes=[stg])
                    sc.dma("pool", lambda h, stg=stg, vb=vb: h.dma_start(
                        out=v_s.rearrange("(tt p) c -> p tt c", p=128)[:, :, vb * 256:(vb + 1) * 256],
                        in_=stg.ap[:, :, :]), reads=[stg], writes=[T_v[vb]])

            sc.fence()
        sc.fence()
    if "B" in phases:
        with ExitStack() as eb:
            def sbb(name, shape, dt):
                return eb.enter_context(nc.sbuf_tensor(f"s{next(_uid)}_" + name, list(shape), dt))
            zt = T(sbb("zt", [128, 2048], F32))
            sc.op("pool", lambda h: h.memset(zt.ap[:, :], 0.0), writes=[zt])

            def zero_fill(hd):
                for i in (2 * hd, 2 * hd + 1):
                    sc.dma("sp", lambda h, i=i: h.dma_start(out=y_s[i * 128:(i + 1) * 128, :], in_=zt.ap[:, :]),
                           reads=[zt], writes=[T()])
            qr = Ring([T(sbb(f"q{i}", [128, S], BF16)) for i in range(2)])
            kr = Ring([T(sbb(f"k{i}", [128, S], BF16)) for i in range(2)])
            vr = Ring([T(sbb(f"v{i}", [128, 16, 132], BF16)) for i in range(2)])
            for t in vr.t:
                sc.op("pool", lambda h, t=t: h.memset(t.ap[:, :, 128:132], 1.0), writes=[t])
            ks32 = T(sbb("ks32", [128, 8], F32))
            ksb = T(sbb("ksb", [128, 8], BF16))
            gm = T(sbb("gm", [128, 128], F32))
            top8 = T(sbb("top8", [128, 128], F32))
            lt = T(sbb("lt", [128, 128], F32))
            Bpr = Ring([T(sbb(f"Bpad{i}", [128, 16, 128], F32)) for i in range(2)])
            for t in Bpr.t:
                sc.op("pool", lambda h, t=t: h.memset(t.ap[:, :, :], 0.0), writes=[t])
            BTr = Ring([T(sbb(f"BT{i}", [128, 512], BF16)) for i in range(2)])
            PT = [[T() for _ in range(16)] for _ in range(2)]
            PTbuf = [sbb(f"PT{i}", [128, 16, 512], BF16) for i in range(2)]
            rinvr = Ring([T(sbb(f"rinv{i}", [128, 512], F32)) for i in range(2)])
            OTr = Ring([T(sbb(f"OTst{i}", [128, S], F32R)) for i in range(2)])
            psST = Ring(PS[0:4])
            psO = Ring(PS[4:5])
            psOT = Ring(PS[5:6])
            psG = PS[6]
            psBT = PS[7]
            def prep_load(hd):
                qT, kT, V = qr.next(), kr.next(), vr.next()
                sc.dma("sp", lambda h: h.dma_start(out=qT.ap[:, :], in_=qk_s[hd]), reads=[T_qk[hd]], writes=[qT])
                sc.dma("sp", lambda h: h.dma_start(out=kT.ap[:, :], in_=qk_s[16 + hd]), reads=[T_qk[16 + hd]], writes=[kT])
                sc.dma("sp", lambda h: h.dma_start(
                    out=V.ap[:, :, 0:128],
                    in_=v_s.rearrange("(tt p) c -> p tt c", p=128)[:, :, hd * 128:(hd + 1) * 128]),
                    reads=[T_v[hd // 2]], writes=[V])
                zero_fill(hd)
                return qT, kT, V

            def prep(hd, loaded):
                qT, kT, V = loaded
                Bpad = Bpr.next()
                sc.op("dve", lambda h: h.tensor_reduce(
                    out=ks32.ap[:, :], in_=kT.ap[:, :].rearrange("p (n k) -> p n k", k=256), axis=AX.X, op=OP.add),
                    reads=[kT], writes=[ks32])
                sc.op("dve", lambda h: h.tensor_copy(out=ksb.ap[:, :], in_=ks32.ap[:, :]), reads=[ks32], writes=[ksb])
                for i in range(16):
                    sc.op("pe", lambda h, i=i: h.matmul(
                        psG.ap[:, i * 8:(i + 1) * 8], lhsT=qT.ap[:, i * 128:(i + 1) * 128], rhs=ksb.ap[:, :],
                        start=True, stop=True), reads=[qT, ksb], writes=[psG])
                sc.op("dve", lambda h: h.tensor_tensor(out=gm.ap[:, :], in0=psG.ap[:, 0:128], in1=pastbias, op=OP.add),
                      reads=[psG, T_cf], writes=[gm])
                for i in range(16):
                    sc.op("dve", lambda h, i=i: h.max(out=top8.ap[:, i * 8:(i + 1) * 8], in_=gm.ap[:, i * 8:(i + 1) * 8]),
                          reads=[gm], writes=[top8])
                sc.op("dve", lambda h: h.tensor_tensor(
                    out=lt.ap[:, :].rearrange("p (i n) -> p i n", n=8),
                    in0=gm.ap[:, :].rearrange("p (i n) -> p i n", n=8),
                    in1=top8.ap[:, :].rearrange("p (i n) -> p i n", n=8)[:, :, 2:3].to_broadcast([128, 16, 8]),
                    op=OP.is_lt), reads=[gm, top8], writes=[lt])
                sc.op("dve", lambda h: h.tensor_tensor(
                    out=Bpad.ap[:, :, 0:8],
                    in0=lt.ap[:, :].rearrange("p (i n) -> p i n", n=8),
                    in1=notown.rearrange("p (i n) -> p i n", n=8), op=OP.mult),
                    reads=[lt, T_cf], writes=[Bpad])
                return qT, kT, V, OTr.next(), Bpad

            def stage1(ctx, c, par):
                qT, kT, V, OTst, Bpad = ctx
                for ii in range(4):
                    sc.op("pe", lambda h, ii=ii: h.transpose(
                        out=psBT.ap[:, ii * 128:(ii + 1) * 128], in_=Bpad.ap[:, 4 * c + ii, :], identity=ident32),
                        reads=[Bpad, T_cf], writes=[psBT])
                BT = BTr.next()
                sc.op("act", lambda h: h.copy(out=BT.ap[:, :], in_=psBT.ap[:, :]), reads=[psBT], writes=[BT])
                for j in range(4 * c + 4):
                    q0 = max(512 * c, 128 * j)
                    off = q0 - 512 * c
                    diag = j >= 4 * c
                    st = psST.next()
                    sc.op("pe", lambda h, st=st, j=j, q0=q0, off=off: h.matmul(
                        st.ap[:, off:512], lhsT=kT.ap[:, j * 128:(j + 1) * 128], rhs=qT.ap[:, q0:512 * c + 512],
                        start=True, stop=False), reads=[kT, qT], writes=[st])
                    sc.op("pe", lambda h, st=st, j=j, off=off, diag=diag: h.matmul(
                        st.ap[:, off:512], lhsT=blkind[:, j * 128:(j + 1) * 128], rhs=BT.ap[:, off:512],
                        start=False, stop=(not diag)), reads=[BT, T_cb], writes=[st])
                    if diag:
                        sc.op("pe", lambda h, st=st, off=off: h.matmul(
                            st.ap[:, off:off + 128], lhsT=identb, rhs=tri, start=False, stop=True,
                            skip_group_check=True), reads=[T_cb], writes=[st])
                    sc.op("act", lambda h, st=st, j=j, off=off: h.activation(
                        out=PTbuf[par][:, j, off:512], in_=st.ap[:, off:512], func=AF.Exp, scale=SCALE),
                        reads=[st], writes=[PT[par][j]])

            def stage2(ctx, c, par, hd):
                qT, kT, V, OTst, Bpad = ctx
                otp = psO.next()
                rsp = psOT.next()
                nj = 4 * c + 4
                for j in range(nj):
                    off = max(512 * c, 128 * j) - 512 * c
                    sc.op("pe", lambda h, j=j, off=off: h.matmul(
                        otp.ap[:, off:512], lhsT=V.ap[:, j, 0:128], rhs=PTbuf[par][:, j, off:512],
                        start=(j == 0), stop=(j == nj - 1), skip_group_check=True), reads=[PT[par][j], V], writes=[otp])
                    sc.op("pe", lambda h, j=j, off=off: h.matmul(
                        rsp.ap[:, off:512], lhsT=onesb, rhs=PTbuf[par][:, j, off:512],
                        start=(j == 0), stop=(j == nj - 1), skip_group_check=True), reads=[PT[par][j], T_cb], writes=[rsp])
                rinv = rinvr.next()
                sc.op("dve", lambda h: h.reciprocal(out=rinv.ap[:, :], in_=rsp.ap[:, :]), reads=[rsp], writes=[rinv])
                sc.op("dve", lambda h: h.tensor_tensor(
                    out=OTst.ap[:, c * 512:(c + 1) * 512], in0=otp.ap[:, :], in1=rinv.ap[:, :], op=OP.mult),
                    reads=[otp, rinv], writes=[OTst])
                if c == 3:
                    sc.dma("pool", lambda h: h.dma_start(out=ot_s[hd], in_=OTst.ap[:, :]), reads=[OTst], writes=[T_ot[hd]])

            items = [(hd, c) for hd in range(NH) for c in range(4)]
            ctxs = {}
            prev = None
            ctxs[0] = prep(0, prep_load(0))
            loaded = None
            for k, (hd, c) in enumerate(items):
                stage1(ctxs[hd], c, k % 2)
                if prev is not None:
                    stage2(ctxs[prev[0]], prev[1], (k - 1) % 2, prev[0])
                if c == 0 and hd + 1 < NH:
                    loaded = prep_load(hd + 1)
                if c == 1 and hd + 1 < NH:
                    ctxs[hd + 1] = prep(hd + 1, loaded)
                prev = (hd, c)
            stage2(ctxs[prev[0]], prev[1], (len(items) - 1) % 2, prev[0])
        sc.fence()
    if "C" in phases:
        with ExitStack() as ec:
            def sbc(name, shape, dt):
                return ec.enter_context(nc.sbuf_tensor(f"s{next(_uid)}_" + name, list(shape), dt))
            cvec = sbc("cvec", [128, 16, 36], F32)
            bG = sbc("bG", [128, 64], F32)
            wr = sbc("wr", [128, 16, 36], F32)
            brt = sbc("br", [128, 36], F32)
            T_cc = T()
            sc.dma("sp", lambda h: h.dma_start(out=cvec[:], in_=cvec_d), writes=[T_cc])
            sc.dma("sp", lambda h: h.dma_start(out=bG[:], in_=bG_d), writes=[T_cc])
            sc.dma("sp", lambda h: h.dma_start(out=wr[:], in_=wr_d), writes=[T_cc])
            sc.dma("sp", lambda h: h.dma_start(out=brt[:], in_=br_d.partition_broadcast(128)), writes=[T_cc])
            sc.dma("pool", lambda h: h.dma_start(out=stab_s, in_=sinit_d), writes=[T_stab])
            halo = T(sbc("halo", [128, 16, 32], F32))
            sc.op("pool", lambda h: h.memset(halo.ap[:, :, :], 0.0), writes=[halo])
            Acum = T(sbc("Acum", [128, 32], F32))
            sc.op("pool", lambda h: h.memset(Acum.ap[:, :], 0.0), writes=[Acum])
            mT = sbc("mT", [128, 16, 512], F32)
            T_mT = [T() for _ in range(16)]
            psA = Ring(PS[0:2])
            psB = Ring(PS[2:4])
            psS = [PS[4], PS[5]]
            psX = Ring(PS[6:8])

            def gemm_fm(wring, wsrc, ps, act, T_act, W=None):
                if W is None:
                    W = wring.next()
                    sc.dma("sp", lambda h, W=W: h.dma_start(out=W.ap[:], in_=wsrc), writes=[W])
                for kc in range(16):
                    sc.op("pe", lambda h, W=W, kc=kc: h.matmul(
                        ps.ap[:, :], lhsT=r32(W.ap[:, kc, :]), rhs=r32(act[:, kc, :]),
                        start=(kc == 0), stop=(kc == 15)),
                        reads=[W, (T_act[kc * len(T_act) // 16] if isinstance(T_act, list) else T_act)], writes=[ps])

            for c in range(4):
                t0 = c * 512
                e12 = ExitStack()
                def sb12(name, shape, dt):
                    return e12.enter_context(nc.sbuf_tensor(f"s{next(_uid)}_" + name, list(shape), dt))
                xTc = sb12("xTc", [128, 16, 512], F32R)
                T_xTc = [T() for _ in range(4)]
                OTc = sb12("OTc", [128, 16, 512], F32R)
                T_OTc = T()
                wring = Ring([T(sb12(f"wC{i}", [128, 16, 128], F32R)) for i in range(3)])
                sgr = Ring([T(sb12(f"sg{i}", [128, 512], F32)) for i in range(2)])
                tmr = Ring([T(sb12(f"tm{i}", [128, 512], F32)) for i in range(2)])
                cT = sb12("cT", [128, 16, 512], F32)
                T_cT = [T() for _ in range(16)]
                hTr = Ring([T(sb12(f"hT{i}", [128, 544], F32)) for i in range(4)])
                dgr = Ring([T(sb12(f"dg{i}", [128, 16, 128], F32)) for i in range(2)])
                mean = T(sb12("mean", [128, 512], F32))
                rstd = T(sb12("rstd", [128, 512], F32))
                Wfirst = []
                for kq in range(4):
                    sc.dma("sp", lambda h, kq=kq: h.dma_start(
                        out=xTc[:, 4 * kq:4 * kq + 4, :],
                        in_=xT_d.rearrange("(kc p) t -> p kc t", p=128)[:, 4 * kq:4 * kq + 4, t0:t0 + 512]),
                        writes=[T_xTc[kq]])
                    if kq == 0:
                        for src in (wG_d[0], wG_d[16]):
                            W = wring.next()
                            sc.dma("sp", lambda h, W=W, src=src: h.dma_start(out=W.ap[:], in_=src), writes=[W])
                            Wfirst.append(W)

                def conv_pe(cc, hT):
                    halves = []
                    for hh in range(2):
                        j0 = 16 * hh
                        n = 16 if hh == 0 else 15
                        dg = dgr.next()
                        sc.op("pool", lambda h, dg=dg, j0=j0, n=n: h.tensor_tensor(
                            out=r32(dg.ap[:, 0:n, :]), in0=ident32.unsqueeze(1).to_broadcast([128, n, 128]),
                            in1=cvec[:, cc, j0:j0 + n].unsqueeze(2).to_broadcast([128, n, 128]), op=OP.mult),
                            reads=[T_cf, T_cc], writes=[dg])
                        halves.append(dg)
                    ps = psX.next()
                    for j in range(31):
                        dg = halves[j // 16]
                        sc.op("pe", lambda h, j=j, dg=dg: h.matmul(
                            ps.ap[:, :], lhsT=r32(dg.ap[:, j % 16, :]), rhs=r32(hT.ap[:, 2 + j:514 + j]),
                            start=(j == 0), stop=(j == 30)), reads=[dg, hT], writes=[ps])
                    sc.op("act", lambda h: h.activation(
                        out=r32(cT[:, cc, :]), in_=ps.ap[:, :], func=AF.Identity, bias=cvec[:, cc, 31:32], scale=1.0),
                        reads=[ps, T_cc], writes=[T_cT[cc]])
                    sc.op("pool", lambda h: h.tensor_copy(out=halo.ap[:, cc, :], in_=hT.ap[:, 512:544]),
                          reads=[hT], writes=[halo])

                pend = None
                for cc in range(16):
                    hT = hTr.next()
                    sc.op("pool", lambda h, cc=cc, hT=hT: h.tensor_copy(out=r32(hT.ap[:, 0:32]), in_=halo.ap[:, cc, :]),
                          reads=[halo], writes=[hT])
                    pa, pg = psA.next(), psB.next()
                    gemm_fm(wring, wG_d[cc], pa, xTc, T_xTc, W=(Wfirst[0] if cc == 0 else None))
                    gemm_fm(wring, wG_d[16 + cc], pg, xTc, T_xTc, W=(Wfirst[1] if cc == 0 else None))
                    if cc == 2:
                        sc.dma("sp", lambda h: h.dma_start(
                            out=OTc[:, :, :], in_=ot_s.rearrange("k p t -> p k t")[:, :, t0:t0 + 512]),
                            reads=T_ot, writes=[T_OTc])
                    sg = sgr.next()
                    sc.op("act", lambda h, sg=sg, pg=pg, cc=cc: h.activation(
                        out=sg.ap[:, :], in_=pg.ap[:, :], func=AF.Sigmoid, bias=bG[:, 16 + cc:17 + cc], scale=1.0),
                        reads=[pg, T_cc], writes=[sg])
                    sc.op("dve", lambda h, sg=sg, pa=pa, cc=cc, hT=hT: h.scalar_tensor_tensor(
                        out=r32(hT.ap[:, 32:544]), in0=pa.ap[:, :], scalar=bG[:, cc:cc + 1], in1=sg.ap[:, :],
                        op0=OP.add, op1=OP.mult), reads=[pa, sg, T_cc, hT], writes=[hT])
                    if pend is not None:
                        conv_pe(*pend)
                    pend = (cc, hT)
                conv_pe(*pend)

                pm, pq = psS
                for cc in range(16):
                    sc.op("pe", lambda h, cc=cc: h.matmul(pm.ap[:, :], lhsT=r32(onesn), rhs=r32(cT[:, cc, :]),
                                                         start=(cc == 0), stop=(cc == 15)),
                          reads=[T_cT[cc], T_cf], writes=[pm])
                for cc in range(16):
                    sq = hTr.next()
                    sc.op("act", lambda h, sq=sq, cc=cc: h.activation(out=r32(sq.ap[:, 0:512]), in_=cT[:, cc, :], func=AF.Square),
                          reads=[T_cT[cc]], writes=[sq])
                    sc.op("pe", lambda h, sq=sq, cc=cc: h.matmul(pq.ap[:, :], lhsT=r32(onesn), rhs=r32(sq.ap[:, 0:512]),
                                                                start=(cc == 0), stop=(cc == 15)),
                          reads=[sq, T_cf], writes=[pq])
                sc.op("dve", lambda h: h.tensor_copy(out=mean.ap[:, :], in_=pm.ap[:, :]), reads=[pm], writes=[mean])
                sc.op("dve", lambda h: h.tensor_tensor(out=rstd.ap[:, :], in0=mean.ap[:, :], in1=mean.ap[:, :], op=OP.mult),
                      reads=[mean], writes=[rstd])
                sc.op("dve", lambda h: h.tensor_tensor(out=rstd.ap[:, :], in0=pq.ap[:, :], in1=rstd.ap[:, :], op=OP.subtract),
                      reads=[pq, rstd], writes=[rstd])
                sc.op("dve", lambda h: h.tensor_scalar(out=rstd.ap[:, :], in0=rstd.ap[:, :], scalar1=LN_EPS, scalar2=None,
                                                       op0=OP.add), reads=[rstd], writes=[rstd])
                sc.op("act", lambda h: h.activation(out=rstd.ap[:, :], in_=rstd.ap[:, :], func=AF.Sqrt),
                      reads=[rstd], writes=[rstd])
                sc.op("dve", lambda h: h.reciprocal(out=rstd.ap[:, :], in_=rstd.ap[:, :]), reads=[rstd], writes=[rstd])

                for j in range(16):
                    cc = j
                    en = "dve" if cc % 2 == 0 else "pool"
                    nt = hTr.next()
                    sc.op(en, lambda h, cc=cc, nt=nt: h.tensor_tensor(out=r32(nt.ap[:, 0:512]), in0=cT[:, cc, :], in1=mean.ap[:, :],
                                                                      op=OP.subtract), reads=[T_cT[cc], mean], writes=[nt])
                    sc.op(en, lambda h, cc=cc, nt=nt: h.tensor_tensor(out=r32(nt.ap[:, 0:512]), in0=nt.ap[:, 0:512], in1=rstd.ap[:, :],
                                                                      op=OP.mult), reads=[nt, rstd], writes=[nt])
                    sc.op("act", lambda h, cc=cc, nt=nt: h.activation(
                        out=r32(cT[:, cc, :]), in_=nt.ap[:, 0:512], func=AF.Silu, bias=cvec[:, cc, 33:34], scale=cvec[:, cc, 32:33]),
                        reads=[nt, T_cc], writes=[T_cT[cc]])
                    py, pg = psA.next(), psB.next()
                    gemm_fm(wring, wo_d[j], py, OTc, T_OTc)
                    gemm_fm(wring, wG_d[32 + j], pg, xTc, T_xTc)
                    sg = sgr.next()
                    sc.op("act", lambda h, sg=sg, pg=pg, j=j: h.activation(
                        out=sg.ap[:, :], in_=pg.ap[:, :], func=AF.Sigmoid, bias=bG[:, 32 + j:33 + j], scale=1.0),
                        reads=[pg, T_cc], writes=[sg])
                    sc.op("dve", lambda h, sg=sg, py=py, j=j: h.tensor_tensor(
                        out=r32(mT[:, j, :]), in0=py.ap[:, :], in1=sg.ap[:, :], op=OP.mult),
                        reads=[py, sg], writes=[T_mT[j]])
                for j in range(16):
                    py, pg = psA.next(), psB.next()
                    gemm_fm(wring, wp_d[j], py, cT, T_cT)
                    gemm_fm(wring, wG_d[48 + j], pg, xTc, T_xTc)
                    sg = sgr.next()
                    sc.op("act", lambda h, sg=sg, pg=pg, j=j: h.activation(
                        out=sg.ap[:, :], in_=pg.ap[:, :], func=AF.Sigmoid, bias=bG[:, 48 + j:49 + j], scale=1.0),
                        reads=[pg, T_cc], writes=[sg])
                    tm = tmr.next()
                    sc.op("dve", lambda h, sg=sg, py=py, j=j, tm=tm: h.scalar_tensor_tensor(
                        out=tm.ap[:, :], in0=py.ap[:, :], scalar=cvec[:, j, 34:35], in1=sg.ap[:, :],
                        op0=OP.add, op1=OP.mult), reads=[py, sg, T_cc], writes=[tm])
                    sc.op("pool", lambda h, tm=tm, j=j: h.tensor_tensor(
                        out=r32(mT[:, j, :]), in0=mT[:, j, :], in1=tm.ap[:, :], op=OP.add),
                        reads=[tm, T_mT[j]], writes=[T_mT[j]])
                e12.close()
                sc.fence()
                with ExitStack() as e3:
                    def sb3(name, shape, dt):
                        return e3.enter_context(nc.sbuf_tensor(f"s{next(_uid)}_" + name, list(shape), dt))
                    wor = Ring([T(sb3(f"wout{i}", [128, 16, 512], F32R)) for i in range(2)])
                    lng = sb3("lng", [128, 2048], F32)
                    lnb = sb3("lnb", [128, 2048], F32)
                    T_lng, T_lnb = T(), T()
                    xr = [T(sb3(f"xt{i}", [128, 2048], F32)) for i in range(4)]
                    zr = xr
                    Wpre = []
                    for cb in range(2):
                        W = wor.next()
                        sc.dma("sp", lambda h, W=W, cb=cb: h.dma_start(out=W.ap[:], in_=wout_d[cb]), writes=[W])
                        Wpre.append(W)
                        if cb == 0:
                            for t in range(4):
                                sc.dma("sp", lambda h, t=t, t0=t0: h.dma_start(
                                    out=xr[t].ap[:, :], in_=x_d[t0 + t * 128:t0 + (t + 1) * 128, :]), writes=[xr[t]])
                    sc.dma("sp", lambda h: h.dma_start(out=lng[:], in_=lnv_d[0:1, :].partition_broadcast(128)), writes=[T_lng])
                    sc.dma("sp", lambda h: h.dma_start(out=lnb[:], in_=lnv_d[1:2, :].partition_broadcast(128)), writes=[T_lnb])
                    for cb in range(4):
                        if cb < 2:
                            W = Wpre[cb]
                        else:
                            W = wor.next()
                            sc.dma("sp", lambda h, W=W, cb=cb: h.dma_start(out=W.ap[:], in_=wout_d[cb]), writes=[W])
                        for t in range(4):
                            ps = psA.next() if t % 2 == 0 else psB.next()
                            for kc in range(16):
                                sc.op("pe", lambda h, ps=ps, W=W, kc=kc, t=t: h.matmul(
                                    ps.ap[:, :], lhsT=r32(mT[:, kc, t * 128:(t + 1) * 128]), rhs=r32(W.ap[:, kc, :]),
                                    start=(kc == 0), stop=(kc == 15)), reads=[W, T_mT[kc]], writes=[ps])
                            sc.op("dve", lambda h, ps=ps, t=t, cb=cb: h.scalar_tensor_tensor(
                                out=zr[t].ap[:, cb * 512:(cb + 1) * 512], in0=xr[t].ap[:, cb * 512:(cb + 1) * 512],
                                scalar=ALPHA, in1=ps.ap[:, :], op0=OP.mult, op1=OP.add),
                                reads=[ps, xr[t]], writes=[zr[t]])
                    def mk_small(t):
                        d = {}
                        for nm, shp, dt in (("st6", [128, 4, 6], F32), ("mv", [128, 2], F32), ("rs", [128, 2], F32),
                                            ("lg", [128, 36], F32), ("sm", [128, 64], F32), ("oh", [128, 4], F32),
                                            ("esel", [128, 8], F32), ("t8", [128, 8], F32), ("mk", [128, 2, 8], F32),
                                            ("Ak", [128, 2, 32], F32), ("Asum", [128, 32], F32), ("val", [128, 32], F32),
                                            ("ov", [128, 32], F32), ("dst", [128, 2], F32), ("dsti", [128, 2], I32),
                                            ("pay", [128, 2, 4], I32), ("tokf", [128, 2], F32)):
                            d[nm] = T(sb3(f"{nm}{t}", shp, dt))
                        return d
                    small = [mk_small(t) for t in range(4)]
                    hTts = [T(sb3(f"hTt{i}", [128, 16, 128], F32)) for i in range(2)]

                    def tail1(t):
                        g = c * 4 + t
                        z = zr[t]
                        d = small[t]
                        st6, mv, rs, lg, sm, oh, esel, t8, mk, Ak, Asum = (d[k] for k in (
                            "st6", "mv", "rs", "lg", "sm", "oh", "esel", "t8", "mk", "Ak", "Asum"))
                        hTt = hTts[t % 2]
                        for q4 in range(4):
                            sc.op("dve", lambda h, q4=q4: h.bn_stats(out=st6.ap[:, q4, :], in_=z.ap[:, q4 * 512:(q4 + 1) * 512]),
                                  reads=[z], writes=[st6])
                        sc.op("dve", lambda h: h.bn_aggr(out=mv.ap[:, :], in_=st6.ap[:, :, :].rearrange("p a b -> p (a b)")),
                              reads=[st6], writes=[mv])
                        sc.op("dve", lambda h: h.tensor_scalar(out=rs.ap[:, 0:1], in0=mv.ap[:, 1:2], scalar1=LN_EPS, scalar2=None,
                                                               op0=OP.add), reads=[mv], writes=[rs])
                        yield
                        sc.op("act", lambda h: h.activation(out=rs.ap[:, 0:1], in_=rs.ap[:, 0:1], func=AF.Sqrt), reads=[rs], writes=[rs])
                        yield
                        sc.op("dve", lambda h: h.reciprocal(out=rs.ap[:, 0:1], in_=rs.ap[:, 0:1]), reads=[rs], writes=[rs])
                        sc.op("dve", lambda h: h.tensor_scalar(out=rs.ap[:, 1:2], in0=mv.ap[:, 0:1], scalar1=rs.ap[:, 0:1], scalar2=-1.0,
                                                               op0=OP.mult, op1=OP.mult), reads=[mv, rs], writes=[rs])
                        yield
                        sc.op("act", lambda h: h.activation(out=z.ap[:, :], in_=z.ap[:, :], func=AF.Identity,
                                                            scale=rs.ap[:, 0:1], bias=rs.ap[:, 1:2]), reads=[z, rs], writes=[z])
                        yield
                        sc.op("pool", lambda h: h.tensor_tensor(out=z.ap[:, :], in0=z.ap[:, :], in1=lng[:, :], op=OP.mult),
                              reads=[z, T_lng], writes=[z])
                        yield
                        sc.op("dve", lambda h: h.tensor_tensor(out=z.ap[:, :], in0=z.ap[:, :], in1=lnb[:, :], op=OP.add),
                              reads=[z, T_lnb], writes=[z])
                        sc.dma("pool", lambda h: h.dma_start(out=h_s[g * 128:(g + 1) * 128, :], in_=z.ap[:, :]),
                               reads=[z], writes=[T_h[g]])
                        yield
                        for k4 in range(4):
                            tp = psX.next()
                            for ii in range(4):
                                kc = k4 * 4 + ii
                                sc.op("pe", lambda h, tp=tp, ii=ii, kc=kc: h.transpose(
                                    out=tp.ap[:, ii * 128:(ii + 1) * 128], in_=z.ap[:, kc * 128:(kc + 1) * 128], identity=ident32),
                                    reads=[z, T_cf], writes=[tp])
                            sc.op("act", lambda h, tp=tp, k4=k4: h.copy(
                                out=hTt.ap[:, k4 * 4:(k4 + 1) * 4, :].rearrange("p a b -> p (a b)"), in_=tp.ap[:, :]),
                                reads=[tp], writes=[hTt])
                        pl = psS[0]
                        for kc in range(16):
                            sc.op("pe", lambda h, kc=kc: h.matmul(pl.ap[:, t * 64:t * 64 + 36], lhsT=hTt.ap[:, kc, :], rhs=wr[:, kc, :],
                                                                  start=(kc == 0), stop=(kc == 15)),
                                  reads=[hTt, T_cc], writes=[pl])
                        yield
                        sc.op("dve", lambda h: h.tensor_tensor(out=lg.ap[:, :], in0=pl.ap[:, t * 64:t * 64 + 36], in1=brt[:, :], op=OP.add),
                              reads=[pl, T_cc], writes=[lg])
                        sc.op("dve", lambda h: h.tensor_reduce(out=sm.ap[:, 0:1], in_=lg.ap[:, 0:4], axis=AX.X, op=OP.max),
                              reads=[lg], writes=[sm])
                        sc.op("dve", lambda h: h.tensor_scalar(out=oh.ap[:, :], in0=lg.ap[:, 0:4], scalar1=sm.ap[:, 0:1], scalar2=None,
                                                               op0=OP.is_equal), reads=[lg, sm], writes=[oh])
                        sc.op("dve", lambda h: h.tensor_scalar(out=sm.ap[:, 1:2], in0=sm.ap[:, 0:1], scalar1=-1.0, scalar2=None,
                                                               op0=OP.mult), reads=[sm], writes=[sm])
                        sc.op("dve", lambda h: h.tensor_scalar(out=esel.ap[:, :], in0=lg.ap[:, 4:12], scalar1=oh.ap[:, 0:1], scalar2=None,
                                                               op0=OP.mult), reads=[lg, oh], writes=[esel])
                        for gi in range(1, 4):
                            sc.op("dve", lambda h, gi=gi: h.scalar_tensor_tensor(
                                out=esel.ap[:, :], in0=lg.ap[:, 4 + 8 * gi:12 + 8 * gi], scalar=oh.ap[:, gi:gi + 1], in1=esel.ap[:, :],
                                op0=OP.mult, op1=OP.add), reads=[lg, oh, esel], writes=[esel])
                        sc.op("dve", lambda h: h.max(out=t8.ap[:, :], in_=esel.ap[:, :]), reads=[esel], writes=[t8])
                        for k in range(2):
                            sc.op("dve", lambda h, k=k: h.tensor_scalar(out=mk.ap[:, k, :], in0=esel.ap[:, :], scalar1=t8.ap[:, k:k + 1],
                                                                        scalar2=None, op0=OP.is_equal), reads=[esel, t8], writes=[mk])
                        sc.op("dve", lambda h: h.tensor_tensor(out=sm.ap[:, 4:5], in0=t8.ap[:, 1:2], in1=t8.ap[:, 0:1], op=OP.subtract),
                              reads=[t8], writes=[sm])
                        yield
                        sc.op("act", lambda h: h.activation(out=sm.ap[:, 10:14], in_=lg.ap[:, 0:4], func=AF.Exp, bias=sm.ap[:, 1:2],
                                                            scale=1.0), reads=[lg, sm], writes=[sm])
                        sc.op("act", lambda h: h.activation(out=sm.ap[:, 5:6], in_=sm.ap[:, 4:5], func=AF.Exp), reads=[sm], writes=[sm])
                        yield
                        sc.op("dve", lambda h: h.tensor_reduce(out=sm.ap[:, 2:3], in_=sm.ap[:, 10:14], axis=AX.X, op=OP.add),
                              reads=[sm], writes=[sm])
                        sc.op("dve", lambda h: h.reciprocal(out=sm.ap[:, 3:4], in_=sm.ap[:, 2:3]), reads=[sm], writes=[sm])
                        sc.op("dve", lambda h: h.tensor_scalar(out=sm.ap[:, 6:7], in0=sm.ap[:, 5:6], scalar1=1.0, scalar2=None, op0=OP.add),
                              reads=[sm], writes=[sm])
                        sc.op("dve", lambda h: h.reciprocal(out=sm.ap[:, 6:7], in_=sm.ap[:, 6:7]), reads=[sm], writes=[sm])
                        sc.op("dve", lambda h: h.tensor_tensor(out=sm.ap[:, 7:8], in0=sm.ap[:, 6:7], in1=sm.ap[:, 3:4], op=OP.mult),
                              reads=[sm], writes=[sm])
                        sc.op("dve", lambda h: h.tensor_tensor(out=sm.ap[:, 8:9], in0=sm.ap[:, 7:8], in1=sm.ap[:, 5:6], op=OP.mult),
                              reads=[sm], writes=[sm])
                        for k in range(2):
                            for gi in range(4):
                                sc.op("dve", lambda h, k=k, gi=gi: h.tensor_scalar(
                                    out=Ak.ap[:, k, gi * 8:(gi + 1) * 8], in0=mk.ap[:, k, :], scalar1=oh.ap[:, gi:gi + 1], scalar2=None,
                                    op0=OP.mult), reads=[mk, oh], writes=[Ak])
                        sc.op("dve", lambda h: h.tensor_tensor(out=Asum.ap[:, :], in0=Ak.ap[:, 0, :], in1=Ak.ap[:, 1, :], op=OP.add),
                              reads=[Ak], writes=[Asum])
                        yield

                    def tail2(t):
                        g = c * 4 + t
                        d = small[t]
                        sm, Ak, Asum, val, ov, dst, dsti, pay, tokf = (d[k] for k in (
                            "sm", "Ak", "Asum", "val", "ov", "dst", "dsti", "pay", "tokf"))
                        pp = psS[1]
                        sc.op("pe", lambda h: h.matmul(pp.ap[:, 0:32], lhsT=U32, rhs=Asum.ap[:, :], start=True, stop=False),
                              reads=[Asum, T_cf], writes=[pp])
                        sc.op("pe", lambda h: h.matmul(pp.ap[:, 0:32], lhsT=ones32, rhs=Acum.ap[:, :], start=False, stop=True),
                              reads=[Acum, T_cf], writes=[pp])
                        sc.op("dve", lambda h: h.tensor_scalar(out=ov.ap[:, :], in0=pp.ap[:, 0:32], scalar1=float(CAP), scalar2=1.0e6,
                                                               op0=OP.is_ge, op1=OP.mult), reads=[pp], writes=[ov])
                        sc.op("dve", lambda h: h.tensor_tensor(out=val.ap[:, :], in0=pp.ap[:, 0:32], in1=slotbase, op=OP.add),
                              reads=[pp, T_cf], writes=[val])
                        sc.op("dve", lambda h: h.tensor_tensor(out=Acum.ap[:, :], in0=Acum.ap[:, :], in1=Asum.ap[:, :], op=OP.add),
                              reads=[Acum, Asum], writes=[Acum])
                        sc.op("dve", lambda h: h.tensor_tensor(out=val.ap[:, :], in0=val.ap[:, :], in1=ov.ap[:, :], op=OP.add),
                              reads=[val, ov], writes=[val])
                        for k in range(2):
                            sc.op("dve", lambda h, k=k: h.tensor_tensor(out=Ak.ap[:, k, :], in0=Ak.ap[:, k, :], in1=val.ap[:, :], op=OP.mult),
                                  reads=[Ak, val], writes=[Ak])
                            sc.op("dve", lambda h, k=k: h.tensor_reduce(out=dst.ap[:, k:k + 1], in_=Ak.ap[:, k, :], axis=AX.X, op=OP.add),
                                  reads=[Ak], writes=[dst])
                        sc.op("dve", lambda h: h.tensor_copy(out=dsti.ap[:, :], in_=dst.ap[:, :]), reads=[dst], writes=[dsti])
                        sc.op("pool", lambda h: h.tensor_scalar(out=tokf.ap[:, 0:1], in0=iota_p, scalar1=float(g * 128), scalar2=None,
                                                                op0=OP.add), reads=[T_cf], writes=[tokf])
                        sc.op("pool", lambda h: h.tensor_scalar(out=tokf.ap[:, 1:2], in0=iota_p, scalar1=float(g * 128 + S), scalar2=None,
                                                                op0=OP.add), reads=[T_cf], writes=[tokf])
                        sc.op("pool", lambda h: h.memset(pay.ap[:, :, :], 0), writes=[pay])
                        sc.op("pool", lambda h: h.tensor_copy(out=pay.ap[:, 0, 0:1], in_=tokf.ap[:, 0:1]), reads=[tokf], writes=[pay])
                        sc.op("pool", lambda h: h.tensor_copy(out=pay.ap[:, 0, 1:2], in_=tokf.ap[:, 0:1]), reads=[tokf], writes=[pay])
                        sc.op("pool", lambda h: h.tensor_copy(out=pay.ap[:, 1, 0:1], in_=tokf.ap[:, 0:1]), reads=[tokf], writes=[pay])
                        sc.op("pool", lambda h: h.tensor_copy(out=pay.ap[:, 1, 1:2], in_=tokf.ap[:, 1:2]), reads=[tokf], writes=[pay])
                        for k in range(2):
                            sc.op("dve", lambda h, k=k: h.tensor_copy(out=pay.ap[:, k, 2:3].bitcast(F32), in_=sm.ap[:, 7 + k:8 + k]),
                                  reads=[sm], writes=[pay])
                        for k in range(2):
                            sc.dma("pool", lambda h, k=k: h.indirect_dma_start(
                                out=stab_s[:, :], out_offset=bass.IndirectOffsetOnAxis(ap=dsti.ap[:, k:k + 1], axis=0),
                                in_=pay.ap[:, k, :], in_offset=None, bounds_check=reg_ns, oob_is_err=False),
                                reads=[pay, dsti], writes=[T_stab])

                    gens = [tail1(t) for t in range(4)]
                    live = []
                    step = 0
                    while gens or live:
                        if gens and step % 3 == 0:
                            live.append(gens.pop(0))
                        for gnr in list(live):
                            try:
                                next(gnr)
                            except StopIteration:
                                live.remove(gnr)
                        step += 1
                    for t in range(4):
                        tail2(t)

                sc.fence()
        sc.fence()
    if "D" in phases:
        with ExitStack() as ed:
            def sbd(name, shape, dt):
                return ed.enter_context(nc.sbuf_tensor(f"s{next(_uid)}_" + name, list(shape), dt))
            str_ = Ring([T(sbd(f"stb{i}", [128, 2, 4], I32)) for i in range(3)])
            hgr = Ring([T(sbd(f"hg{i}", [128, 2048], F32)) for i in range(2)])
            hgTr = Ring([T(sbd(f"hgT{i}", [128, 16, 256], F32R)) for i in range(2)])
            wer = Ring([T(sbd(f"web{i}", [128, 4096], F32R)) for i in range(7)])
            sgr = Ring([T(sbd(f"sgd{i}", [128, 256], F32)) for i in range(2)])
            actT = sbd("actT", [128, 8, 256], F32R)
            T_act = [T() for _ in range(8)]
            your = Ring([T(sbd(f"yout{i}", [128, 2048], F32)) for i in range(2)])
            psT = Ring(PS[0:2])
            psa = Ring(PS[2:4])
            psb = Ring(PS[4:6])
            psy = Ring(PS[6:8])

            def gather(e):
                stb = str_.next()
                sc.dma("pool", lambda h: h.dma_start(
                    out=stb.ap[:, :, :], in_=stab_s[e * CAP:(e + 1) * CAP, :].rearrange("(s p) c -> p s c", p=128)),
                    reads=[T_stab], writes=[stb])
                hgs = []
                for s in range(2):
                    hg = hgr.next()
                    sc.dma("pool", lambda h, hg=hg, s=s: h.indirect_dma_start(
                        out=hg.ap[:, :], out_offset=None, in_=h_s[:, :],
                        in_offset=bass.IndirectOffsetOnAxis(ap=stb.ap[:, s, 0:1], axis=0)),
                        reads=[stb] + T_h, writes=[hg])
                    hgs.append(hg)
                return stb, hgs

            def transposes(hgs):
                hgT = hgTr.next()
                for s in range(2):
                    hg = hgs[s]
                    for k4 in range(4):
                        tp = psT.next()
                        for ii in range(4):
                            kc = k4 * 4 + ii
                            sc.op("pe", lambda h, tp=tp, ii=ii, kc=kc, hg=hg: h.transpose(
                                out=tp.ap[:, ii * 128:(ii + 1) * 128], in_=hg.ap[:, kc * 128:(kc + 1) * 128], identity=ident32),
                                reads=[hg, T_cf], writes=[tp])
                        if k4 % 2 == 0:
                            sc.op("dve", lambda h, tp=tp, k4=k4, s=s: h.tensor_copy(
                                out=hgT.ap[:, k4 * 4:(k4 + 1) * 4, s * 128:(s + 1) * 128],
                                in_=tp.ap[:, :].rearrange("p (a b) -> p a b", b=128)), reads=[tp], writes=[hgT])
                        else:
                            sc.op("act", lambda h, tp=tp, k4=k4, s=s: h.copy(
                                out=hgT.ap[:, k4 * 4:(k4 + 1) * 4, s * 128:(s + 1) * 128],
                                in_=tp.ap[:, :].rearrange("p (a b) -> p a b", b=128)), reads=[tp], writes=[hgT])
                return hgT

            nxt = gather(0)
            hgT_next = transposes(nxt[1])
            for e in range(NE):
                stb, hgs = nxt
                hgT = hgT_next
                if e + 1 < NE:
                    nxt = gather(e + 1)
                for fb in range(4):
                    W1, W3 = wer.next(), wer.next()
                    sc.dma("sp", lambda h, W1=W1, fb=fb: h.dma_start(
                        out=W1.ap[:, :], in_=w1_d[e, fb].rearrange("p k c -> p (k c)")), writes=[W1])
                    sc.dma("sp", lambda h, W3=W3, fb=fb: h.dma_start(
                        out=W3.ap[:, :], in_=w3_d[e, fb].rearrange("p k c -> p (k c)")), writes=[W3])
                    for half in range(2):
                        fc = fb * 2 + half
                        pa, pb = psa.next(), psb.next()
                        for kc in range(16):
                            sc.op("pe", lambda h, pa=pa, W1=W1, kc=kc, half=half: h.matmul(
                                pa.ap[:, 0:256], lhsT=r32(W1.ap[:, kc * 256 + half * 128:kc * 256 + (half + 1) * 128]), rhs=r32(hgT.ap[:, kc, :]),
                                start=(kc == 0), stop=(kc == 15)), reads=[W1, hgT], writes=[pa])
                        for kc in range(16):
                            sc.op("pe", lambda h, pb=pb, W3=W3, kc=kc, half=half: h.matmul(
                                pb.ap[:, 0:256], lhsT=r32(W3.ap[:, kc * 256 + half * 128:kc * 256 + (half + 1) * 128]), rhs=r32(hgT.ap[:, kc, :]),
                                start=(kc == 0), stop=(kc == 15)), reads=[W3, hgT], writes=[pb])
                        sg = sgr.next()
                        sc.op("act", lambda h, sg=sg, pa=pa: h.activation(out=sg.ap[:, :], in_=pa.ap[:, 0:256], func=AF.Silu),
                              reads=[pa], writes=[sg])
                        sc.op("dve", lambda h, sg=sg, pb=pb, fc=fc: h.tensor_tensor(
                            out=actT[:, fc, :], in0=pb.ap[:, 0:256], in1=sg.ap[:, :], op=OP.mult),
                            reads=[pb, sg], writes=[T_act[fc]])
                if e + 1 < NE:
                    hgT_next = transposes(nxt[1])
                youts = [your.next(), your.next()]
                for cb in range(4):
                    W2 = wer.next()
                    sc.dma("sp", lambda h, W2=W2, cb=cb: h.dma_start(
                        out=W2.ap[:, :], in_=w2_d[e, cb].rearrange("p k c -> p (k c)")), writes=[W2])
                    for s in range(2):
                        py = psy.next()
                        for fc in range(8):
                            sc.op("pe", lambda h, py=py, W2=W2, fc=fc, s=s: h.matmul(
                                py.ap[:, :], lhsT=r32(actT[:, fc, s * 128:(s + 1) * 128]), rhs=r32(W2.ap[:, fc * 512:(fc + 1) * 512]),
                                start=(fc == 0), stop=(fc == 7)), reads=[W2, T_act[fc]], writes=[py])
                        yo = youts[s]
                        sc.op("act", lambda h, py=py, yo=yo, cb=cb, s=s: h.activation(
                            out=yo.ap[:, cb * 512:(cb + 1) * 512], in_=py.ap[:, :], func=AF.Copy,
                            scale=stb.ap[:, s, 2:3].bitcast(F32)), reads=[py, stb], writes=[yo])
                for s in range(2):
                    yo = youts[s]
                    sc.dma("pool", lambda h, yo=yo, s=s: h.indirect_dma_start(
                        out=y_s[:, :], out_offset=bass.IndirectOffsetOnAxis(ap=stb.ap[:, s, 1:2], axis=0),
                        in_=yo.ap[:, :], in_offset=None, bounds_check=reg_y, oob_is_err=False),
                        reads=[yo, stb], writes=[T_y])
        sc.fence()
    if "E" in phases:
        with ExitStack() as ee:
            def sbe(name, shape, dt):
                return ee.enter_context(nc.sbuf_tensor(f"s{next(_uid)}_" + name, list(shape), dt))
            lng = sbe("lng2", [128, 2048], F32)
            lnb = sbe("lnb2", [128, 2048], F32)
            T_ln = T()
            sc.dma("sp", lambda h: h.dma_start(out=lng[:], in_=lnv_d[2:3, :].partition_broadcast(128)), writes=[T_ln])
            sc.dma("sp", lambda h: h.dma_start(out=lnb[:], in_=lnv_d[3:4, :].partition_broadcast(128)), writes=[T_ln])
            hr = Ring([T(sbe(f"he{i}", [128, 2048], F32)) for i in range(4)])
            y0r = Ring([T(sbe(f"y0e{i}", [128, 2048], F32)) for i in range(3)])
            y1r = Ring([T(sbe(f"y1e{i}", [128, 2048], F32)) for i in range(3)])
            smr = Ring([(T(sbe(f"st6e{i}", [128, 4, 6], F32)), T(sbe(f"mve{i}", [128, 2], F32)), T(sbe(f"rse{i}", [128, 2], F32)))
                        for i in range(4)])
            T_out = T()

            def etile(g):
                ht, y0, y1 = hr.next(), y0r.next(), y1r.next()
                st6, mv, rs = smr.next()
                sc.dma("sp", lambda h: h.dma_start(out=ht.ap[:, :], in_=h_s[g * 128:(g + 1) * 128, :]), reads=[T_h[g]], writes=[ht])
                sc.dma("sp", lambda h: h.dma_start(out=y0.ap[:, :], in_=y_s[g * 128:(g + 1) * 128, :]), reads=[T_y], writes=[y0])
                sc.dma("sp", lambda h: h.dma_start(out=y1.ap[:, :], in_=y_s[S + g * 128:S + (g + 1) * 128, :]), reads=[T_y], writes=[y1])
                yield
                sc.op("dve", lambda h: h.scalar_tensor_tensor(
                    out=ht.ap[:, :], in0=ht.ap[:, :], scalar=ALPHA, in1=y0.ap[:, :], op0=OP.mult, op1=OP.add),
                    reads=[ht, y0], writes=[ht])
                sc.op("dve", lambda h: h.tensor_tensor(out=ht.ap[:, :], in0=ht.ap[:, :], in1=y1.ap[:, :], op=OP.add),
                      reads=[ht, y1], writes=[ht])
                for q4 in range(4):
                    sc.op("dve", lambda h, q4=q4: h.bn_stats(out=st6.ap[:, q4, :], in_=ht.ap[:, q4 * 512:(q4 + 1) * 512]),
                          reads=[ht], writes=[st6])
                sc.op("dve", lambda h: h.bn_aggr(out=mv.ap[:, :], in_=st6.ap[:, :, :].rearrange("p a b -> p (a b)")),
                      reads=[st6], writes=[mv])
                sc.op("dve", lambda h: h.tensor_scalar(out=rs.ap[:, 0:1], in0=mv.ap[:, 1:2], scalar1=LN_EPS, scalar2=None, op0=OP.add),
                      reads=[mv], writes=[rs])
                yield
                sc.op("act", lambda h: h.activation(out=rs.ap[:, 0:1], in_=rs.ap[:, 0:1], func=AF.Sqrt), reads=[rs], writes=[rs])
                yield
                sc.op("dve", lambda h: h.reciprocal(out=rs.ap[:, 0:1], in_=rs.ap[:, 0:1]), reads=[rs], writes=[rs])
                sc.op("dve", lambda h: h.tensor_scalar(out=rs.ap[:, 1:2], in0=mv.ap[:, 0:1], scalar1=rs.ap[:, 0:1], scalar2=-1.0,
                                                       op0=OP.mult, op1=OP.mult), reads=[mv, rs], writes=[rs])
                yield
                sc.op("act", lambda h: h.activation(out=ht.ap[:, :], in_=ht.ap[:, :], func=AF.Identity,
                                                    scale=rs.ap[:, 0:1], bias=rs.ap[:, 1:2]), reads=[ht, rs], writes=[ht])
                yield
                sc.op("pool", lambda h: h.tensor_tensor(out=ht.ap[:, :], in0=ht.ap[:, :], in1=lng[:, :], op=OP.mult),
                      reads=[ht, T_ln], writes=[ht])
                yield
                sc.op("dve", lambda h: h.tensor_tensor(out=ht.ap[:, :], in0=ht.ap[:, :], in1=lnb[:, :], op=OP.add),
                      reads=[ht, T_ln], writes=[ht])
                sc.dma("pool", lambda h: h.dma_start(out=out_d[g * 128:(g + 1) * 128, :], in_=ht.ap[:, :]),
                       reads=[ht], writes=[T()])
                yield

            pending = list(range(16))
            live = []
            while pending or live:
                if pending and len(live) < 3:
                    live.append(etile(pending.pop(0)))
                for gnr in list(live):
                    try:
                        next(gnr)
                    except StopIteration:
                        live.remove(gnr)
        sc.fence()
        sc.fence()
    sc.finish()
    es.close()
    return nc


def _st_blocks(w, cols=128):
    K, N = w.shape
    return np.ascontiguousarray(w.reshape(K // 128, 128, N // cols, cols).transpose(2, 1, 0, 3))


def _consts():
    half = 64
    inv_freq = np.power(np.float32(10000.0), -np.arange(half, dtype=np.float32) * np.float32(2.0 / 128)).astype(np.float32)
    ang = np.arange(S, dtype=np.float32)[:, None] * inv_freq[None, :]
    cos = np.cos(ang).astype(np.float32).T
    sin = np.sin(ang).astype(np.float32).T
    rope = np.concatenate([np.concatenate([cos, cos], 0), np.concatenate([sin, sin], 0)], axis=1)
    cf = np.zeros((128, 1024), np.float32)
    cf[:, 0:128] = np.eye(128, dtype=np.float32)
    p = np.arange(128)
    for i in range(16):
        blk = i // 2
        for n in range(8):
            cf[:, 128 + i * 8 + n] = 0.0 if n < blk else -1.0e30
            cf[:, 256 + i * 8 + n] = 0.0 if n == blk else NEG
    cf[:, 384:512] = 1.0 / 2048.0
    cf[:, 512:640] = (p[:, None] < p[None, :]).astype(np.float32)
    cf[:, 640:768] = 1.0
    cf[:, 768:800] = (np.arange(32) * CAP).astype(np.float32)[None, :]
    cf[:, 800] = p.astype(np.float32)
    cb = np.zeros((128, 2688), np.float32)
    for d in range(64):
        cb[d + 64, d] = -1.0
        cb[d, 64 + d] = 1.0
    cb[:, 128:256] = np.eye(128)
    cb[:, 256:384] = np.where(p[:, None] <= p[None, :], 0.0, NEG)
    cb[:, 384:512] = 1.0
    k = np.arange(S)
    for n in range(8):
        cb[n, 640:2688] = (k // 256 == n).astype(np.float32)
    sinit = np.zeros((NS, 4), np.int32)
    sinit[:, 1] = OOBROW
    return rope, cf, cb.astype(ml_dtypes.bfloat16), sinit


def _prep(inp):
    f = lambda a: np.asarray(a, dtype=np.float32)
    w_in = f(inp["w_in"])[0]
    b_in = f(inp["b_in"])[0]
    sh = {}
    sh["wA"] = _st_blocks(w_in[:, 0:4096])
    sh["wV"] = _st_blocks(w_in[:, 4096:6144], 256)
    sh["wG"] = _st_blocks(w_in[:, 6144:14336])
    sh["bq"] = np.ascontiguousarray(b_in[0:4096].reshape(32, 128).T)
    sh["bv"] = np.ascontiguousarray(b_in[4096:6144].reshape(1, 2048))
    sh["bG"] = np.ascontiguousarray(b_in[6144:14336].reshape(64, 128).T)
    sh["wo"] = _st_blocks(f(inp["w_o_attn"])[0])
    sh["wp"] = _st_blocks(f(inp["w_pw2"])[0])
    sh["wout"] = _st_blocks(f(inp["w_out"])[0], 512)
    cvec = np.zeros((128, 16, 36), np.float32)
    cvec[:, :, 0:31] = f(inp["w_dw"])[0].T.reshape(16, 128, 31).transpose(1, 0, 2)
    for i, nm in enumerate(["b_dw", "conv_ln_g", "conv_ln_b", "b_pw2"]):
        cvec[:, :, 31 + i] = f(inp[nm])[0].reshape(16, 128).T
    sh["cvec"] = cvec
    sh["lnv"] = np.ascontiguousarray(np.stack([f(inp["ln1_g"])[0], f(inp["ln1_b"])[0], f(inp["ln2_g"])[0], f(inp["ln2_b"])[0]]))
    wr = np.concatenate([f(inp["w_rg"])[0]] + [f(inp["w_re"])[0, g] for g in range(4)], axis=1)
    sh["wr"] = np.ascontiguousarray(wr.reshape(16, 128, 36).transpose(1, 0, 2))
    sh["br"] = np.concatenate([f(inp["b_rg"])[0], f(inp["b_re"])[0].reshape(-1)]).reshape(1, 36).astype(np.float32)
    w1 = f(inp["w1"])[0]
    w3 = f(inp["w3"])[0]
    w2 = f(inp["w2"])[0]
    sh["w1"] = np.ascontiguousarray(w1.reshape(NE, 16, 128, 4, 256).transpose(0, 3, 2, 1, 4))
    sh["w3"] = np.ascontiguousarray(w3.reshape(NE, 16, 128, 4, 256).transpose(0, 3, 2, 1, 4))
    sh["w2"] = np.ascontiguousarray(w2.reshape(NE, 8, 128, 4, 512).transpose(0, 3, 2, 1, 4))
    rope, cf, cb, sinit = _consts()
    sh["rope"], sh["cf"], sh["cb"], sh["sinit"] = rope, cf, cb, sinit
    sh["onesn"] = np.full((128, 128), 1.0 / 2048.0, np.float32)
    return sh


def kernel(**inputs):
    x = np.asarray(inputs["x"], dtype=np.float32)
    shared = _prep(inputs)
    nc = build("ABCDE", dbg=False)
    in_maps = []
    for b in range(8):
        m = dict(shared)
        m["x"] = np.ascontiguousarray(x[b])
        m["xT"] = np.ascontiguousarray(x[b].T)
        in_maps.append(m)
    res = run_bass_kernel_spmd(nc, in_maps, core_ids=list(range(8)))
    return np.stack([np.asarray(r["out"], dtype=np.float32) for r in res.results], axis=0)
```
